# Optimizing a Trainium2 kernel written in Bass

```python
import jax, jax.numpy as jnp
from jax import lax
import numpy as np

D_MODEL = 2048
BATCH = 2
SEQ = 8192
DEPTH = 1

GRID_W = 64
CTX_LEN = 256
D_MIX = D_MODEL
D_A = D_MIX // 2
D_B = D_MIX - D_A
LRU_HEADS = 8
LRU_BLK = D_A // LRU_HEADS
LRU_C = 8.0
CONV_A_W = 4
CONV_A_LEFT = 2
CONV_B_W = 3
CONV_B_LEFT = 1
N_EXPERTS = 32
TOP_K = 4
D_FF = D_MODEL
SWIGLU_LIMIT = 7.0
SWIGLU_ALPHA = 1.702
MOE_BLOCK = 256
EPS = 1e-6
N_IN = 2 * D_A + 3 * D_B

kernel_name = "hybrid_rglru_shortconv_moe_dit"


def rms_norm(x, g):
    xf = x.astype(jnp.float32)
    y = xf * lax.rsqrt(jnp.mean(xf * xf, axis=-1, keepdims=True) + EPS)
    return (y * g.astype(jnp.float32)).astype(x.dtype)


def modulate(x, shift, scale):
    return x * (1 + scale) + shift


def dwconv(x, w, axis, left):
    k_w = w.shape[0]
    n = x.shape[axis]
    pad = [(0, 0)] * x.ndim
    pad[axis] = (left, k_w - 1 - left)
    xp = jnp.pad(x, pad)
    out = lax.slice_in_dim(xp, 0, n, axis=axis) * w[0]
    for k in range(1, k_w):
        out = out + lax.slice_in_dim(xp, k, k + n, axis=axis) * w[k]
    return out


def conv_b_latent(z, w):
    bsz, n, ch = z.shape
    rows = n // GRID_W
    half = ch // 2
    zg = z.reshape(bsz, rows, GRID_W, ch)
    horiz = dwconv(zg[..., :half], w[:, :half], 2, CONV_B_LEFT)
    vert = dwconv(zg[..., half:], w[:, half:], 1, CONV_B_LEFT)
    return jnp.concatenate([horiz, vert], axis=-1).reshape(bsz, n, ch)


def split_in(u):
    return jnp.split(u, [D_A, 2 * D_A, 2 * D_A + D_B, 2 * D_A + 2 * D_B], axis=-1)


def rglru_coeffs(xa, conv_w, conv_b, w_r, b_r, w_i, b_i, lam):
    xc = dwconv(xa, conv_w, 1, CONV_A_LEFT) + conv_b
    bsz, n, _ = xc.shape
    xh = xc.reshape(bsz, n, LRU_HEADS, LRU_BLK)
    r = jax.nn.sigmoid(jnp.einsum('blhi,dhij->dblhj', xh, w_r).reshape(2, bsz, n, D_A) + b_r[:, None, None])
    i = jax.nn.sigmoid(jnp.einsum('blhi,dhij->dblhj', xh, w_i).reshape(2, bsz, n, D_A) + b_i[:, None, None])
    log_a = -LRU_C * r.astype(jnp.float32) * jax.nn.softplus(-lam.astype(jnp.float32))[:, None, None]
    a = jnp.exp(log_a)
    b = jnp.sqrt(-jnp.expm1(2.0 * log_a)) * (i.astype(jnp.float32) * xc.astype(jnp.float32)[None])
    return a, b


def linear_scan(a, b, h0, reverse):
    def combine(lhs, rhs):
        return lhs[0] * rhs[0], rhs[0] * lhs[1] + rhs[1]
    a_cum, b_cum = lax.associative_scan(combine, (a, b), axis=1, reverse=reverse)
    return a_cum * h0[:, None] + b_cum


def mixer_out(y_a, y_b, g_out_a, g_out_b, w_out):
    y = jnp.concatenate([rms_norm(y_a, g_out_a), rms_norm(y_b, g_out_b)], axis=-1)
    return y @ w_out


def moe_ffn(xt, w_router, b_router, w_gate, b_gate, w_up, b_up, w_down, b_down):
    n_tok, d = xt.shape
    logits = (xt @ w_router + b_router).astype(jnp.float32)
    top_v, top_e = lax.top_k(logits, TOP_K)
    top_w = jax.nn.softmax(top_v, axis=-1).astype(xt.dtype)
    n_slot = n_tok * TOP_K
    flat_e = top_e.reshape(n_slot)
    order = jnp.argsort(flat_e)
    sorted_e = flat_e[order]
    slot_tok = (order // TOP_K).astype(jnp.int32)
    slot_w = top_w.reshape(n_slot)[order]
    counts = jnp.bincount(flat_e, length=N_EXPERTS)
    padded = (counts + MOE_BLOCK - 1) // MOE_BLOCK * MOE_BLOCK
    pad_end = jnp.cumsum(padded)
    pad_start = pad_end - padded
    grp_start = jnp.cumsum(counts) - counts
    dest = pad_start[sorted_e] + jnp.arange(n_slot, dtype=jnp.int32) - grp_start[sorted_e]
    n_blocks = -(-n_slot // MOE_BLOCK) + N_EXPERTS
    n_buf = n_blocks * MOE_BLOCK
    buf_tok = jnp.zeros((n_buf,), jnp.int32).at[dest].set(slot_tok)
    buf_w = jnp.zeros((n_buf,), xt.dtype).at[dest].set(slot_w)
    block_e = jnp.minimum(
        jnp.searchsorted(pad_end, jnp.arange(n_blocks, dtype=jnp.int32) * MOE_BLOCK, side='right'),
        N_EXPERTS - 1)

    def expert_block(args):
        idx, wts, e = args
        h = jnp.take(xt, idx, axis=0)
        gate = jnp.minimum(h @ w_gate[e] + b_gate[e], SWIGLU_LIMIT)
        up = jnp.clip(h @ w_up[e] + b_up[e], -SWIGLU_LIMIT, SWIGLU_LIMIT)
        act = (up + 1) * gate * jax.nn.sigmoid(SWIGLU_ALPHA * gate)
        return (act @ w_down[e] + b_down[e]) * wts[:, None]

    y = lax.map(expert_block, (buf_tok.reshape(n_blocks, MOE_BLOCK),
                               buf_w.reshape(n_blocks, MOE_BLOCK), block_e))
    return jax.ops.segment_sum(y.reshape(n_buf, d), buf_tok, num_segments=n_tok)


def setup_inputs(seed: int = 0) -> dict:
    key = jax.random.key(seed)
    ks = jax.random.split(key, 32)
    f32 = jnp.float32

    def nrm(k, shape, scale):
        return jax.random.normal(k, shape, f32) * scale

    u = jax.random.uniform(ks[14], (DEPTH, 2, D_A), f32, minval=0.9, maxval=0.999)
    a0 = u ** (1.0 / LRU_C)
    lru_lam = jnp.log(a0) - jnp.log1p(-a0)
    return {
        "x": nrm(ks[0], (BATCH, SEQ, D_MODEL), 1.0),
        "c": nrm(ks[1], (BATCH, D_MODEL), 1.0),
        "ctx": nrm(ks[2], (BATCH, CTX_LEN, D_MODEL), 1.0),
        "c_ctx": nrm(ks[3], (D_MODEL,), 1.0),
        "w_mod": nrm(ks[4], (DEPTH, D_MODEL, 6 * D_MODEL), 0.5 * D_MODEL ** -0.5),
        "b_mod": nrm(ks[5], (DEPTH, 6 * D_MODEL), 0.02),
        "g_mix": 1.0 + nrm(ks[6], (DEPTH, D_MODEL), 0.02),
        "w_in": nrm(ks[7], (DEPTH, D_MODEL, N_IN), D_MODEL ** -0.5),
        "conv_a_w": nrm(ks[8], (DEPTH, CONV_A_W, D_A), CONV_A_W ** -0.5),
        "conv_a_b": nrm(ks[9], (DEPTH, D_A), 0.02),
        "lru_w_r": nrm(ks[10], (DEPTH, 2, LRU_HEADS, LRU_BLK, LRU_BLK), LRU_BLK ** -0.5),
        "lru_b_r": nrm(ks[11], (DEPTH, 2, D_A), 0.02),
        "lru_w_i": nrm(ks[12], (DEPTH, 2, LRU_HEADS, LRU_BLK, LRU_BLK), LRU_BLK ** -0.5),
        "lru_b_i": nrm(ks[13], (DEPTH, 2, D_A), 0.02),
        "lru_lam": lru_lam,
        "conv_b_w": nrm(ks[15], (DEPTH, CONV_B_W, D_B), CONV_B_W ** -0.5),
        "g_out_a": 1.0 + nrm(ks[16], (DEPTH, D_A), 0.02),
        "g_out_b": 1.0 + nrm(ks[17], (DEPTH, D_B), 0.02),
        "w_out": nrm(ks[18], (DEPTH, D_MIX, D_MODEL), D_MIX ** -0.5),
        "g_ffn": 1.0 + nrm(ks[19], (DEPTH, D_MODEL), 0.02),
        "w_router": nrm(ks[20], (DEPTH, D_MODEL, N_EXPERTS), D_MODEL ** -0.5),
        "b_router": nrm(ks[21], (DEPTH, N_EXPERTS), 0.01),
        "w_gate": nrm(ks[22], (DEPTH, N_EXPERTS, D_MODEL, D_FF), D_MODEL ** -0.5),
        "b_gate": nrm(ks[23], (DEPTH, N_EXPERTS, D_FF), 0.01),
        "w_up": nrm(ks[24], (DEPTH, N_EXPERTS, D_MODEL, D_FF), D_MODEL ** -0.5),
        "b_up": nrm(ks[25], (DEPTH, N_EXPERTS, D_FF), 0.01),
        "w_down": nrm(ks[26], (DEPTH, N_EXPERTS, D_FF, D_MODEL), D_FF ** -0.5),
        "b_down": nrm(ks[27], (DEPTH, N_EXPERTS, D_MODEL), 0.01),
        "g_final": 1.0 + nrm(ks[28], (D_MODEL,), 0.02),
    }


def reference(x, c, ctx, c_ctx, w_mod, b_mod, g_mix, w_in, conv_a_w, conv_a_b, lru_w_r, lru_b_r,
              lru_w_i, lru_b_i, lru_lam, conv_b_w, g_out_a, g_out_b, w_out, g_ffn, w_router,
              b_router, w_gate, b_gate, w_up, b_up, w_down, b_down, g_final):
    bsz, n_lat, d = x.shape
    n_ctx = ctx.shape[1]
    s = ctx
    for l in range(DEPTH):
        last = l == DEPTH - 1
        mod = jax.nn.silu(c) @ w_mod[l] + b_mod[l]
        mod_s = jax.nn.silu(c_ctx) @ w_mod[l] + b_mod[l]
        sh1, sc1, gt1, sh2, sc2, gt2 = [m[:, None] for m in jnp.split(mod, 6, axis=-1)]
        ssh1, ssc1, sgt1, ssh2, ssc2, sgt2 = jnp.split(mod_s, 6, axis=-1)
        lru_p = (conv_a_w[l], conv_a_b[l], lru_w_r[l], lru_b_r[l], lru_w_i[l], lru_b_i[l], lru_lam[l])
        zero_state = jnp.zeros((bsz, D_A), jnp.float32)

        sn = modulate(rms_norm(s, g_mix[l]), ssh1, ssc1)
        if last:
            s_ax = sn @ w_in[l][:, D_A:2 * D_A]
        else:
            s_ag, s_ax, s_bb, s_bc, s_bh = split_in(sn @ w_in[l])
        a_s, b_s = rglru_coeffs(s_ax, *lru_p)
        hf_s = linear_scan(a_s[0], b_s[0], zero_state, False)
        hb_s = linear_scan(a_s[1], b_s[1], zero_state, True)
        if not last:
            ys_a = jax.nn.gelu(s_ag) * (hf_s + hb_s).astype(s.dtype)
            ys_b = s_bb * dwconv(s_bc * s_bh, conv_b_w[l], 1, CONV_B_LEFT)
            s = s + sgt1 * mixer_out(ys_a, ys_b, g_out_a[l], g_out_b[l], w_out[l])

        xn = modulate(rms_norm(x, g_mix[l]), sh1, sc1)
        x_ag, x_ax, x_bb, x_bc, x_bh = split_in(xn @ w_in[l])
        a_x, b_x = rglru_coeffs(x_ax, *lru_p)
        hf = linear_scan(a_x[0], b_x[0], hf_s[:, -1], False)
        hb = linear_scan(a_x[1], b_x[1], hb_s[:, 0], True)
        y_a = jax.nn.gelu(x_ag) * (hf + hb).astype(x.dtype)
        y_b = x_bb * conv_b_latent(x_bc * x_bh, conv_b_w[l])
        x = x + gt1 * mixer_out(y_a, y_b, g_out_a[l], g_out_b[l], w_out[l])

        moe_p = (w_router[l], b_router[l], w_gate[l], b_gate[l], w_up[l], b_up[l], w_down[l], b_down[l])
        xn = modulate(rms_norm(x, g_ffn[l]), sh2, sc2).reshape(bsz * n_lat, d)
        if last:
            x = x + gt2 * moe_ffn(xn, *moe_p).reshape(bsz, n_lat, d)
        else:
            sn = modulate(rms_norm(s, g_ffn[l]), ssh2, ssc2).reshape(bsz * n_ctx, d)
            out = moe_ffn(jnp.concatenate([xn, sn], axis=0), *moe_p)
            x = x + gt2 * out[:bsz * n_lat].reshape(bsz, n_lat, d)
            s = s + sgt2 * out[bsz * n_lat:].reshape(bsz, n_ctx, d)
    return rms_norm(x, g_final)
```

```python
import numpy as np
from contextlib import ExitStack
import concourse.bass as bass
import concourse.mybir as mybir
from concourse.bass_utils import run_bass_kernel_spmd

F32 = mybir.dt.float32
BF16 = mybir.dt.bfloat16
AF = mybir.ActivationFunctionType
ALU = mybir.AluOpType
AX = mybir.AxisListType

D = 2048
L = 2048
HALO = 64
W = L + 2 * HALO
LC = 256
WC = LC + 2 * HALO
NCORE = 8
EPS = 1e-6
NEXP = 32
WIN_L = [LC, L, L, L, L]
WIN_ROW = [0, WC, WC + W, WC + 2 * W, WC + 3 * W]
XROWS = WC + 4 * W

_o = 0
def _col(n):
    global _o
    r = _o
    _o += n
    return r
C_CT = _col(16); C_CX = _col(16); C_CAW = _col(32); C_CAB = _col(8); C_LBR = _col(16); C_LBI = _col(16)
C_LAM = _col(16); C_CBW = _col(24); C_GOA = _col(8); C_GOB = _col(8); C_HM = _col(10); C_CM = _col(6)
C_BG = _col(512); C_BU = _col(512); C_BRT = _col(32); C_ID = _col(128); C_ONE = _col(128)
NS = _o

ENGS = ("pe", "act", "dve", "pool", "sp")
SAME_ENGINE_SYNC = True


class Buf:
    __slots__ = ("name", "w", "r")

    def __init__(self, name=""):
        self.name = name
        self.w = None
        self.r = {}


class Sem:
    def __init__(self, h, name):
        self.h = h
        self.n = 0
        self.name = name


class Prog:
    def __init__(self):
        self.ops = {e: [] for e in ENGS}
        self.esem = {}
        self.waited = {e: {} for e in ENGS}
        self.pending = {e: [] for e in ENGS}
        self.allsems = []

    def op(self, eng, fn, reads=(), writes=(), dma=None):
        deps = self.pending[eng]
        self.pending[eng] = []
        for b in reads:
            if b.w is not None:
                deps.append(b.w)
        for b in writes:
            if b.w is not None:
                deps.append(b.w)
            deps.extend(b.r.values())
        wd = self.waited[eng]
        best = {}
        own = self.esem.get(eng)
        for (s, v) in deps:
            if (not SAME_ENGINE_SYNC) and s is own:
                continue
            if wd.get(id(s), 0) >= v:
                continue
            if id(s) not in best or best[id(s)][1] < v:
                best[id(s)] = (s, v)
        waits = []
        for s, v in best.values():
            waits.append((s, v))
            wd[id(s)] = v
        if dma is not None:
            dma.n += 16
            ev = (dma, dma.n)
            inc = (dma, 16)
        else:
            s = self.esem[eng]
            s.n += 1
            ev = (s, s.n)
            inc = (s, 1)
        for b in reads:
            old = b.r.get(id(ev[0]))
            if old is None or old[1] < ev[1]:
                b.r[id(ev[0])] = ev
        for b in writes:
            b.w = ev
            b.r = {}
        self.ops[eng].append((waits, fn, inc))
        return ev

    def barrier(self):
        for e in ENGS:
            self.pending[e] = [(s, s.n) for s in self.allsems if s.n > 0]

    def final_wait(self, eng, sems):
        waits = [(s, s.n) for s in sems if s.n > 0]
        self.ops[eng].append((waits, None, None))

    def emit(self, block):
        def mk(name):
            lst = self.ops[name]

            def body(eng):
                for waits, fn, inc in lst:
                    for s, v in waits:
                        eng.wait_ge(s.h, v)
                    if fn is not None:
                        ins = fn(eng)
                        ins.then_inc(inc[0].h, inc[1])
            return body
        block.sync(mk("sp"))
        block.tensor(mk("pe"))
        block.scalar(mk("act"))
        block.vector(mk("dve"))
        block.gpsimd(mk("pool"))


def rev(ap):
    a = ap.ap
    assert len(a) == 2 and a[1][0] == 1, a
    n = a[1][1]
    return bass.AP(ap.tensor, ap.offset + (n - 1), [list(a[0]), [-1, n]])


def chunks(lo, hi, step=512):
    out = []
    while lo < hi:
        out.append((lo, min(lo + step, hi)))
        lo += step
    return out


def build(stage=3, stop=None, nq=4):
    nc = bass.Bass("TRN2", target_bir_lowering=False)
    P = Prog()
    es = ExitStack()

    def din(name, shape, dt=F32):
        return nc.dram_tensor(name, list(shape), dt, kind="ExternalInput").ap()

    x_all = din("x_all", [XROWS, D])
    smallc_d = din("smallc", [128, NS])
    wmod_d = din("w_mod", [D, 6 * D])
    bmod_d = din("b_mod", [1, 6 * D])
    gmix_d = din("g_mix_b", [128, D])
    gffn_d = din("g_ffn_b", [128, D])
    gfin_d = din("g_fin_b", [128, D])
    win_d = din("w_in_h", [40, 128, 16 * 128])
    wr_d = din("wr_h", [128, 16 * 128])
    wi_d = din("wi_h", [128, 16 * 128])
    wout_d = din("w_out", [D, D])
    if stage >= 3:
        wrt_d = din("w_router_h", [128, 16 * 32])
        wgu_d = din("wgu_h", [NEXP * 16, 128, 2 * 16 * 128])
        wd_d = din("w_down", [NEXP, D, D])
        bd_d = din("b_down", [NEXP, D])
    out_d = nc.dram_tensor("out", [L, D], F32, kind="ExternalOutput").ap()
    modb_d = nc.dram_tensor("modb", [8, 128, D], F32).ap()
    y_d = nc.dram_tensor("y_scr", [16, 128, L], BF16).ap()
    x1_d = nc.dram_tensor("x1_scr", [L, D], F32).ap()

    ARENA = 52000
    arena = es.enter_context(nc.sbuf_tensor("arena", [128, ARENA], F32))
    PS = [es.enter_context(nc.psum_tensor(f"ps{i}", [128, 512], F32)) for i in range(8)]
    PB = [Buf(f"ps{i}") for i in range(8)]
    for e in ENGS:
        P.esem[e] = Sem(es.enter_context(nc.semaphore(f"s_{e}")), e)
        P.allsems.append(P.esem[e])
    nsem = [0]

    def newsem(name):
        nsem[0] += 1
        s = Sem(es.enter_context(nc.semaphore(f"d{nsem[0]}_{name}")), name)
        P.allsems.append(s)
        return s

    bank_ctr = [0]

    def nbank():
        b = bank_ctr[0] % 8
        bank_ctr[0] += 1
        return b

    top = [0]

    def alloc(n_f32, name=""):
        a = top[0]
        top[0] += n_f32
        assert top[0] <= ARENA, (name, top[0])
        return arena[:, a:a + n_f32]

    def alloc_bf(n_bf, name=""):
        assert n_bf % 2 == 0
        return alloc(n_bf // 2, name).bitcast(BF16)

    def dma(q, out, in_, sem, reads=(), writes=()):
        return P.op(q, lambda e: e.dma_start(out=out, in_=in_), reads, writes, dma=sem)

    def act(out, in_, func, reads, writes, bias=None, scale=None, accum_out=None):
        kw = {}
        if bias is not None:
            kw["bias"] = bias
        if scale is not None:
            kw["scale"] = scale
        if accum_out is not None:
            kw["accum_out"] = accum_out
        return P.op("act", lambda e: e.activation(out=out, in_=in_, func=func, **kw), reads, writes)

    def ts(eng, out, in0, s1, s2, op0, op1, reads, writes):
        if s2 is None:
            return P.op(eng, lambda e: e.tensor_scalar(out=out, in0=in0, scalar1=s1, scalar2=None, op0=op0), reads, writes)
        return P.op(eng, lambda e: e.tensor_scalar(out=out, in0=in0, scalar1=s1, scalar2=s2, op0=op0, op1=op1), reads, writes)

    def tt(eng, out, in0, in1, op, reads, writes):
        return P.op(eng, lambda e: e.tensor_tensor(out=out, in0=in0, in1=in1, op=op), reads, writes)

    def stt(eng, out, in0, scalar, in1, op0, op1, reads, writes):
        return P.op(eng, lambda e: e.scalar_tensor_tensor(out=out, in0=in0, scalar=scalar, in1=in1, op0=op0, op1=op1), reads, writes)

    def mmg(items, reads, writes):
        def fn(e):
            ins = None
            for (o, l, r, s, t) in items:
                ins = e.matmul(o, l, r, start=s, stop=t)
            return ins
        return P.op("pe", fn, reads, writes)

    def trg(items, reads, writes):
        def fn(e):
            ins = None
            for (o, i_, idn) in items:
                ins = e.transpose(o, i_, idn)
            return ins
        return P.op("pe", fn, reads, writes)

    class Ring:
        def __init__(self, n, n_f32, name, bf=False, shape=None):
            self.slots = []
            for i in range(n):
                ap = alloc(n_f32, name)
                if bf:
                    ap = ap.bitcast(BF16)
                self.slots.append((ap, Buf(f"{name}{i}"), newsem(f"{name}{i}")))
            self.i = 0

        def next(self):
            s = self.slots[self.i % len(self.slots)]
            self.i += 1
            return s

    def finish_early():
        S_e = newsem("early")
        P.barrier()
        dma("sp", out_d[0:128, :], arena[:, 0:2048], S_e)
        P.final_wait("sp", [S_e])
        with nc.Block() as block:
            P.emit(block)
        es.close()
        return nc

    smallc = alloc(NS, "smallc")
    B_smallc = Buf("smallc")
    S_const = newsem("const")
    dma("sp", smallc, smallc_d[:, :], S_const, writes=[B_smallc])
    wr_bf = alloc_bf(2048, "wr")
    wi_bf = alloc_bf(2048, "wi")
    B_wri = Buf("wri")
    S_wri = newsem("wri")
    dma("pool", wr_bf, wr_d[:, :], S_wri, writes=[B_wri])
    dma("pool", wi_bf, wi_d[:, :], S_wri, writes=[B_wri])
    if stage >= 3:
        wrt_bf = alloc_bf(512, "wrt")
        B_wrt = Buf("wrt")
        S_wrt = newsem("wrt")
        dma("pool", wrt_bf, wrt_d[:, :], S_wrt, writes=[B_wrt])
    ident_bf = alloc_bf(128, "identbf")
    B_ident = Buf("ident")
    smalld = alloc(512, "smalld")
    ident_f = smallc[:, C_ID:C_ID + 128]
    ones_f = smallc[:, C_ONE:C_ONE + 128]
    P.op("act", lambda e: e.activation(out=ident_bf, in_=ident_f, func=AF.Copy), [B_smallc], [B_ident])
    SD_SC = 0; SD_SX = 16; SD_CNEG = 32; SD_CARRY = 48; SD_RSTD = 64; SD_SUMH = 128; SD_SUMA = 208; SD_TMP = 288
    B_sc = Buf("sc"); B_cneg = Buf("cneg"); B_carry = Buf("carry"); B_rstd = Buf("rstdab")
    B_sum = [Buf(f"sum{w}") for w in range(5)]
    sc = smalld[:, SD_SC:SD_SC + 16]
    sx = smalld[:, SD_SX:SD_SX + 16]
    cneg = smalld[:, SD_CNEG:SD_CNEG + 16]
    carry = smalld[:, SD_CARRY:SD_CARRY + 16]
    act(sc, smallc[:, C_CT:C_CT + 16], AF.Silu, [B_smallc], [B_sc])
    act(sx, smallc[:, C_CX:C_CX + 16], AF.Silu, [B_smallc], [B_sc])
    act(cneg, smallc[:, C_LAM:C_LAM + 16], AF.Exp, [B_smallc], [B_cneg], scale=-1.0)
    act(cneg, cneg, AF.Ln, [B_cneg], [B_cneg], bias=1.0)
    ts("dve", cneg, cneg, -8.0, None, ALU.mult, None, [B_cneg], [B_cneg])
    persist_top = top[0]

    cb_l = alloc(2048, "cb_l").rearrange("p (k m) -> p k m", k=16)
    cx_l = alloc(2048, "cx_l").rearrange("p (k m) -> p k m", k=16)
    B_cbl = Buf("cbl")
    for k in range(16):
        ts("dve", cb_l[:, k, :], ones_f, sc[:, k:k + 1], None, ALU.mult, None, [B_smallc, B_sc], [B_cbl])
        ts("dve", cx_l[:, k, :], ones_f, sx[:, k:k + 1], None, ALU.mult, None, [B_smallc, B_sc], [B_cbl])
    wm_ring = Ring(3, 2048, "wm")
    ev_ring = Ring(2, 2048, "ev")
    bm_ring = Ring(2, 2048, "bm")
    gbuf = alloc(2048, "gbuf")
    B_gbuf = Buf("gbuf")
    S_g = newsem("gload")
    for g in range(6):
        use_ctx = g < 2
        bm_ap, bm_b, bm_s = bm_ring.next()
        dma("sp", bm_ap[0:1, :], bmod_d[0:1, g * D:(g + 1) * D], bm_s, writes=[bm_b])
        if g in (1, 4):
            dma("sp", gbuf, (gmix_d if g == 1 else gffn_d)[:, :], S_g, writes=[B_gbuf])
        banks = [nbank() for _ in range(4)]
        xbanks = [nbank() for _ in range(4)] if use_ctx else []
        for k in range(16):
            w_ap, w_b, w_s = wm_ring.next()
            dma("sp", w_ap, wmod_d[k * 128:(k + 1) * 128, g * D:(g + 1) * D], w_s, writes=[w_b])
            for n in range(4):
                mmg([(PS[banks[n]][:, :], cb_l[:, k, :], w_ap[:, n * 512:(n + 1) * 512], k == 0, False)],
                    [w_b, B_cbl], [PB[banks[n]]])
                if use_ctx:
                    mmg([(PS[xbanks[n]][:, :], cx_l[:, k, :], w_ap[:, n * 512:(n + 1) * 512], k == 0, False)],
                        [w_b, B_cbl], [PB[xbanks[n]]])
        for n in range(4):
            mmg([(PS[banks[n]][:, :], ones_f[0:1, :], bm_ap[0:1, n * 512:(n + 1) * 512], False, True)],
                [bm_b, B_smallc], [PB[banks[n]]])
            if use_ctx:
                mmg([(PS[xbanks[n]][:, :], ones_f[0:1, :], bm_ap[0:1, n * 512:(n + 1) * 512], False, True)],
                    [bm_b, B_smallc], [PB[xbanks[n]]])
        for (bks, idx) in ((banks, g), (xbanks, 6 + g)):
            if not bks:
                continue
            e_ap, e_b, e_s = ev_ring.next()
            for n in range(4):
                dst = e_ap[:, n * 512:(n + 1) * 512]
                if g in (1, 4):
                    stt("dve", dst, PS[bks[n]][:, :], 1.0, gbuf[:, n * 512:(n + 1) * 512], ALU.add, ALU.mult,
                        [PB[bks[n]], B_gbuf], [e_b])
                else:
                    act(dst, PS[bks[n]][:, :], AF.Copy, [PB[bks[n]]], [e_b])
            dma("sp", modb_d[idx], e_ap, e_s, reads=[e_b], writes=[])
    P.barrier()
    if stop == "p0":
        return finish_early()
    top[0] = persist_top
    xnT = alloc_bf(16 * W, "xnT").rearrange("p (k n) -> p k n", k=16)
    B_xnT = Buf("xnT")
    gs_b = alloc(2048, "gs_b")
    sh_b = alloc(2048, "sh_b")
    B_gs = Buf("gs")
    S_gs = newsem("gs")
    w_ring = Ring(3, 1024, "wring", bf=True)
    acc_a = alloc(2048, "acc_a")
    acc_b = alloc(2048, "acc_b")
    B_acca = Buf("acca"); B_accb = Buf("accb")
    ph1_top = top[0]
    xs_ring = Ring(2, 2048, "xs")
    t1 = alloc(2048, "t1"); B_t1 = Buf("t1")
    xnb_ring = Ring(2, 1024, "xnb", bf=True)
    junk = alloc_bf(2048, "junk"); B_junk = Buf("junk")
    top[0] = ph1_top
    T0 = alloc(W, "T0"); T1 = alloc(L, "T1"); Ta = alloc(L, "Ta"); Tb = alloc(L, "Tb")
    TsBig = alloc(2 * L, "TsBig"); Ts = TsBig[:, 0:L]; Ts2 = TsBig[:, L:2 * L]; Tz = TsBig[:, 0:W]
    xc_bf = alloc_bf(L, "xcbf")
    yb_ring = Ring(2, 1024, "ybf", bf=True)
    B_T0 = Buf("T0"); B_T1 = Buf("T1"); B_Ta = Buf("Ta"); B_Tb = Buf("Tb"); B_Ts = Buf("Ts"); B_Ts2 = Buf("Ts2"); B_xcbf = Buf("xcbf")
    B_ssq = Buf("ssq")
    ssq = smalld[:, SD_TMP:SD_TMP + 32]
    rs = smalld[:, SD_TMP + 32:SD_TMP + 64]
    B_rs = Buf("rs")
    cp_ctr = [0]

    def copy_any(out, in_, reads, writes):
        cp_ctr[0] += 1
        if cp_ctr[0] % 2:
            return act(out, in_, AF.Copy, reads, writes)
        return P.op("dve", lambda e: e.tensor_copy(out=out, in_=in_), reads, writes)

    def rstd_chain(dst, src, scale, rb, wb):
        ts("dve", dst, src, scale, EPS, ALU.mult, ALU.add, rb, wb)
        act(dst, dst, AF.Sqrt, wb, wb)
        P.op("dve", lambda e: e.reciprocal(out=dst, in_=dst), wb, wb)

    def build_xnT(row0, ntile, dstT, B_dst, g_ap, s_ap, B_g):
        for i in range(ntile):
            x_ap, x_b, x_s = xs_ring.next()
            if isinstance(row0, tuple):
                src = row0[0][row0[1] + i * 128: row0[1] + (i + 1) * 128, :]
            else:
                src = x_all[row0 + i * 128: row0 + (i + 1) * 128, :]
            dma("sp", x_ap, src, x_s, writes=[x_b])
            col = i % 32
            act(junk, x_ap, AF.Square, [x_b], [B_junk, B_ssq], accum_out=ssq[:, col:col + 1])
            rstd_chain(rs[:, col:col + 1], ssq[:, col:col + 1], 1.0 / D, [B_ssq], [B_rs])
            stt("dve", t1, x_ap, rs[:, col:col + 1], g_ap, ALU.mult, ALU.mult, [x_b, B_rs, B_g], [B_t1])
            n_ap, n_b, _ = xnb_ring.next()
            tt("pool", n_ap, t1, s_ap, ALU.add, [B_t1, B_g], [n_b])
            for kb in range(4):
                bk = nbank()
                pv = PS[bk][:, :].bitcast(BF16)
                trg([(pv[:, j * 128:(j + 1) * 128], n_ap[:, (kb * 4 + j) * 128:(kb * 4 + j + 1) * 128], ident_bf) for j in range(4)],
                    [n_b, B_ident], [PB[bk]])
                copy_any(dstT[:, kb * 4:kb * 4 + 4, i * 128:(i + 1) * 128],
                         pv[:, 0:512].rearrange("p (k n) -> p k n", k=4), [PB[bk]], [B_dst])

    def load_w(ct):
        w_ap, w_b, w_s = w_ring.next()
        dma("pool", w_ap, win_d[ct], w_s, writes=[w_b])
        return w_ap.rearrange("p (k j) -> p k j", k=16), w_b

    def inproj(w3, w_b, c0, c1, evac):
        for (a, b) in chunks(c0, c1):
            bk = nbank()
            mmg([(PS[bk][:, 0:b - a], w3[:, k, :], xnT[:, k, a:b], k == 0, k == 15) for k in range(16)],
                [w_b, B_xnT], [PB[bk]])
            evac(PS[bk][:, 0:b - a], PB[bk], a, b)

    def colp(base, idx):
        return smallc[:, base + idx: base + idx + 1]

    sumH = smalld[:, SD_SUMH:SD_SUMH + 80]
    sumA = smalld[:, SD_SUMA:SD_SUMA + 80]
    S_y = newsem("ystore")

    for w in range(5):
        Lw = WIN_L[w]
        Ww = Lw + 2 * HALO
        mine = (w == 4)
        if w == 0:
            dma("sp", gs_b, modb_d[7], S_gs, writes=[B_gs])
            dma("sp", sh_b, modb_d[6], S_gs, writes=[B_gs])
        if w == 1:
            dma("sp", gs_b, modb_d[1], S_gs, writes=[B_gs])
            dma("sp", sh_b, modb_d[0], S_gs, writes=[B_gs])
        build_xnT(WIN_ROW[w], Ww // 128, xnT, B_xnT, gs_b, sh_b, B_gs)
        P.barrier()
        if stop == f"x{w}":
            return finish_early()
        if mine:
            tmpc = smalld[:, SD_TMP + 64:SD_TMP + 80]
            B_tc = Buf("tmpc")
            P.op("dve", lambda e: e.tensor_copy(out=carry, in_=sumH[:, 0:16]), [B_sum[0]], [B_carry])
            for (lo, order, mbase) in ((0, (1, 2, 3), 0), (8, (3, 2, 1), 3)):
                for wo in order:
                    cs = carry[:, lo:lo + 8]
                    tcs = tmpc[:, lo:lo + 8]
                    tt("dve", tcs, sumA[:, wo * 16 + lo: wo * 16 + lo + 8], cs, ALU.mult, [B_sum[wo], B_carry], [B_tc])
                    tt("dve", tcs, tcs, sumH[:, wo * 16 + lo: wo * 16 + lo + 8], ALU.add, [B_sum[wo], B_tc], [B_tc])
                    tt("dve", tcs, tcs, cs, ALU.subtract, [B_tc, B_carry], [B_tc])
                    stt("dve", cs, tcs, colp(C_CM, mbase + wo - 1), cs, ALU.mult, ALU.add, [B_tc, B_carry, B_smallc], [B_carry])
            P.op("pool", lambda e: e.memset(acc_a, 0.0), [], [B_acca])
            P.op("pool", lambda e: e.memset(acc_b, 0.0), [], [B_accb])
        hm_l = colp(C_HM, 2 * w)
        hm_r = colp(C_HM, 2 * w + 1)
        for c in range(8):
            w3, w_b = load_w(8 + c)
            inproj(w3, w_b, 0, Ww, lambda ps, pb, a, b: act(T0[:, a:b], ps, AF.Copy, [pb], [B_T0]))
            ts("dve", T0[:, 0:HALO], T0[:, 0:HALO], hm_l, None, ALU.mult, None, [B_T0, B_smallc], [B_T0])
            ts("dve", T0[:, HALO + Lw:Ww], T0[:, HALO + Lw:Ww], hm_r, None, ALU.mult, None, [B_T0, B_smallc], [B_T0])
            xc = T1[:, 0:Lw]
            ts("dve", xc, T0[:, 62:62 + Lw], colp(C_CAW, c * 4 + 0), colp(C_CAB, c), ALU.mult, ALU.add, [B_T0, B_smallc], [B_T1])
            for tap in (1, 2, 3):
                stt("dve", xc, T0[:, 62 + tap:62 + tap + Lw], colp(C_CAW, c * 4 + tap), xc, ALU.mult, ALU.add, [B_T0, B_T1, B_smallc], [B_T1])
            act(xc_bf[:, 0:Lw], xc, AF.Copy, [B_T1], [B_xcbf])
            for d in range(2):
                dc = d * 8 + c
                hbuf, B_h = (Ts, B_Ts) if d == 0 else (Ts2, B_Ts2)
                for (a, b) in chunks(0, Lw):
                    bk = nbank()
                    mmg([(PS[bk][:, 0:b - a], wr_bf[:, dc * 128:(dc + 1) * 128], xc_bf[:, a:b], True, True)], [B_wri, B_xcbf], [PB[bk]])
                    act(Ta[:, a:b], PS[bk][:, 0:b - a], AF.Sigmoid, [PB[bk], B_smallc], [B_Ta], bias=colp(C_LBR, dc))
                    bk = nbank()
                    mmg([(PS[bk][:, 0:b - a], wi_bf[:, dc * 128:(dc + 1) * 128], xc_bf[:, a:b], True, True)], [B_wri, B_xcbf], [PB[bk]])
                    act(Tb[:, a:b], PS[bk][:, 0:b - a], AF.Sigmoid, [PB[bk], B_smallc], [B_Tb], bias=colp(C_LBI, dc))
                av = Ta[:, 0:Lw]; bv = Tb[:, 0:Lw]; hv = hbuf[:, 0:Lw]
                act(av, av, AF.Exp, [B_Ta, B_cneg], [B_Ta], scale=cneg[:, dc:dc + 1])
                act(hv, av, AF.Square, [B_Ta], [B_h])
                act(hv, hv, AF.Sqrt, [B_h], [B_h], scale=-1.0, bias=1.0)
                tt("dve", bv, bv, xc, ALU.mult, [B_Tb, B_T1], [B_Tb])
                tt("dve", bv, bv, hv, ALU.mult, [B_Tb, B_h], [B_Tb])
                init = carry[:, dc:dc + 1] if mine else 0.0
                if d == 0:
                    P.op("dve", lambda e, hv=hv, av=av, bv=bv, init=init: e.tensor_tensor_scan(
                        out=hv, data0=av, data1=bv, initial=init, op0=ALU.mult, op1=ALU.add), [B_Ta, B_Tb, B_carry], [B_h])
                else:
                    P.op("dve", lambda e, hv=hv, av=av, bv=bv, init=init: e.tensor_tensor_scan(
                        out=rev(hv), data0=rev(av), data1=rev(bv), initial=init, op0=ALU.mult, op1=ALU.add), [B_Ta, B_Tb, B_carry], [B_h])
                if not mine:
                    endcol = hv[:, Lw - 1:Lw] if d == 0 else hv[:, 0:1]
                    P.op("dve", lambda e, endcol=endcol, dc=dc, w=w: e.tensor_copy(out=sumH[:, w * 16 + dc:w * 16 + dc + 1], in_=endcol), [B_h], [B_sum[w]])
                    P.op("dve", lambda e, av=av, dc=dc, w=w: e.tensor_reduce(out=sumA[:, w * 16 + dc:w * 16 + dc + 1], in_=av, axis=AX.X, op=ALU.mult), [B_Ta], [B_sum[w]])
            if not mine:
                continue
            tt("dve", Ts, Ts, Ts2, ALU.add, [B_Ts, B_Ts2], [B_Ts])
            w3, w_b = load_w(c)
            Tg = T0[:, 0:L]
            inproj(w3, w_b, HALO, HALO + L, lambda ps, pb, a, b: act(Tg[:, a - HALO:b - HALO], ps, AF.Copy, [pb], [B_T0]))
            Tq = Ta
            tt("dve", Tq, Tg, Tg, ALU.mult, [B_T0], [B_Ta])
            ts("dve", Tq, Tq, 0.044715, 1.0, ALU.mult, ALU.add, [B_Ta], [B_Ta])
            tt("dve", Tq, Tq, Tg, ALU.mult, [B_Ta, B_T0], [B_Ta])
            act(Tq, Tq, AF.Sigmoid, [B_Ta], [B_Ta], scale=1.5957691216057308)
            tt("dve", Tq, Tq, Tg, ALU.mult, [B_Ta, B_T0], [B_Ta])
            tt("dve", Tq, Tq, Ts, ALU.mult, [B_Ta, B_Ts], [B_Ta])
            tt("pool", Tb, Tq, Tq, ALU.mult, [B_Ta], [B_Tb])
            tt("pool", acc_a, acc_a, Tb, ALU.add, [B_Tb, B_acca], [B_acca])
            y_ap, y_b, y_s = yb_ring.next()
            act(y_ap, Tq, AF.Copy, [B_Ta, B_smallc], [y_b], scale=colp(C_GOA, c))
            dma("sp", y_d[c], y_ap, y_s, reads=[y_b])
        if not mine:
            P.barrier()
            if stop == f"w{w}":
                return finish_early()
            continue
        for c in range(8):
            Tc = T0
            w3, w_b = load_w(24 + c)
            inproj(w3, w_b, 0, W, lambda ps, pb, a, b: act(Tc[:, a:b], ps, AF.Copy, [pb], [B_T0]))
            w3, w_b = load_w(32 + c)
            inproj(w3, w_b, 0, W, lambda ps, pb, a, b: tt("dve", Tz[:, a:b], ps, Tc[:, a:b], ALU.mult, [pb, B_T0], [B_Ts, B_Ts2]))
            ts("dve", Tz[:, 0:HALO], Tz[:, 0:HALO], hm_l, None, ALU.mult, None, [B_Ts, B_Ts2, B_smallc], [B_Ts, B_Ts2])
            ts("dve", Tz[:, HALO + L:W], Tz[:, HALO + L:W], hm_r, None, ALU.mult, None, [B_Ts, B_Ts2, B_smallc], [B_Ts, B_Ts2])
            Tcv = T1
            w0 = colp(C_CBW, c * 3 + 0); w1 = colp(C_CBW, c * 3 + 1); w2 = colp(C_CBW, c * 3 + 2)
            ts("dve", Tcv, Tz[:, HALO:HALO + L], w1, None, ALU.mult, None, [B_Ts, B_Ts2, B_smallc], [B_T1])
            if c < 4:
                zv = Tz[:, HALO:HALO + L].rearrange("p (r c) -> p r c", c=64)
                ov = Tcv.rearrange("p (r c) -> p r c", c=64)
                stt("dve", ov[:, :, 1:64], zv[:, :, 0:63], w0, ov[:, :, 1:64], ALU.mult, ALU.add, [B_Ts, B_Ts2, B_T1, B_smallc], [B_T1])
                stt("dve", ov[:, :, 0:63], zv[:, :, 1:64], w2, ov[:, :, 0:63], ALU.mult, ALU.add, [B_Ts, B_Ts2, B_T1, B_smallc], [B_T1])
            else:
                stt("dve", Tcv, Tz[:, 0:L], w0, Tcv, ALU.mult, ALU.add, [B_Ts, B_Ts2, B_T1, B_smallc], [B_T1])
                stt("dve", Tcv, Tz[:, 2 * HALO:2 * HALO + L], w2, Tcv, ALU.mult, ALU.add, [B_Ts, B_Ts2, B_T1, B_smallc], [B_T1])
            w3, w_b = load_w(16 + c)
            Ty = Ta
            inproj(w3, w_b, HALO, HALO + L, lambda ps, pb, a, b: tt("dve", Ty[:, a - HALO:b - HALO], ps, Tcv[:, a - HALO:b - HALO], ALU.mult, [pb, B_T1], [B_Ta]))
            tt("pool", Tb, Ty, Ty, ALU.mult, [B_Ta], [B_Tb])
            tt("pool", acc_b, acc_b, Tb, ALU.add, [B_Tb, B_accb], [B_accb])
            y_ap, y_b, y_s = yb_ring.next()
            act(y_ap, Ty, AF.Copy, [B_Ta, B_smallc], [y_b], scale=colp(C_GOB, c))
            dma("sp", y_d[8 + c], y_ap, y_s, reads=[y_b])
        bk = nbank()
        for g, (acc, B_acc) in enumerate(((acc_a, B_acca), (acc_b, B_accb))):
            for i in range(16):
                j = g * 16 + i
                mmg([(PS[bk][:, 2 * j:2 * j + 2], acc[:, i * 128:(i + 1) * 128], ones_f[:, 0:2], True, True)], [B_acc, B_smallc], [PB[bk]])
        rstd_ab = smalld[:, SD_RSTD:SD_RSTD + 64]
        P.op("dve", lambda e, src=PS[bk][:, 0:64], dst=rstd_ab: e.tensor_copy(out=dst, in_=src), [PB[bk]], [B_rstd])
        rstd_chain(rstd_ab, rstd_ab, 1.0 / 1024, [B_rstd], [B_rstd])
    P.barrier()
    if stop == "p1":
        return finish_early()

    top[0] = persist_top
    wout = alloc_bf(16 * D, "wout").rearrange("p (c n) -> p c n", c=16)
    B_wout = Buf("wout")
    S_wout = newsem("wout")
    for c in range(16):
        dma("pool", wout[:, c, :], wout_d[c * 128:(c + 1) * 128, :], S_wout, writes=[B_wout])
    if stop == "p2a":
        return finish_early()
    ys_ring = Ring(2, 4096, "ysb", bf=True)
    xs2_ring = Ring(2, 2048, "xs2")
    gt1_b = alloc(2048, "gt1")
    B_gt1 = Buf("gt1")
    dma("sp", gt1_b, modb_d[2], S_gs, writes=[B_gt1])
    tA_ring = Ring(2, 512, "tA")
    tB_ring = Ring(2, 512, "tB")
    S_x1 = newsem("x1store")
    rstd_ab = smalld[:, SD_RSTD:SD_RSTD + 64]
    mine_row = WIN_ROW[4] + HALO
    ys3 = None
    for i in range(16):
        if i % 4 == 0:
            y_ap, y_b, y_s = ys_ring.next()
            ys3 = y_ap.rearrange("p (c n) -> p c n", c=16)
            for c in range(16):
                dma("sp", ys3[:, c, :], y_d[c][:, (i // 4) * 512:(i // 4 + 1) * 512], y_s, reads=[], writes=[y_b])
            ys_b = y_b
        x_ap, x_b, x_s = xs2_ring.next()
        dma("sp", x_ap, x_all[mine_row + i * 128: mine_row + (i + 1) * 128, :], x_s, writes=[x_b])
        to = (i % 4) * 128
        for n in range(4):
            bA = nbank(); bB = nbank()
            mmg([(PS[bA][:, :], ys3[:, c, to:to + 128], wout[:, c, n * 512:(n + 1) * 512], c == 0, c == 7) for c in range(8)], [ys_b, B_wout], [PB[bA]])
            mmg([(PS[bB][:, :], ys3[:, c, to:to + 128], wout[:, c, n * 512:(n + 1) * 512], c == 8, c == 15) for c in range(8, 16)], [ys_b, B_wout], [PB[bB]])
            ta, ta_b, _ = tA_ring.next()
            tb, tb_b, _ = tB_ring.next()
            act(ta, PS[bA][:, :], AF.Copy, [PB[bA], B_rstd], [ta_b], scale=rstd_ab[:, 2 * i:2 * i + 1])
            stt("dve", tb, PS[bB][:, :], rstd_ab[:, 32 + 2 * i:32 + 2 * i + 1], ta, ALU.mult, ALU.add, [PB[bB], B_rstd, ta_b], [tb_b])
            tt("pool", tb, tb, gt1_b[:, n * 512:(n + 1) * 512], ALU.mult, [tb_b, B_gt1], [tb_b])
            tt("pool", x_ap[:, n * 512:(n + 1) * 512], x_ap[:, n * 512:(n + 1) * 512], tb, ALU.add, [tb_b, x_b], [x_b])
        if stop == "p2b":
            return finish_early()
        dst = out_d if stage < 3 else x1_d
        dma("sp", dst[i * 128:(i + 1) * 128, :], x_ap, x_s, reads=[x_b])
    P.barrier()
    out_sems = [s for (_, _, s) in xs2_ring.slots]

    if stage >= 3:
        top[0] = persist_top
        acc = [alloc(2048, f"acc{i}") for i in range(4)]
        B_accs = [Buf(f"acc{i}") for i in range(4)]
        S_acc = [newsem(f"acc{i}") for i in range(4)]
        xn2T = alloc_bf(16 * 512, "xn2T").rearrange("p (k n) -> p k n", k=16)
        B_xn2T = Buf("xn2T")
        actb = alloc_bf(16 * 512, "actb").rearrange("p (f n) -> p f n", f=16)
        B_actb = Buf("actb")
        wd_ring = Ring(2, 4096, "wd", bf=True)
        wgu_ring = Ring(3, 2048, "wgu", bf=True)
        mod0 = alloc(2048, "mod0"); mod1 = alloc(2048, "mod1")
        B_mod0 = Buf("mod0"); B_mod1 = Buf("mod1")
        S_mod0 = newsem("mod0"); S_mod1 = newsem("mod1"); S_bd = newsem("bd")
        bd_sb = alloc(2048, "bd")
        B_bd = Buf("bd")
        dma("sp", bd_sb[0:32, :], bd_d[:, :], S_bd, writes=[B_bd])
        G = alloc(128, "G").rearrange("p (i e) -> p i e", i=4)
        B_G = Buf("G")
        GT = alloc(512, "GT")
        B_GT = Buf("GT")
        lg = alloc(32, "lg"); ex = alloc(32, "ex"); mk = alloc(32, "mk"); m8 = alloc(8, "m8"); sm = alloc(4, "sm")
        B_lg = Buf("lg"); B_ex = Buf("ex"); B_mk = Buf("mk"); B_m8 = Buf("m8"); B_sm = Buf("sm")
        tmp_top = top[0]
        t1m = alloc(2048, "t1m"); B_t1m = Buf("t1m")
        xnbm = alloc_bf(2048, "xnbm"); B_xnbm = Buf("xnbm")
        junkm = alloc_bf(2048, "junkm"); B_junkm = Buf("junkm")
        top[0] = tmp_top
        tg_ring = Ring(2, 512, "tg"); tsg_ring = Ring(2, 512, "tsg"); tu_ring = Ring(2, 512, "tu"); td_ring = Ring(2, 512, "td")
        bgc = smallc[:, C_BG:C_BG + 512]
        buc = smallc[:, C_BU:C_BU + 512]
        brt = smallc[:, C_BRT:C_BRT + 32]
        out_sems = S_acc
        for q in range(nq):
            dma("sp", mod0, modb_d[4], S_mod0, writes=[B_mod0])
            dma("sp", mod1, modb_d[3], S_mod1, writes=[B_mod1])
            for i in range(4):
                r0 = (q * 4 + i) * 128
                dma("sp", acc[i], x1_d[r0:r0 + 128, :], S_acc[i], writes=[B_accs[i]])
                act(junkm, acc[i], AF.Square, [B_accs[i]], [B_junkm, B_ssq], accum_out=ssq[:, i:i + 1])
                rstd_chain(rs[:, i:i + 1], ssq[:, i:i + 1], 1.0 / D, [B_ssq], [B_rs])
                stt("dve", t1m, acc[i], rs[:, i:i + 1], mod0, ALU.mult, ALU.mult, [B_accs[i], B_rs, B_mod0], [B_t1m])
                tt("pool", xnbm, t1m, mod1, ALU.add, [B_t1m, B_mod1], [B_xnbm])
                for kb in range(4):
                    bk = nbank()
                    pv = PS[bk][:, :].bitcast(BF16)
                    trg([(pv[:, j * 128:(j + 1) * 128], xnbm[:, (kb * 4 + j) * 128:(kb * 4 + j + 1) * 128], ident_bf) for j in range(4)],
                        [B_xnbm, B_ident], [PB[bk]])
                    copy_any(xn2T[:, kb * 4:kb * 4 + 4, i * 128:(i + 1) * 128],
                             pv[:, 0:512].rearrange("p (k n) -> p k n", k=4), [PB[bk]], [B_xn2T])
                bk = nbank()
                wrt3 = wrt_bf.rearrange("p (k e) -> p k e", k=16)
                mmg([(PS[bk][:, 0:32], xn2T[:, k, i * 128:(i + 1) * 128], wrt3[:, k, :], k == 0, k == 15) for k in range(16)],
                    [B_xn2T, B_wrt], [PB[bk]])
                tt("dve", lg, PS[bk][:, 0:32], brt, ALU.add, [PB[bk], B_smallc], [B_lg])
                P.op("dve", lambda e: e.max(out=m8, in_=lg), [B_lg], [B_m8])
                ts("dve", sm[:, 0:1], m8[:, 0:1], -1.0, None, ALU.mult, None, [B_m8], [B_sm])
                act(ex, lg, AF.Exp, [B_lg, B_sm], [B_ex], bias=sm[:, 0:1])
                ts("dve", mk, lg, m8[:, 3:4], None, ALU.is_ge, None, [B_lg, B_m8], [B_mk])
                tt("dve", ex, ex, mk, ALU.mult, [B_ex, B_mk], [B_ex])
                P.op("dve", lambda e: e.tensor_reduce(out=sm[:, 1:2], in_=ex, axis=AX.X, op=ALU.add), [B_ex], [B_sm])
                P.op("dve", lambda e: e.reciprocal(out=sm[:, 2:3], in_=sm[:, 1:2]), [B_sm], [B_sm])
                ts("dve", G[:, i, :], ex, sm[:, 2:3], None, ALU.mult, None, [B_ex, B_sm], [B_G])
                bk = nbank()
                trg([(PS[bk][0:32, 0:128], G[:, i, :], ident_f)], [B_G, B_smallc], [PB[bk]])
                act(GT[0:32, i * 128:(i + 1) * 128], PS[bk][0:32, 0:128], AF.Copy, [PB[bk]], [B_GT])
            P.barrier()
            dma("sp", mod0, modb_d[5], S_mod0, writes=[B_mod0])
            dma("sp", mod1, gfin_d[:, :], S_mod1, writes=[B_mod1])
            for i in range(4):
                for m in range(4):
                    bk = nbank()
                    mmg([(PS[bk][:, :], GT[0:32, i * 128:(i + 1) * 128], bd_sb[0:32, m * 512:(m + 1) * 512], True, True)], [B_GT, B_bd], [PB[bk]])
                    td, td_b, _ = td_ring.next()
                    tt("dve", td, PS[bk][:, :], mod0[:, m * 512:(m + 1) * 512], ALU.mult, [PB[bk], B_mod0], [td_b])
                    tt("pool", acc[i][:, m * 512:(m + 1) * 512], acc[i][:, m * 512:(m + 1) * 512], td, ALU.add, [td_b, B_accs[i]], [B_accs[i]])
            for ex_i in range(NEXP):
                for f in range(16):
                    u_ap, u_b, u_s = wgu_ring.next()
                    dma("pool", u_ap, wgu_d[ex_i * 16 + f], u_s, writes=[u_b])
                    u4 = u_ap.rearrange("p (t k j) -> p t k j", t=2, k=16)
                    bG = nbank(); bU = nbank()
                    mmg([(PS[bG][:, :], u4[:, 0, k, :], xn2T[:, k, :], k == 0, k == 15) for k in range(16)], [u_b, B_xn2T], [PB[bG]])
                    mmg([(PS[bU][:, :], u4[:, 1, k, :], xn2T[:, k, :], k == 0, k == 15) for k in range(16)], [u_b, B_xn2T], [PB[bU]])
                    tg, tg_b, _ = tg_ring.next(); tsg, tsg_b, _ = tsg_ring.next(); tu, tu_b, _ = tu_ring.next()
                    cb = ex_i * 16 + f
                    ts("dve", tg, PS[bG][:, :], bgc[:, cb:cb + 1], 7.0, ALU.add, ALU.min, [PB[bG], B_smallc], [tg_b])
                    act(tsg, tg, AF.Sigmoid, [tg_b], [tsg_b], scale=1.702)
                    act(tu, PS[bU][:, :], AF.Identity, [PB[bU], B_smallc], [tu_b], bias=buc[:, cb:cb + 1])
                    ts("dve", tu, tu, 7.0, -7.0, ALU.min, ALU.max, [tu_b], [tu_b])
                    stt("dve", tu, tu, 1.0, tg, ALU.add, ALU.mult, [tu_b, tg_b], [tu_b])
                    tt("pool", actb[:, f, :], tu, tsg, ALU.mult, [tu_b, tsg_b], [B_actb])
                for m in range(4):
                    d_ap, d_b, d_s = wd_ring.next()
                    d3 = d_ap.rearrange("p (f n) -> p f n", f=16)
                    dma("pool", d3, wd_d[ex_i].rearrange("(f p) d -> p f d", p=128)[:, :, m * 512:(m + 1) * 512], d_s, writes=[d_b])
                    for i in range(4):
                        bk = nbank()
                        mmg([(PS[bk][:, :], actb[:, f, i * 128:(i + 1) * 128], d3[:, f, :], f == 0, f == 15) for f in range(16)], [B_actb, d_b], [PB[bk]])
                        td, td_b, _ = td_ring.next()
                        stt("dve", td, PS[bk][:, :], G[:, i, ex_i:ex_i + 1], mod0[:, m * 512:(m + 1) * 512], ALU.mult, ALU.mult, [PB[bk], B_G, B_mod0], [td_b])
                        tt("pool", acc[i][:, m * 512:(m + 1) * 512], acc[i][:, m * 512:(m + 1) * 512], td, ALU.add, [td_b, B_accs[i]], [B_accs[i]])
            P.barrier()
            for i in range(4):
                r0 = (q * 4 + i) * 128
                act(junkm, acc[i], AF.Square, [B_accs[i]], [B_junkm, B_ssq], accum_out=ssq[:, 8 + i:9 + i])
                rstd_chain(rs[:, 8 + i:9 + i], ssq[:, 8 + i:9 + i], 1.0 / D, [B_ssq], [B_rs])
                stt("dve", acc[i], acc[i], rs[:, 8 + i:9 + i], mod1, ALU.mult, ALU.mult, [B_accs[i], B_rs, B_mod1], [B_accs[i]])
                dma("sp", out_d[r0:r0 + 128, :], acc[i], S_acc[i], reads=[B_accs[i]])
            P.barrier()
    P.final_wait("sp", out_sems)
    with nc.Block() as block:
        P.emit(block)
    es.close()
    return nc


_NC_CACHE = {}


def _prep_shared(inp, stage):
    f = np.float32
    sh = {}
    sh["w_mod"] = np.ascontiguousarray(inp["w_mod"][0], dtype=f)
    sh["b_mod"] = np.ascontiguousarray(inp["b_mod"][0].reshape(1, -1), dtype=f)
    sh["g_mix_b"] = np.ascontiguousarray(np.broadcast_to(inp["g_mix"][0], (128, D)), dtype=f)
    sh["g_ffn_b"] = np.ascontiguousarray(np.broadcast_to(inp["g_ffn"][0], (128, D)), dtype=f)
    sh["g_fin_b"] = np.ascontiguousarray(np.broadcast_to(inp["g_final"], (128, D)), dtype=f)
    w_in = np.asarray(inp["w_in"][0], dtype=f)
    sh["w_in_h"] = np.ascontiguousarray(w_in.reshape(16, 128, 40, 128).transpose(2, 1, 0, 3)).reshape(40, 128, 2048)
    sh["wr_h"] = np.ascontiguousarray(np.asarray(inp["lru_w_r"][0], dtype=f).transpose(2, 0, 1, 3)).reshape(128, 2048)
    sh["wi_h"] = np.ascontiguousarray(np.asarray(inp["lru_w_i"][0], dtype=f).transpose(2, 0, 1, 3)).reshape(128, 2048)
    sh["w_out"] = np.ascontiguousarray(inp["w_out"][0], dtype=f)
    if stage >= 3:
        sh["w_router_h"] = np.ascontiguousarray(np.asarray(inp["w_router"][0], dtype=f).reshape(16, 128, 32).transpose(1, 0, 2)).reshape(128, 512)
        wg = np.asarray(inp["w_gate"][0], dtype=f).reshape(NEXP, 16, 128, 16, 128)
        wu = np.asarray(inp["w_up"][0], dtype=f).reshape(NEXP, 16, 128, 16, 128)
        wgu = np.empty((NEXP, 16, 128, 2, 16, 128), dtype=f)
        wgu[:, :, :, 0] = wg.transpose(0, 3, 2, 1, 4)
        wgu[:, :, :, 1] = wu.transpose(0, 3, 2, 1, 4)
        sh["wgu_h"] = wgu.reshape(NEXP * 16, 128, 4096)
        sh["w_down"] = np.ascontiguousarray(inp["w_down"][0], dtype=f)
        sh["b_down"] = np.ascontiguousarray(inp["b_down"][0], dtype=f)
    return sh


def _prep_core(inp, k):
    f = np.float32
    b, j = k // 4, k % 4
    x = np.asarray(inp["x"], dtype=f)
    ctx = np.asarray(inp["ctx"], dtype=f)
    S = x.shape[1]
    x_all = np.zeros((XROWS, D), dtype=f)
    x_all[HALO:HALO + LC] = ctx[b]
    others = [jj for jj in range(4) if jj != j]
    chunks_ = others + [j]
    hm = np.zeros(10, dtype=f)
    for wi, jj in enumerate(chunks_):
        w = wi + 1
        t0 = jj * L - HALO
        lo, hi = max(t0, 0), min(t0 + W, S)
        r0 = WIN_ROW[w] + (lo - t0)
        x_all[r0:r0 + (hi - lo)] = x[b, lo:hi]
        hm[2 * w] = 1.0 if jj > 0 else 0.0
        hm[2 * w + 1] = 1.0 if jj < 3 else 0.0
    cm = np.zeros(6, dtype=f)
    for o, jj in enumerate(others):
        cm[o] = 1.0 if jj < j else 0.0
        cm[3 + o] = 1.0 - cm[o]
    sc = np.zeros((128, NS), dtype=f)

    def colT(v, n):
        return np.asarray(v, dtype=f).reshape(n, 128).T

    sc[:, C_CT:C_CT + 16] = colT(inp["c"][b], 16)
    sc[:, C_CX:C_CX + 16] = colT(inp["c_ctx"], 16)
    caw = np.asarray(inp["conv_a_w"][0], dtype=f)
    sc[:, C_CAW:C_CAW + 32] = caw.reshape(4, 8, 128).transpose(2, 1, 0).reshape(128, 32)
    sc[:, C_CAB:C_CAB + 8] = colT(inp["conv_a_b"][0], 8)
    sc[:, C_LBR:C_LBR + 16] = colT(np.asarray(inp["lru_b_r"][0]).reshape(-1), 16)
    sc[:, C_LBI:C_LBI + 16] = colT(np.asarray(inp["lru_b_i"][0]).reshape(-1), 16)
    sc[:, C_LAM:C_LAM + 16] = colT(np.asarray(inp["lru_lam"][0]).reshape(-1), 16)
    cbw = np.asarray(inp["conv_b_w"][0], dtype=f)
    sc[:, C_CBW:C_CBW + 24] = cbw.reshape(3, 8, 128).transpose(2, 1, 0).reshape(128, 24)
    sc[:, C_GOA:C_GOA + 8] = colT(inp["g_out_a"][0], 8)
    sc[:, C_GOB:C_GOB + 8] = colT(inp["g_out_b"][0], 8)
    sc[:, C_HM:C_HM + 10] = hm[None, :]
    sc[:, C_CM:C_CM + 6] = cm[None, :]
    sc[:, C_BG:C_BG + 512] = np.asarray(inp["b_gate"][0], dtype=f).reshape(NEXP, 16, 128).transpose(2, 0, 1).reshape(128, 512)
    sc[:, C_BU:C_BU + 512] = np.asarray(inp["b_up"][0], dtype=f).reshape(NEXP, 16, 128).transpose(2, 0, 1).reshape(128, 512)
    sc[:, C_BRT:C_BRT + 32] = np.asarray(inp["b_router"][0], dtype=f)[None, :]
    sc[:, C_ID:C_ID + 128] = np.eye(128, dtype=f)
    sc[:, C_ONE:C_ONE + 128] = 1.0
    return {"x_all": x_all, "smallc": sc}


def run(inputs, stage=3, stop=None):
    if (stage, stop) not in _NC_CACHE:
        _NC_CACHE[(stage, stop)] = build(stage, stop)
    nc = _NC_CACHE[(stage, stop)]
    shared = _prep_shared(inputs, stage)
    in_maps = []
    for k in range(NCORE):
        m = dict(shared)
        m.update(_prep_core(inputs, k))
        in_maps.append(m)
    res = run_bass_kernel_spmd(nc, in_maps, core_ids=list(range(NCORE)))
    outs = [np.asarray(r["out"]) for r in res.results]
    return np.concatenate(outs, axis=0).reshape(2, 4 * L, D).astype(np.float32)


def kernel(**inputs):
    return run(inputs, stage=3)
```

```python
import numpy as np
from contextlib import ExitStack
import concourse.bass as bass
import concourse.mybir as mybir
from concourse.bass_utils import run_bass_kernel_spmd

F32 = mybir.dt.float32
BF16 = mybir.dt.bfloat16
AF = mybir.ActivationFunctionType
ALU = mybir.AluOpType
AX = mybir.AxisListType

D = 2048
L = 2048
HALO = 64
W = L + 2 * HALO
LC = 256
WC = LC + 2 * HALO
NCORE = 8
EPS = 1e-6
NEXP = 32
WIN_L = [LC, L, L, L, L]
WIN_ROW = [0, WC, WC + W, WC + 2 * W, WC + 3 * W]
XROWS = WC + 4 * W

_o = 0
def _col(n):
    global _o
    r = _o
    _o += n
    return r
C_CT = _col(16); C_CX = _col(16); C_CAW = _col(32); C_CAB = _col(8); C_LBR = _col(16); C_LBI = _col(16)
C_LAM = _col(16); C_CBW = _col(24); C_GOA = _col(8); C_GOB = _col(8); C_HM = _col(10); C_CM = _col(6)
C_BG = _col(512); C_BU = _col(512); C_BRT = _col(32); C_ID = _col(128); C_ONE = _col(128)
NS = _o

ENGS = ("pe", "act", "dve", "pool", "sp")
SAME_ENGINE_SYNC = True


class Buf:
    __slots__ = ("name", "w", "r")

    def __init__(self, name=""):
        self.name = name
        self.w = None
        self.r = {}


class Sem:
    def __init__(self, h, name):
        self.h = h
        self.n = 0
        self.name = name


class Prog:
    def __init__(self):
        self.ops = {e: [] for e in ENGS}
        self.esem = {}
        self.waited = {e: {} for e in ENGS}
        self.pending = {e: [] for e in ENGS}
        self.allsems = []

    def op(self, eng, fn, reads=(), writes=(), dma=None):
        deps = self.pending[eng]
        self.pending[eng] = []
        for b in reads:
            if b.w is not None:
                deps.append(b.w)
        for b in writes:
            if b.w is not None:
                deps.append(b.w)
            deps.extend(b.r.values())
        wd = self.waited[eng]
        best = {}
        own = self.esem.get(eng)
        for (s, v) in deps:
            if (not SAME_ENGINE_SYNC) and s is own:
                continue
            if wd.get(id(s), 0) >= v:
                continue
            if id(s) not in best or best[id(s)][1] < v:
                best[id(s)] = (s, v)
        waits = []
        for s, v in best.values():
            waits.append((s, v))
            wd[id(s)] = v
        if dma is not None:
            dma.n += 16
            ev = (dma, dma.n)
            inc = (dma, 16)
        else:
            s = self.esem[eng]
            s.n += 1
            ev = (s, s.n)
            inc = (s, 1)
        for b in reads:
            old = b.r.get(id(ev[0]))
            if old is None or old[1] < ev[1]:
                b.r[id(ev[0])] = ev
        for b in writes:
            b.w = ev
            b.r = {}
        self.ops[eng].append((waits, fn, inc))
        return ev

    def barrier(self):
        for e in ENGS:
            self.pending[e] = [(s, s.n) for s in self.allsems if s.n > 0]

    def final_wait(self, eng, sems):
        waits = [(s, s.n) for s in sems if s.n > 0]
        self.ops[eng].append((waits, None, None))

    def emit(self, block):
        def mk(name):
            lst = self.ops[name]

            def body(eng):
                for waits, fn, inc in lst:
                    for s, v in waits:
                        eng.wait_ge(s.h, v)
                    if fn is not None:
                        ins = fn(eng)
                        ins.then_inc(inc[0].h, inc[1])
            return body
        block.sync(mk("sp"))
        block.tensor(mk("pe"))
        block.scalar(mk("act"))
        block.vector(mk("dve"))
        block.gpsimd(mk("pool"))


def rev(ap):
    a = ap.ap
    assert len(a) == 2 and a[1][0] == 1, a
    n = a[1][1]
    return bass.AP(ap.tensor, ap.offset + (n - 1), [list(a[0]), [-1, n]])


def chunks(lo, hi, step=512):
    out = []
    while lo < hi:
        out.append((lo, min(lo + step, hi)))
        lo += step
    return out


def build(stage=3, stop=None, nq=4):
    nc = bass.Bass("TRN2", target_bir_lowering=False)
    P = Prog()
    es = ExitStack()

    def din(name, shape, dt=F32):
        return nc.dram_tensor(name, list(shape), dt, kind="ExternalInput").ap()

    x_all = din("x_all", [XROWS, D])
    smallc_d = din("smallc", [128, NS])
    wmod_d = din("w_mod", [D, 6 * D])
    bmod_d = din("b_mod", [1, 6 * D])
    gmix_d = din("g_mix_b", [128, D])
    gffn_d = din("g_ffn_b", [128, D])
    gfin_d = din("g_fin_b", [128, D])
    win_d = din("w_in_h", [40, 128, 16 * 128])
    wr_d = din("wr_h", [128, 16 * 128])
    wi_d = din("wi_h", [128, 16 * 128])
    wout_d = din("w_out", [D, D])
    if stage >= 3:
        wrt_d = din("w_router_h", [128, 16 * 32])
        wgu_d = din("wgu_h", [NEXP * 16, 128, 2 * 16 * 128])
        wd_d = din("w_down", [NEXP, D, D])
        bd_d = din("b_down", [NEXP, D])
    out_d = nc.dram_tensor("out", [L, D], F32, kind="ExternalOutput").ap()
    modb_d = nc.dram_tensor("modb", [8, 128, D], F32).ap()
    y_d = nc.dram_tensor("y_scr", [16, 128, L], BF16).ap()
    x1_d = nc.dram_tensor("x1_scr", [L, D], F32).ap()

    ARENA = 53000
    arena = es.enter_context(nc.sbuf_tensor("arena", [128, ARENA], F32))
    PS = [es.enter_context(nc.psum_tensor(f"ps{i}", [128, 512], F32)) for i in range(8)]
    PB = [Buf(f"ps{i}") for i in range(8)]
    for e in ENGS:
        P.esem[e] = Sem(es.enter_context(nc.semaphore(f"s_{e}")), e)
        P.allsems.append(P.esem[e])
    nsem = [0]

    def newsem(name):
        nsem[0] += 1
        s = Sem(es.enter_context(nc.semaphore(f"d{nsem[0]}_{name}")), name)
        P.allsems.append(s)
        return s

    bank_ctr = [0]

    def nbank():
        b = bank_ctr[0] % 8
        bank_ctr[0] += 1
        return b

    top = [0]

    def alloc(n_f32, name=""):
        a = top[0]
        top[0] += n_f32
        assert top[0] <= ARENA, (name, top[0])
        return arena[:, a:a + n_f32]

    def alloc_bf(n_bf, name=""):
        assert n_bf % 2 == 0
        return alloc(n_bf // 2, name).bitcast(BF16)

    def dma(q, out, in_, sem, reads=(), writes=()):
        return P.op(q, lambda e: e.dma_start(out=out, in_=in_), reads, writes, dma=sem)

    def act(out, in_, func, reads, writes, bias=None, scale=None, accum_out=None):
        kw = {}
        if bias is not None:
            kw["bias"] = bias
        if scale is not None:
            kw["scale"] = scale
        if accum_out is not None:
            kw["accum_out"] = accum_out
        return P.op("act", lambda e: e.activation(out=out, in_=in_, func=func, **kw), reads, writes)

    def ts(eng, out, in0, s1, s2, op0, op1, reads, writes):
        if s2 is None:
            return P.op(eng, lambda e: e.tensor_scalar(out=out, in0=in0, scalar1=s1, scalar2=None, op0=op0), reads, writes)
        return P.op(eng, lambda e: e.tensor_scalar(out=out, in0=in0, scalar1=s1, scalar2=s2, op0=op0, op1=op1), reads, writes)

    def tt(eng, out, in0, in1, op, reads, writes):
        return P.op(eng, lambda e: e.tensor_tensor(out=out, in0=in0, in1=in1, op=op), reads, writes)

    def stt(eng, out, in0, scalar, in1, op0, op1, reads, writes):
        return P.op(eng, lambda e: e.scalar_tensor_tensor(out=out, in0=in0, scalar=scalar, in1=in1, op0=op0, op1=op1), reads, writes)

    def mmg(items, reads, writes):
        def fn(e):
            ins = None
            for (o, l, r, s, t) in items:
                ins = e.matmul(o, l, r, start=s, stop=t)
            return ins
        return P.op("pe", fn, reads, writes)

    def trg(items, reads, writes):
        def fn(e):
            ins = None
            for (o, i_, idn) in items:
                ins = e.transpose(o, i_, idn)
            return ins
        return P.op("pe", fn, reads, writes)

    class Ring:
        def __init__(self, n, n_f32, name, bf=False, shape=None):
            self.slots = []
            for i in range(n):
                ap = alloc(n_f32, name)
                if bf:
                    ap = ap.bitcast(BF16)
                self.slots.append((ap, Buf(f"{name}{i}"), newsem(f"{name}{i}")))
            self.i = 0

        def next(self):
            s = self.slots[self.i % len(self.slots)]
            self.i += 1
            return s

    def finish_early():
        S_e = newsem("early")
        P.barrier()
        dma("sp", out_d[0:128, :], arena[:, 0:2048], S_e)
        P.final_wait("sp", [S_e])
        with nc.Block() as block:
            P.emit(block)
        es.close()
        return nc

    smallc = alloc(NS, "smallc")
    B_smallc = Buf("smallc")
    S_const = newsem("const")
    dma("sp", smallc, smallc_d[:, :], S_const, writes=[B_smallc])
    wr_bf = alloc_bf(2048, "wr")
    wi_bf = alloc_bf(2048, "wi")
    B_wri = Buf("wri")
    S_wri = newsem("wri")
    dma("pool", wr_bf, wr_d[:, :], S_wri, writes=[B_wri])
    dma("pool", wi_bf, wi_d[:, :], S_wri, writes=[B_wri])
    if stage >= 3:
        wrt_bf = alloc_bf(512, "wrt")
        B_wrt = Buf("wrt")
        S_wrt = newsem("wrt")
        dma("pool", wrt_bf, wrt_d[:, :], S_wrt, writes=[B_wrt])
    ident_bf = alloc_bf(128, "identbf")
    B_ident = Buf("ident")
    smalld = alloc(512, "smalld")
    ident_f = smallc[:, C_ID:C_ID + 128]
    ones_f = smallc[:, C_ONE:C_ONE + 128]
    P.op("act", lambda e: e.activation(out=ident_bf, in_=ident_f, func=AF.Copy), [B_smallc], [B_ident])
    SD_SC = 0; SD_SX = 16; SD_CNEG = 32; SD_CARRY = 48; SD_RSTD = 64; SD_SUMH = 128; SD_SUMA = 208; SD_TMP = 288
    B_sc = Buf("sc"); B_cneg = Buf("cneg"); B_carry = Buf("carry"); B_rstd = Buf("rstdab")
    B_sum = [Buf(f"sum{w}") for w in range(5)]
    sc = smalld[:, SD_SC:SD_SC + 16]
    sx = smalld[:, SD_SX:SD_SX + 16]
    cneg = smalld[:, SD_CNEG:SD_CNEG + 16]
    carry = smalld[:, SD_CARRY:SD_CARRY + 16]
    act(sc, smallc[:, C_CT:C_CT + 16], AF.Silu, [B_smallc], [B_sc])
    act(sx, smallc[:, C_CX:C_CX + 16], AF.Silu, [B_smallc], [B_sc])
    act(cneg, smallc[:, C_LAM:C_LAM + 16], AF.Exp, [B_smallc], [B_cneg], scale=-1.0)
    act(cneg, cneg, AF.Ln, [B_cneg], [B_cneg], bias=1.0)
    ts("dve", cneg, cneg, -8.0, None, ALU.mult, None, [B_cneg], [B_cneg])
    persist_top = top[0]

    cb_l = alloc(2048, "cb_l").rearrange("p (k m) -> p k m", k=16)
    cx_l = alloc(2048, "cx_l").rearrange("p (k m) -> p k m", k=16)
    B_cbl = Buf("cbl")
    for k in range(16):
        ts("dve", cb_l[:, k, :], ones_f, sc[:, k:k + 1], None, ALU.mult, None, [B_smallc, B_sc], [B_cbl])
        ts("dve", cx_l[:, k, :], ones_f, sx[:, k:k + 1], None, ALU.mult, None, [B_smallc, B_sc], [B_cbl])
    wm_ring = Ring(3, 2048, "wm")
    ev_ring = Ring(2, 2048, "ev")
    bm_ring = Ring(2, 2048, "bm")
    gbuf = alloc(2048, "gbuf")
    B_gbuf = Buf("gbuf")
    S_g = newsem("gload")
    for g in range(6):
        use_ctx = g < 2
        bm_ap, bm_b, bm_s = bm_ring.next()
        dma("sp", bm_ap[0:1, :], bmod_d[0:1, g * D:(g + 1) * D], bm_s, writes=[bm_b])
        if g in (1, 4):
            dma("sp", gbuf, (gmix_d if g == 1 else gffn_d)[:, :], S_g, writes=[B_gbuf])
        banks = [nbank() for _ in range(4)]
        xbanks = [nbank() for _ in range(4)] if use_ctx else []
        for k in range(16):
            w_ap, w_b, w_s = wm_ring.next()
            dma("sp", w_ap, wmod_d[k * 128:(k + 1) * 128, g * D:(g + 1) * D], w_s, writes=[w_b])
            for n in range(4):
                mmg([(PS[banks[n]][:, :], cb_l[:, k, :], w_ap[:, n * 512:(n + 1) * 512], k == 0, False)],
                    [w_b, B_cbl], [PB[banks[n]]])
                if use_ctx:
                    mmg([(PS[xbanks[n]][:, :], cx_l[:, k, :], w_ap[:, n * 512:(n + 1) * 512], k == 0, False)],
                        [w_b, B_cbl], [PB[xbanks[n]]])
        for n in range(4):
            mmg([(PS[banks[n]][:, :], ones_f[0:1, :], bm_ap[0:1, n * 512:(n + 1) * 512], False, True)],
                [bm_b, B_smallc], [PB[banks[n]]])
            if use_ctx:
                mmg([(PS[xbanks[n]][:, :], ones_f[0:1, :], bm_ap[0:1, n * 512:(n + 1) * 512], False, True)],
                    [bm_b, B_smallc], [PB[xbanks[n]]])
        for (bks, idx) in ((banks, g), (xbanks, 6 + g)):
            if not bks:
                continue
            e_ap, e_b, e_s = ev_ring.next()
            for n in range(4):
                dst = e_ap[:, n * 512:(n + 1) * 512]
                if g in (1, 4):
                    stt("dve", dst, PS[bks[n]][:, :], 1.0, gbuf[:, n * 512:(n + 1) * 512], ALU.add, ALU.mult,
                        [PB[bks[n]], B_gbuf], [e_b])
                else:
                    act(dst, PS[bks[n]][:, :], AF.Copy, [PB[bks[n]]], [e_b])
            dma("sp", modb_d[idx], e_ap, e_s, reads=[e_b], writes=[])
    P.barrier()
    if stop == "p0":
        return finish_early()
    top[0] = persist_top
    xnT = alloc_bf(16 * W, "xnT").rearrange("p (k n) -> p k n", k=16)
    B_xnT = Buf("xnT")
    gs_b = alloc(2048, "gs_b")
    sh_b = alloc(2048, "sh_b")
    B_gs = Buf("gs")
    S_gs = newsem("gs")
    w_ring = Ring(3, 1024, "wring", bf=True)
    acc_a = alloc(2048, "acc_a")
    acc_b = alloc(2048, "acc_b")
    B_acca = Buf("acca"); B_accb = Buf("accb")
    ph1_top = top[0]
    xs_ring = Ring(2, 2048, "xs")
    t1 = alloc(2048, "t1"); B_t1 = Buf("t1")
    xnb_ring = Ring(2, 1024, "xnb", bf=True)
    junk = alloc_bf(2048, "junk"); B_junk = Buf("junk")
    top[0] = ph1_top
    T0 = alloc(W, "T0"); T1 = alloc(L, "T1"); Ta = alloc(L, "Ta"); Tb = alloc(L, "Tb")
    TsBig = alloc(2 * L, "TsBig"); Ts = TsBig[:, 0:L]; Ts2 = TsBig[:, L:2 * L]; Tz = TsBig[:, 0:W]
    xc_bf = alloc_bf(L, "xcbf")
    yb_ring = Ring(2, 1024, "ybf", bf=True)
    B_T0 = Buf("T0"); B_T1 = Buf("T1"); B_Ta = Buf("Ta"); B_Tb = Buf("Tb"); B_Ts = Buf("Ts"); B_Ts2 = Buf("Ts2"); B_xcbf = Buf("xcbf")
    B_ssq = Buf("ssq")
    ssq = smalld[:, SD_TMP:SD_TMP + 32]
    rs = smalld[:, SD_TMP + 32:SD_TMP + 64]
    B_rs = Buf("rs")
    cp_ctr = [0]

    def copy_any(out, in_, reads, writes):
        cp_ctr[0] += 1
        if cp_ctr[0] % 2:
            return act(out, in_, AF.Copy, reads, writes)
        return P.op("dve", lambda e: e.tensor_copy(out=out, in_=in_), reads, writes)

    def rstd_chain(dst, src, scale, rb, wb):
        ts("dve", dst, src, scale, EPS, ALU.mult, ALU.add, rb, wb)
        act(dst, dst, AF.Sqrt, wb, wb)
        P.op("dve", lambda e: e.reciprocal(out=dst, in_=dst), wb, wb)

    def build_xnT(row0, ntile, dstT, B_dst, g_ap, s_ap, B_g):
        for i in range(ntile):
            x_ap, x_b, x_s = xs_ring.next()
            if isinstance(row0, tuple):
                src = row0[0][row0[1] + i * 128: row0[1] + (i + 1) * 128, :]
            else:
                src = x_all[row0 + i * 128: row0 + (i + 1) * 128, :]
            dma("sp", x_ap, src, x_s, writes=[x_b])
            col = i % 32
            act(junk, x_ap, AF.Square, [x_b], [B_junk, B_ssq], accum_out=ssq[:, col:col + 1])
            rstd_chain(rs[:, col:col + 1], ssq[:, col:col + 1], 1.0 / D, [B_ssq], [B_rs])
            stt("dve", t1, x_ap, rs[:, col:col + 1], g_ap, ALU.mult, ALU.mult, [x_b, B_rs, B_g], [B_t1])
            n_ap, n_b, _ = xnb_ring.next()
            tt("pool", n_ap, t1, s_ap, ALU.add, [B_t1, B_g], [n_b])
            for kb in range(4):
                bk = nbank()
                pv = PS[bk][:, :].bitcast(BF16)
                trg([(pv[:, j * 128:(j + 1) * 128], n_ap[:, (kb * 4 + j) * 128:(kb * 4 + j + 1) * 128], ident_bf) for j in range(4)],
                    [n_b, B_ident], [PB[bk]])
                copy_any(dstT[:, kb * 4:kb * 4 + 4, i * 128:(i + 1) * 128],
                         pv[:, 0:512].rearrange("p (k n) -> p k n", k=4), [PB[bk]], [B_dst])

    def load_w(ct):
        w_ap, w_b, w_s = w_ring.next()
        dma("pool", w_ap, win_d[ct], w_s, writes=[w_b])
        return w_ap.rearrange("p (k j) -> p k j", k=16), w_b

    def inproj(w3, w_b, c0, c1, evac):
        for (a, b) in chunks(c0, c1):
            bk = nbank()
            mmg([(PS[bk][:, 0:b - a], w3[:, k, :], xnT[:, k, a:b], k == 0, k == 15) for k in range(16)],
                [w_b, B_xnT], [PB[bk]])
            evac(PS[bk][:, 0:b - a], PB[bk], a, b)

    def colp(base, idx):
        return smallc[:, base + idx: base + idx + 1]

    sumH = smalld[:, SD_SUMH:SD_SUMH + 80]
    sumA = smalld[:, SD_SUMA:SD_SUMA + 80]
    S_y = newsem("ystore")

    for w in range(5):
        Lw = WIN_L[w]
        Ww = Lw + 2 * HALO
        mine = (w == 4)
        if w == 0:
            dma("sp", gs_b, modb_d[7], S_gs, writes=[B_gs])
            dma("sp", sh_b, modb_d[6], S_gs, writes=[B_gs])
        if w == 1:
            dma("sp", gs_b, modb_d[1], S_gs, writes=[B_gs])
            dma("sp", sh_b, modb_d[0], S_gs, writes=[B_gs])
        build_xnT(WIN_ROW[w], Ww // 128, xnT, B_xnT, gs_b, sh_b, B_gs)
        P.barrier()
        if stop == f"x{w}":
            return finish_early()
        if mine:
            tmpc = smalld[:, SD_TMP + 64:SD_TMP + 80]
            B_tc = Buf("tmpc")
            P.op("dve", lambda e: e.tensor_copy(out=carry, in_=sumH[:, 0:16]), [B_sum[0]], [B_carry])
            for (lo, order, mbase) in ((0, (1, 2, 3), 0), (8, (3, 2, 1), 3)):
                for wo in order:
                    cs = carry[:, lo:lo + 8]
                    tcs = tmpc[:, lo:lo + 8]
                    tt("dve", tcs, sumA[:, wo * 16 + lo: wo * 16 + lo + 8], cs, ALU.mult, [B_sum[wo], B_carry], [B_tc])
                    tt("dve", tcs, tcs, sumH[:, wo * 16 + lo: wo * 16 + lo + 8], ALU.add, [B_sum[wo], B_tc], [B_tc])
                    tt("dve", tcs, tcs, cs, ALU.subtract, [B_tc, B_carry], [B_tc])
                    stt("dve", cs, tcs, colp(C_CM, mbase + wo - 1), cs, ALU.mult, ALU.add, [B_tc, B_carry, B_smallc], [B_carry])
            P.op("pool", lambda e: e.memset(acc_a, 0.0), [], [B_acca])
            P.op("pool", lambda e: e.memset(acc_b, 0.0), [], [B_accb])
        hm_l = colp(C_HM, 2 * w)
        hm_r = colp(C_HM, 2 * w + 1)
        for c in range(8):
            w3, w_b = load_w(8 + c)
            inproj(w3, w_b, 0, Ww, lambda ps, pb, a, b: act(T0[:, a:b], ps, AF.Copy, [pb], [B_T0]))
            ts("dve", T0[:, 0:HALO], T0[:, 0:HALO], hm_l, None, ALU.mult, None, [B_T0, B_smallc], [B_T0])
            ts("dve", T0[:, HALO + Lw:Ww], T0[:, HALO + Lw:Ww], hm_r, None, ALU.mult, None, [B_T0, B_smallc], [B_T0])
            xc = T1[:, 0:Lw]
            ts("dve", xc, T0[:, 62:62 + Lw], colp(C_CAW, c * 4 + 0), colp(C_CAB, c), ALU.mult, ALU.add, [B_T0, B_smallc], [B_T1])
            for tap in (1, 2, 3):
                stt("dve", xc, T0[:, 62 + tap:62 + tap + Lw], colp(C_CAW, c * 4 + tap), xc, ALU.mult, ALU.add, [B_T0, B_T1, B_smallc], [B_T1])
            act(xc_bf[:, 0:Lw], xc, AF.Copy, [B_T1], [B_xcbf])
            for d in range(2):
                dc = d * 8 + c
                hbuf, B_h = (Ts, B_Ts) if d == 0 else (Ts2, B_Ts2)
                for (a, b) in chunks(0, Lw):
                    bk = nbank()
                    mmg([(PS[bk][:, 0:b - a], wr_bf[:, dc * 128:(dc + 1) * 128], xc_bf[:, a:b], True, True)], [B_wri, B_xcbf], [PB[bk]])
                    act(Ta[:, a:b], PS[bk][:, 0:b - a], AF.Sigmoid, [PB[bk], B_smallc], [B_Ta], bias=colp(C_LBR, dc))
                    bk = nbank()
                    mmg([(PS[bk][:, 0:b - a], wi_bf[:, dc * 128:(dc + 1) * 128], xc_bf[:, a:b], True, True)], [B_wri, B_xcbf], [PB[bk]])
                    act(Tb[:, a:b], PS[bk][:, 0:b - a], AF.Sigmoid, [PB[bk], B_smallc], [B_Tb], bias=colp(C_LBI, dc))
                av = Ta[:, 0:Lw]; bv = Tb[:, 0:Lw]; hv = hbuf[:, 0:Lw]
                act(av, av, AF.Exp, [B_Ta, B_cneg], [B_Ta], scale=cneg[:, dc:dc + 1])
                act(hv, av, AF.Square, [B_Ta], [B_h])
                act(hv, hv, AF.Sqrt, [B_h], [B_h], scale=-1.0, bias=1.0)
                tt("dve", bv, bv, xc, ALU.mult, [B_Tb, B_T1], [B_Tb])
                tt("dve", bv, bv, hv, ALU.mult, [B_Tb, B_h], [B_Tb])
                init = carry[:, dc:dc + 1] if mine else 0.0
                if d == 0:
                    P.op("dve", lambda e, hv=hv, av=av, bv=bv, init=init: e.tensor_tensor_scan(
                        out=hv, data0=av, data1=bv, initial=init, op0=ALU.mult, op1=ALU.add), [B_Ta, B_Tb, B_carry], [B_h])
                else:
                    P.op("dve", lambda e, hv=hv, av=av, bv=bv, init=init: e.tensor_tensor_scan(
                        out=rev(hv), data0=rev(av), data1=rev(bv), initial=init, op0=ALU.mult, op1=ALU.add), [B_Ta, B_Tb, B_carry], [B_h])
                if not mine:
                    endcol = hv[:, Lw - 1:Lw] if d == 0 else hv[:, 0:1]
                    P.op("dve", lambda e, endcol=endcol, dc=dc, w=w: e.tensor_copy(out=sumH[:, w * 16 + dc:w * 16 + dc + 1], in_=endcol), [B_h], [B_sum[w]])
                    P.op("dve", lambda e, av=av, dc=dc, w=w: e.tensor_reduce(out=sumA[:, w * 16 + dc:w * 16 + dc + 1], in_=av, axis=AX.X, op=ALU.mult), [B_Ta], [B_sum[w]])
            if not mine:
                continue
            tt("dve", Ts, Ts, Ts2, ALU.add, [B_Ts, B_Ts2], [B_Ts])
            w3, w_b = load_w(c)
            Tg = T0[:, 0:L]
            inproj(w3, w_b, HALO, HALO + L, lambda ps, pb, a, b: act(Tg[:, a - HALO:b - HALO], ps, AF.Copy, [pb], [B_T0]))
            Tq = Ta
            tt("dve", Tq, Tg, Tg, ALU.mult, [B_T0], [B_Ta])
            ts("dve", Tq, Tq, 0.044715, 1.0, ALU.mult, ALU.add, [B_Ta], [B_Ta])
            tt("dve", Tq, Tq, Tg, ALU.mult, [B_Ta, B_T0], [B_Ta])
            act(Tq, Tq, AF.Sigmoid, [B_Ta], [B_Ta], scale=1.5957691216057308)
            tt("dve", Tq, Tq, Tg, ALU.mult, [B_Ta, B_T0], [B_Ta])
            tt("dve", Tq, Tq, Ts, ALU.mult, [B_Ta, B_Ts], [B_Ta])
            tt("pool", Tb, Tq, Tq, ALU.mult, [B_Ta], [B_Tb])
            tt("pool", acc_a, acc_a, Tb, ALU.add, [B_Tb, B_acca], [B_acca])
            y_ap, y_b, y_s = yb_ring.next()
            act(y_ap, Tq, AF.Copy, [B_Ta, B_smallc], [y_b], scale=colp(C_GOA, c))
            dma("sp", y_d[c], y_ap, y_s, reads=[y_b])
        if not mine:
            P.barrier()
            if stop == f"w{w}":
                return finish_early()
            continue
        for c in range(8):
            Tc = T0
            w3, w_b = load_w(24 + c)
            inproj(w3, w_b, 0, W, lambda ps, pb, a, b: act(Tc[:, a:b], ps, AF.Copy, [pb], [B_T0]))
            w3, w_b = load_w(32 + c)
            inproj(w3, w_b, 0, W, lambda ps, pb, a, b: tt("dve", Tz[:, a:b], ps, Tc[:, a:b], ALU.mult, [pb, B_T0], [B_Ts, B_Ts2]))
            ts("dve", Tz[:, 0:HALO], Tz[:, 0:HALO], hm_l, None, ALU.mult, None, [B_Ts, B_Ts2, B_smallc], [B_Ts, B_Ts2])
            ts("dve", Tz[:, HALO + L:W], Tz[:, HALO + L:W], hm_r, None, ALU.mult, None, [B_Ts, B_Ts2, B_smallc], [B_Ts, B_Ts2])
            Tcv = T1
            w0 = colp(C_CBW, c * 3 + 0); w1 = colp(C_CBW, c * 3 + 1); w2 = colp(C_CBW, c * 3 + 2)
            ts("dve", Tcv, Tz[:, HALO:HALO + L], w1, None, ALU.mult, None, [B_Ts, B_Ts2, B_smallc], [B_T1])
            if c < 4:
                zv = Tz[:, HALO:HALO + L].rearrange("p (r c) -> p r c", c=64)
                ov = Tcv.rearrange("p (r c) -> p r c", c=64)
                stt("dve", ov[:, :, 1:64], zv[:, :, 0:63], w0, ov[:, :, 1:64], ALU.mult, ALU.add, [B_Ts, B_Ts2, B_T1, B_smallc], [B_T1])
                stt("dve", ov[:, :, 0:63], zv[:, :, 1:64], w2, ov[:, :, 0:63], ALU.mult, ALU.add, [B_Ts, B_Ts2, B_T1, B_smallc], [B_T1])
            else:
                stt("dve", Tcv, Tz[:, 0:L], w0, Tcv, ALU.mult, ALU.add, [B_Ts, B_Ts2, B_T1, B_smallc], [B_T1])
                stt("dve", Tcv, Tz[:, 2 * HALO:2 * HALO + L], w2, Tcv, ALU.mult, ALU.add, [B_Ts, B_Ts2, B_T1, B_smallc], [B_T1])
            w3, w_b = load_w(16 + c)
            Ty = Ta
            inproj(w3, w_b, HALO, HALO + L, lambda ps, pb, a, b: tt("dve", Ty[:, a - HALO:b - HALO], ps, Tcv[:, a - HALO:b - HALO], ALU.mult, [pb, B_T1], [B_Ta]))
            tt("pool", Tb, Ty, Ty, ALU.mult, [B_Ta], [B_Tb])
            tt("pool", acc_b, acc_b, Tb, ALU.add, [B_Tb, B_accb], [B_accb])
            y_ap, y_b, y_s = yb_ring.next()
            act(y_ap, Ty, AF.Copy, [B_Ta, B_smallc], [y_b], scale=colp(C_GOB, c))
            dma("sp", y_d[8 + c], y_ap, y_s, reads=[y_b])
        bk = nbank()
        for g, (acc, B_acc) in enumerate(((acc_a, B_acca), (acc_b, B_accb))):
            for i in range(16):
                j = g * 16 + i
                mmg([(PS[bk][:, 2 * j:2 * j + 2], acc[:, i * 128:(i + 1) * 128], ones_f[:, 0:2], True, True)], [B_acc, B_smallc], [PB[bk]])
        rstd_ab = smalld[:, SD_RSTD:SD_RSTD + 64]
        P.op("dve", lambda e, src=PS[bk][:, 0:64], dst=rstd_ab: e.tensor_copy(out=dst, in_=src), [PB[bk]], [B_rstd])
        rstd_chain(rstd_ab, rstd_ab, 1.0 / 1024, [B_rstd], [B_rstd])
    P.barrier()
    if stop == "p1":
        return finish_early()

    top[0] = persist_top
    wout = alloc_bf(16 * D, "wout").rearrange("p (c n) -> p c n", c=16)
    B_wout = Buf("wout")
    S_wout = newsem("wout")
    for c in range(16):
        dma("pool", wout[:, c, :], wout_d[c * 128:(c + 1) * 128, :], S_wout, writes=[B_wout])
    if stop == "p2a":
        return finish_early()
    ys_ring = Ring(2, 4096, "ysb", bf=True)
    xs2_ring = Ring(2, 2048, "xs2")
    gt1_b = alloc(2048, "gt1")
    B_gt1 = Buf("gt1")
    dma("sp", gt1_b, modb_d[2], S_gs, writes=[B_gt1])
    tA_ring = Ring(2, 512, "tA")
    tB_ring = Ring(2, 512, "tB")
    S_x1 = newsem("x1store")
    rstd_ab = smalld[:, SD_RSTD:SD_RSTD + 64]
    mine_row = WIN_ROW[4] + HALO
    ys3 = None
    for i in range(16):
        if i % 4 == 0:
            y_ap, y_b, y_s = ys_ring.next()
            ys3 = y_ap.rearrange("p (c n) -> p c n", c=16)
            for c in range(16):
                dma("sp", ys3[:, c, :], y_d[c][:, (i // 4) * 512:(i // 4 + 1) * 512], y_s, reads=[], writes=[y_b])
            ys_b = y_b
        x_ap, x_b, x_s = xs2_ring.next()
        dma("sp", x_ap, x_all[mine_row + i * 128: mine_row + (i + 1) * 128, :], x_s, writes=[x_b])
        to = (i % 4) * 128
        for n in range(4):
            bA = nbank(); bB = nbank()
            mmg([(PS[bA][:, :], ys3[:, c, to:to + 128], wout[:, c, n * 512:(n + 1) * 512], c == 0, c == 7) for c in range(8)], [ys_b, B_wout], [PB[bA]])
            mmg([(PS[bB][:, :], ys3[:, c, to:to + 128], wout[:, c, n * 512:(n + 1) * 512], c == 8, c == 15) for c in range(8, 16)], [ys_b, B_wout], [PB[bB]])
            ta, ta_b, _ = tA_ring.next()
            tb, tb_b, _ = tB_ring.next()
            act(ta, PS[bA][:, :], AF.Copy, [PB[bA], B_rstd], [ta_b], scale=rstd_ab[:, 2 * i:2 * i + 1])
            stt("dve", tb, PS[bB][:, :], rstd_ab[:, 32 + 2 * i:32 + 2 * i + 1], ta, ALU.mult, ALU.add, [PB[bB], B_rstd, ta_b], [tb_b])
            tt("pool", tb, tb, gt1_b[:, n * 512:(n + 1) * 512], ALU.mult, [tb_b, B_gt1], [tb_b])
            tt("pool", x_ap[:, n * 512:(n + 1) * 512], x_ap[:, n * 512:(n + 1) * 512], tb, ALU.add, [tb_b, x_b], [x_b])
        if stop == "p2b":
            return finish_early()
        dst = out_d if stage < 3 else x1_d
        dma("sp", dst[i * 128:(i + 1) * 128, :], x_ap, x_s, reads=[x_b])
    P.barrier()
    out_sems = [s for (_, _, s) in xs2_ring.slots]

    if stage >= 3:
        top[0] = persist_top
        acc = [alloc(2048, f"acc{i}") for i in range(4)]
        B_accs = [Buf(f"acc{i}") for i in range(4)]
        S_acc = [newsem(f"acc{i}") for i in range(4)]
        xn2T = alloc_bf(16 * 512, "xn2T").rearrange("p (k n) -> p k n", k=16)
        B_xn2T = Buf("xn2T")
        actb = alloc_bf(16 * 512, "actb").rearrange("p (f n) -> p f n", f=16)
        B_actb = Buf("actb")
        wd_ring = Ring(3, 4096, "wd", bf=True)
        wgu_ring = Ring(4, 2048, "wgu", bf=True)
        mod0 = alloc(2048, "mod0"); mod1 = alloc(2048, "mod1")
        B_mod0 = Buf("mod0"); B_mod1 = Buf("mod1")
        S_mod0 = newsem("mod0"); S_mod1 = newsem("mod1"); S_bd = newsem("bd")
        bd_sb = alloc(2048, "bd")
        B_bd = Buf("bd")
        dma("sp", bd_sb[0:32, :], bd_d[:, :], S_bd, writes=[B_bd])
        G = alloc(128, "G").rearrange("p (i e) -> p i e", i=4)
        B_G = Buf("G")
        GT = alloc(512, "GT")
        B_GT = Buf("GT")
        lg = alloc(32, "lg"); ex = alloc(32, "ex"); mk = alloc(32, "mk"); m8 = alloc(8, "m8"); sm = alloc(4, "sm")
        B_lg = Buf("lg"); B_ex = Buf("ex"); B_mk = Buf("mk"); B_m8 = Buf("m8"); B_sm = Buf("sm")
        tmp_top = top[0]
        t1m = alloc(2048, "t1m"); B_t1m = Buf("t1m")
        xnbm = alloc_bf(2048, "xnbm"); B_xnbm = Buf("xnbm")
        junkm = alloc_bf(2048, "junkm"); B_junkm = Buf("junkm")
        top[0] = tmp_top
        tg_ring = Ring(2, 512, "tg"); tsg_ring = Ring(2, 512, "tsg"); tu_ring = Ring(2, 512, "tu"); td_ring = Ring(2, 512, "td")
        bgc = smallc[:, C_BG:C_BG + 512]
        buc = smallc[:, C_BU:C_BU + 512]
        brt = smallc[:, C_BRT:C_BRT + 32]
        out_sems = S_acc
        for q in range(nq):
            dma("sp", mod0, modb_d[4], S_mod0, writes=[B_mod0])
            dma("sp", mod1, modb_d[3], S_mod1, writes=[B_mod1])
            for i in range(4):
                r0 = (q * 4 + i) * 128
                dma("sp", acc[i], x1_d[r0:r0 + 128, :], S_acc[i], writes=[B_accs[i]])
                act(junkm, acc[i], AF.Square, [B_accs[i]], [B_junkm, B_ssq], accum_out=ssq[:, i:i + 1])
                rstd_chain(rs[:, i:i + 1], ssq[:, i:i + 1], 1.0 / D, [B_ssq], [B_rs])
                stt("dve", t1m, acc[i], rs[:, i:i + 1], mod0, ALU.mult, ALU.mult, [B_accs[i], B_rs, B_mod0], [B_t1m])
                tt("pool", xnbm, t1m, mod1, ALU.add, [B_t1m, B_mod1], [B_xnbm])
                for kb in range(4):
                    bk = nbank()
                    pv = PS[bk][:, :].bitcast(BF16)
                    trg([(pv[:, j * 128:(j + 1) * 128], xnbm[:, (kb * 4 + j) * 128:(kb * 4 + j + 1) * 128], ident_bf) for j in range(4)],
                        [B_xnbm, B_ident], [PB[bk]])
                    copy_any(xn2T[:, kb * 4:kb * 4 + 4, i * 128:(i + 1) * 128],
                             pv[:, 0:512].rearrange("p (k n) -> p k n", k=4), [PB[bk]], [B_xn2T])
                bk = nbank()
                wrt3 = wrt_bf.rearrange("p (k e) -> p k e", k=16)
                mmg([(PS[bk][:, 0:32], xn2T[:, k, i * 128:(i + 1) * 128], wrt3[:, k, :], k == 0, k == 15) for k in range(16)],
                    [B_xn2T, B_wrt], [PB[bk]])
                tt("dve", lg, PS[bk][:, 0:32], brt, ALU.add, [PB[bk], B_smallc], [B_lg])
                P.op("dve", lambda e: e.max(out=m8, in_=lg), [B_lg], [B_m8])
                ts("dve", sm[:, 0:1], m8[:, 0:1], -1.0, None, ALU.mult, None, [B_m8], [B_sm])
                act(ex, lg, AF.Exp, [B_lg, B_sm], [B_ex], bias=sm[:, 0:1])
                ts("dve", mk, lg, m8[:, 3:4], None, ALU.is_ge, None, [B_lg, B_m8], [B_mk])
                tt("dve", ex, ex, mk, ALU.mult, [B_ex, B_mk], [B_ex])
                P.op("dve", lambda e: e.tensor_reduce(out=sm[:, 1:2], in_=ex, axis=AX.X, op=ALU.add), [B_ex], [B_sm])
                P.op("dve", lambda e: e.reciprocal(out=sm[:, 2:3], in_=sm[:, 1:2]), [B_sm], [B_sm])
                ts("dve", G[:, i, :], ex, sm[:, 2:3], None, ALU.mult, None, [B_ex, B_sm], [B_G])
                bk = nbank()
                trg([(PS[bk][0:32, 0:128], G[:, i, :], ident_f)], [B_G, B_smallc], [PB[bk]])
                act(GT[0:32, i * 128:(i + 1) * 128], PS[bk][0:32, 0:128], AF.Copy, [PB[bk]], [B_GT])
            P.barrier()
            dma("sp", mod0, modb_d[5], S_mod0, writes=[B_mod0])
            dma("sp", mod1, gfin_d[:, :], S_mod1, writes=[B_mod1])
            for i in range(4):
                for m in range(4):
                    bk = nbank()
                    mmg([(PS[bk][:, :], GT[0:32, i * 128:(i + 1) * 128], bd_sb[0:32, m * 512:(m + 1) * 512], True, True)], [B_GT, B_bd], [PB[bk]])
                    td, td_b, _ = td_ring.next()
                    tt("dve", td, PS[bk][:, :], mod0[:, m * 512:(m + 1) * 512], ALU.mult, [PB[bk], B_mod0], [td_b])
                    tt("pool", acc[i][:, m * 512:(m + 1) * 512], acc[i][:, m * 512:(m + 1) * 512], td, ALU.add, [td_b, B_accs[i]], [B_accs[i]])
            units = [(e_, f_) for e_ in range(NEXP) for f_ in range(16)]
            uslot = {}
            dslot = {}
            PF = 3

            def issue_wgu(u):
                e_, f_ = units[u]
                u_ap, u_b, u_s = wgu_ring.next()
                dma("pool", u_ap, wgu_d[e_ * 16 + f_], u_s, writes=[u_b])
                uslot[u] = (u_ap, u_b)

            def issue_wd(e_, m_):
                d_ap, d_b, d_s = wd_ring.next()
                d3_ = d_ap.rearrange("p (f n) -> p f n", f=16)
                dma("pool", d3_, wd_d[e_].rearrange("(f p) d -> p f d", p=128)[:, :, m_ * 512:(m_ + 1) * 512], d_s, writes=[d_b])
                dslot[(e_, m_)] = (d3_, d_b)

            for u in range(PF):
                issue_wgu(u)
            for u, (ex_i, f) in enumerate(units):
                if u + PF < len(units):
                    issue_wgu(u + PF)
                if f in (3, 7, 11):
                    issue_wd(ex_i, (f - 3) // 4)
                u_ap, u_b = uslot.pop(u)
                u4 = u_ap.rearrange("p (t k j) -> p t k j", t=2, k=16)
                bG = nbank(); bU = nbank()
                mmg([(PS[bG][:, :], u4[:, 0, k, :], xn2T[:, k, :], k == 0, k == 15) for k in range(16)], [u_b, B_xn2T], [PB[bG]])
                mmg([(PS[bU][:, :], u4[:, 1, k, :], xn2T[:, k, :], k == 0, k == 15) for k in range(16)], [u_b, B_xn2T], [PB[bU]])
                tg, tg_b, _ = tg_ring.next(); tsg, tsg_b, _ = tsg_ring.next(); tu, tu_b, _ = tu_ring.next()
                cb = ex_i * 16 + f
                ts("dve", tg, PS[bG][:, :], bgc[:, cb:cb + 1], 7.0, ALU.add, ALU.min, [PB[bG], B_smallc], [tg_b])
                act(tsg, tg, AF.Sigmoid, [tg_b], [tsg_b], scale=1.702)
                act(tu, PS[bU][:, :], AF.Identity, [PB[bU], B_smallc], [tu_b], bias=buc[:, cb:cb + 1])
                ts("dve", tu, tu, 7.0, -7.0, ALU.min, ALU.max, [tu_b], [tu_b])
                stt("dve", tu, tu, 1.0, tg, ALU.add, ALU.mult, [tu_b, tg_b], [tu_b])
                tt("pool", actb[:, f, :], tu, tsg, ALU.mult, [tu_b, tsg_b], [B_actb])
                if f != 15:
                    continue
                for m in range(4):
                    d3, d_b = dslot.pop((ex_i, m))
                    for i in range(4):
                        bk = nbank()
                        mmg([(PS[bk][:, :], actb[:, ff, i * 128:(i + 1) * 128], d3[:, ff, :], ff == 0, ff == 15) for ff in range(16)], [B_actb, d_b], [PB[bk]])
                        td, td_b, _ = td_ring.next()
                        stt("dve", td, PS[bk][:, :], G[:, i, ex_i:ex_i + 1], mod0[:, m * 512:(m + 1) * 512], ALU.mult, ALU.mult, [PB[bk], B_G, B_mod0], [td_b])
                        tt("pool", acc[i][:, m * 512:(m + 1) * 512], acc[i][:, m * 512:(m + 1) * 512], td, ALU.add, [td_b, B_accs[i]], [B_accs[i]])
                    if m == 0:
                        issue_wd(ex_i, 3)
            P.barrier()
            for i in range(4):
                r0 = (q * 4 + i) * 128
                act(junkm, acc[i], AF.Square, [B_accs[i]], [B_junkm, B_ssq], accum_out=ssq[:, 8 + i:9 + i])
                rstd_chain(rs[:, 8 + i:9 + i], ssq[:, 8 + i:9 + i], 1.0 / D, [B_ssq], [B_rs])
                stt("dve", acc[i], acc[i], rs[:, 8 + i:9 + i], mod1, ALU.mult, ALU.mult, [B_accs[i], B_rs, B_mod1], [B_accs[i]])
                dma("sp", out_d[r0:r0 + 128, :], acc[i], S_acc[i], reads=[B_accs[i]])
            P.barrier()
    P.final_wait("sp", out_sems)
    with nc.Block() as block:
        P.emit(block)
    es.close()
    return nc


_NC_CACHE = {}


def _prep_shared(inp, stage):
    f = np.float32
    sh = {}
    sh["w_mod"] = np.ascontiguousarray(inp["w_mod"][0], dtype=f)
    sh["b_mod"] = np.ascontiguousarray(inp["b_mod"][0].reshape(1, -1), dtype=f)
    sh["g_mix_b"] = np.ascontiguousarray(np.broadcast_to(inp["g_mix"][0], (128, D)), dtype=f)
    sh["g_ffn_b"] = np.ascontiguousarray(np.broadcast_to(inp["g_ffn"][0], (128, D)), dtype=f)
    sh["g_fin_b"] = np.ascontiguousarray(np.broadcast_to(inp["g_final"], (128, D)), dtype=f)
    w_in = np.asarray(inp["w_in"][0], dtype=f)
    sh["w_in_h"] = np.ascontiguousarray(w_in.reshape(16, 128, 40, 128).transpose(2, 1, 0, 3)).reshape(40, 128, 2048)
    sh["wr_h"] = np.ascontiguousarray(np.asarray(inp["lru_w_r"][0], dtype=f).transpose(2, 0, 1, 3)).reshape(128, 2048)
    sh["wi_h"] = np.ascontiguousarray(np.asarray(inp["lru_w_i"][0], dtype=f).transpose(2, 0, 1, 3)).reshape(128, 2048)
    sh["w_out"] = np.ascontiguousarray(inp["w_out"][0], dtype=f)
    if stage >= 3:
        sh["w_router_h"] = np.ascontiguousarray(np.asarray(inp["w_router"][0], dtype=f).reshape(16, 128, 32).transpose(1, 0, 2)).reshape(128, 512)
        wg = np.asarray(inp["w_gate"][0], dtype=f).reshape(NEXP, 16, 128, 16, 128)
        wu = np.asarray(inp["w_up"][0], dtype=f).reshape(NEXP, 16, 128, 16, 128)
        wgu = np.empty((NEXP, 16, 128, 2, 16, 128), dtype=f)
        wgu[:, :, :, 0] = wg.transpose(0, 3, 2, 1, 4)
        wgu[:, :, :, 1] = wu.transpose(0, 3, 2, 1, 4)
        sh["wgu_h"] = wgu.reshape(NEXP * 16, 128, 4096)
        sh["w_down"] = np.ascontiguousarray(inp["w_down"][0], dtype=f)
        sh["b_down"] = np.ascontiguousarray(inp["b_down"][0], dtype=f)
    return sh


def _prep_core(inp, k):
    f = np.float32
    b, j = k // 4, k % 4
    x = np.asarray(inp["x"], dtype=f)
    ctx = np.asarray(inp["ctx"], dtype=f)
    S = x.shape[1]
    x_all = np.zeros((XROWS, D), dtype=f)
    x_all[HALO:HALO + LC] = ctx[b]
    others = [jj for jj in range(4) if jj != j]
    chunks_ = others + [j]
    hm = np.zeros(10, dtype=f)
    for wi, jj in enumerate(chunks_):
        w = wi + 1
        t0 = jj * L - HALO
        lo, hi = max(t0, 0), min(t0 + W, S)
        r0 = WIN_ROW[w] + (lo - t0)
        x_all[r0:r0 + (hi - lo)] = x[b, lo:hi]
        hm[2 * w] = 1.0 if jj > 0 else 0.0
        hm[2 * w + 1] = 1.0 if jj < 3 else 0.0
    cm = np.zeros(6, dtype=f)
    for o, jj in enumerate(others):
        cm[o] = 1.0 if jj < j else 0.0
        cm[3 + o] = 1.0 - cm[o]
    sc = np.zeros((128, NS), dtype=f)

    def colT(v, n):
        return np.asarray(v, dtype=f).reshape(n, 128).T

    sc[:, C_CT:C_CT + 16] = colT(inp["c"][b], 16)
    sc[:, C_CX:C_CX + 16] = colT(inp["c_ctx"], 16)
    caw = np.asarray(inp["conv_a_w"][0], dtype=f)
    sc[:, C_CAW:C_CAW + 32] = caw.reshape(4, 8, 128).transpose(2, 1, 0).reshape(128, 32)
    sc[:, C_CAB:C_CAB + 8] = colT(inp["conv_a_b"][0], 8)
    sc[:, C_LBR:C_LBR + 16] = colT(np.asarray(inp["lru_b_r"][0]).reshape(-1), 16)
    sc[:, C_LBI:C_LBI + 16] = colT(np.asarray(inp["lru_b_i"][0]).reshape(-1), 16)
    sc[:, C_LAM:C_LAM + 16] = colT(np.asarray(inp["lru_lam"][0]).reshape(-1), 16)
    cbw = np.asarray(inp["conv_b_w"][0], dtype=f)
    sc[:, C_CBW:C_CBW + 24] = cbw.reshape(3, 8, 128).transpose(2, 1, 0).reshape(128, 24)
    sc[:, C_GOA:C_GOA + 8] = colT(inp["g_out_a"][0], 8)
    sc[:, C_GOB:C_GOB + 8] = colT(inp["g_out_b"][0], 8)
    sc[:, C_HM:C_HM + 10] = hm[None, :]
    sc[:, C_CM:C_CM + 6] = cm[None, :]
    sc[:, C_BG:C_BG + 512] = np.asarray(inp["b_gate"][0], dtype=f).reshape(NEXP, 16, 128).transpose(2, 0, 1).reshape(128, 512)
    sc[:, C_BU:C_BU + 512] = np.asarray(inp["b_up"][0], dtype=f).reshape(NEXP, 16, 128).transpose(2, 0, 1).reshape(128, 512)
    sc[:, C_BRT:C_BRT + 32] = np.asarray(inp["b_router"][0], dtype=f)[None, :]
    sc[:, C_ID:C_ID + 128] = np.eye(128, dtype=f)
    sc[:, C_ONE:C_ONE + 128] = 1.0
    return {"x_all": x_all, "smallc": sc}


def run(inputs, stage=3, stop=None):
    if (stage, stop) not in _NC_CACHE:
        _NC_CACHE[(stage, stop)] = build(stage, stop)
    nc = _NC_CACHE[(stage, stop)]
    shared = _prep_shared(inputs, stage)
    in_maps = []
    for k in range(NCORE):
        m = dict(shared)
        m.update(_prep_core(inputs, k))
        in_maps.append(m)
    res = run_bass_kernel_spmd(nc, in_maps, core_ids=list(range(NCORE)))
    outs = [np.asarray(r["out"]) for r in res.results]
    return np.concatenate(outs, axis=0).reshape(2, 4 * L, D).astype(np.float32)


def kernel(**inputs):
    return run(inputs, stage=3)
```

```python
import numpy as np
from contextlib import ExitStack
import concourse.bass as bass
import concourse.mybir as mybir
from concourse.bass_utils import run_bass_kernel_spmd

F32 = mybir.dt.float32
BF16 = mybir.dt.bfloat16
AF = mybir.ActivationFunctionType
ALU = mybir.AluOpType
AX = mybir.AxisListType

D = 2048
L = 2048
HALO = 64
W = L + 2 * HALO
LC = 256
WC = LC + 2 * HALO
NCORE = 8
EPS = 1e-6
NEXP = 32
WIN_L = [LC, L, L, L, L]
WIN_ROW = [0, WC, WC + W, WC + 2 * W, WC + 3 * W]
XROWS = WC + 4 * W

_o = 0
def _col(n):
    global _o
    r = _o
    _o += n
    return r
C_CT = _col(16); C_CX = _col(16); C_CAW = _col(32); C_CAB = _col(8); C_LBR = _col(16); C_LBI = _col(16)
C_LAM = _col(16); C_CBW = _col(24); C_GOA = _col(8); C_GOB = _col(8); C_HM = _col(10); C_CM = _col(6)
C_BG = _col(512); C_BU = _col(512); C_BRT = _col(32); C_ID = _col(128); C_ONE = _col(128)
C_TRI = _col(128); C_PIDX = _col(128); C_IOTA = _col(384); C_SIDX = _col(3)
NS = _o
CAP = 384
NST = CAP // 128

ENGS = ("pe", "act", "dve", "pool", "sp")
SAME_ENGINE_SYNC = True


ALLBUFS = []
ALLRINGS = []


class Buf:
    __slots__ = ("name", "w", "r")

    def __init__(self, name=""):
        self.name = name
        self.w = None
        self.r = {}
        ALLBUFS.append(self)


class Sem:
    def __init__(self, h, name):
        self.h = h
        self.n = 0
        self.name = name


class Prog:
    def __init__(self):
        self.opsr = {r: {e: [] for e in ENGS} for r in ("pre", "A", "B")}
        self.region = "pre"
        self.regs = {}
        self.esem = {}
        self.waited = {e: {} for e in ENGS}
        self.pending = {e: [] for e in ENGS}
        self.allsems = []

    def op(self, eng, fn, reads=(), writes=(), dma=None):
        deps = self.pending[eng]
        self.pending[eng] = []
        for b in reads:
            if b.w is not None:
                deps.append(b.w)
        for b in writes:
            if b.w is not None:
                deps.append(b.w)
            deps.extend(b.r.values())
        wd = self.waited[eng]
        best = {}
        own = self.esem.get(eng)
        for (s, v) in deps:
            if (not SAME_ENGINE_SYNC) and s is own:
                continue
            if wd.get(id(s), 0) >= v:
                continue
            if id(s) not in best or best[id(s)][1] < v:
                best[id(s)] = (s, v)
        waits = []
        for s, v in best.values():
            waits.append((s, v))
            wd[id(s)] = v
        if dma is not None:
            dma.n += 16
            ev = (dma, dma.n)
            inc = (dma, 16)
        else:
            s = self.esem[eng]
            s.n += 1
            ev = (s, s.n)
            inc = (s, 1)
        for b in reads:
            old = b.r.get(id(ev[0]))
            if old is None or old[1] < ev[1]:
                b.r[id(ev[0])] = ev
        for b in writes:
            b.w = ev
            b.r = {}
        self.opsr[self.region][eng].append((waits, fn, inc))
        return ev

    def barrier(self):
        for e in ENGS:
            self.pending[e] = [(s, s.n) for s in self.allsems if s.n > 0]

    def final_wait(self, eng, sems):
        waits = [(s, s.n) for s in sems if s.n > 0]
        self.opsr[self.region][eng].append((waits, None, None))

    def snapshot(self, counters):
        return dict(bufs=[(b, b.w, dict(b.r)) for b in ALLBUFS], sems=[(s, s.n) for s in self.allsems],
                    waited={e: dict(self.waited[e]) for e in ENGS}, pending={e: list(self.pending[e]) for e in ENGS},
                    rings=[(r, r.i) for r in ALLRINGS], counters=[(c, c[0]) for c in counters], nsems=len(self.allsems))

    def restore(self, st):
        for b, w, r in st["bufs"]:
            b.w = w
            b.r = dict(r)
        for s_, n in st["sems"]:
            s_.n = n
        del self.allsems[st["nsems"]:]
        self.waited = {e: dict(st["waited"][e]) for e in ENGS}
        self.pending = {e: list(st["pending"][e]) for e in ENGS}
        for r, i in st["rings"]:
            r.i = i
        for c, v in st["counters"]:
            c[0] = v

    def emit(self, block, branched=False):
        def mk(name):
            def body(eng):
                def run(lst):
                    for waits, fn, inc in lst:
                        for s, v in waits:
                            eng.wait_ge(s.h, v)
                        if fn is not None:
                            ins = fn(eng)
                            ins.then_inc(inc[0].h, inc[1])
                if not branched:
                    run(self.opsr["pre"][name])
                    return
                reg = eng.alloc_register(f"flag_{name}")
                self.regs[name] = reg
                run(self.opsr["pre"][name])
                with eng.If(eng.snap(reg) > 0):
                    run(self.opsr["B"][name])
                with eng.Else():
                    run(self.opsr["A"][name])
            return body
        block.sync(mk("sp"))
        block.tensor(mk("pe"))
        block.scalar(mk("act"))
        block.vector(mk("dve"))
        block.gpsimd(mk("pool"))


def rev(ap):
    a = ap.ap
    assert len(a) == 2 and a[1][0] == 1, a
    n = a[1][1]
    return bass.AP(ap.tensor, ap.offset + (n - 1), [list(a[0]), [-1, n]])


def chunks(lo, hi, step=512):
    out = []
    while lo < hi:
        out.append((lo, min(lo + step, hi)))
        lo += step
    return out


def build(stage=3, stop=None, nq=4, sparse=True):
    del ALLBUFS[:]
    del ALLRINGS[:]
    nc = bass.Bass("TRN2", target_bir_lowering=False)
    P = Prog()
    es = ExitStack()

    def din(name, shape, dt=F32):
        return nc.dram_tensor(name, list(shape), dt, kind="ExternalInput").ap()

    x_all = din("x_all", [XROWS, D])
    smallc_d = din("smallc", [128, NS])
    wmod_d = din("w_mod", [D, 6 * D])
    bmod_d = din("b_mod", [1, 6 * D])
    gmix_d = din("g_mix_b", [128, D])
    gffn_d = din("g_ffn_b", [128, D])
    gfin_d = din("g_fin_b", [128, D])
    win_d = din("w_in_h", [40, 128, 16 * 128])
    wr_d = din("wr_h", [128, 16 * 128])
    wi_d = din("wi_h", [128, 16 * 128])
    wout_d = din("w_out", [D, D])
    if stage >= 3:
        wrt_d = din("w_router_h", [128, 16 * 32])
        wgu_d = din("wgu_h", [NEXP * 16, 128, 2 * 16 * 128])
        wd_d = din("w_down", [NEXP, D, D])
        bd_d = din("b_down", [NEXP, D])
    out_d = nc.dram_tensor("out", [L, D], F32, kind="ExternalOutput").ap()
    modb_d = nc.dram_tensor("modb", [8, 128, D], F32).ap()
    y_d = nc.dram_tensor("y_scr", [16, 128, L], BF16).ap()
    x1_d = nc.dram_tensor("x1_scr", [L, D], F32).ap()

    ARENA = 53200
    arena = es.enter_context(nc.sbuf_tensor("arena", [128, ARENA], F32))
    PS = [es.enter_context(nc.psum_tensor(f"ps{i}", [128, 512], F32)) for i in range(8)]
    PB = [Buf(f"ps{i}") for i in range(8)]
    for e in ENGS:
        P.esem[e] = Sem(es.enter_context(nc.semaphore(f"s_{e}")), e)
        P.allsems.append(P.esem[e])
    nsem = [0]

    def newsem(name):
        nsem[0] += 1
        s = Sem(es.enter_context(nc.semaphore(f"d{nsem[0]}_{name}")), name)
        P.allsems.append(s)
        return s

    bank_ctr = [0]

    def nbank():
        b = bank_ctr[0] % 8
        bank_ctr[0] += 1
        return b

    top = [0]

    def alloc(n_f32, name=""):
        a = top[0]
        top[0] += n_f32
        assert top[0] <= ARENA, (name, top[0])
        return arena[:, a:a + n_f32]

    def alloc_bf(n_bf, name=""):
        assert n_bf % 2 == 0
        return alloc(n_bf // 2, name).bitcast(BF16)

    def dma(q, out, in_, sem, reads=(), writes=()):
        return P.op(q, lambda e: e.dma_start(out=out, in_=in_), reads, writes, dma=sem)

    def act(out, in_, func, reads, writes, bias=None, scale=None, accum_out=None):
        kw = {}
        if bias is not None:
            kw["bias"] = bias
        if scale is not None:
            kw["scale"] = scale
        if accum_out is not None:
            kw["accum_out"] = accum_out
        return P.op("act", lambda e: e.activation(out=out, in_=in_, func=func, **kw), reads, writes)

    def ts(eng, out, in0, s1, s2, op0, op1, reads, writes):
        if s2 is None:
            return P.op(eng, lambda e: e.tensor_scalar(out=out, in0=in0, scalar1=s1, scalar2=None, op0=op0), reads, writes)
        return P.op(eng, lambda e: e.tensor_scalar(out=out, in0=in0, scalar1=s1, scalar2=s2, op0=op0, op1=op1), reads, writes)

    def tt(eng, out, in0, in1, op, reads, writes):
        return P.op(eng, lambda e: e.tensor_tensor(out=out, in0=in0, in1=in1, op=op), reads, writes)

    def stt(eng, out, in0, scalar, in1, op0, op1, reads, writes):
        return P.op(eng, lambda e: e.scalar_tensor_tensor(out=out, in0=in0, scalar=scalar, in1=in1, op0=op0, op1=op1), reads, writes)

    def mmg(items, reads, writes):
        def fn(e):
            ins = None
            for (o, l, r, s, t) in items:
                ins = e.matmul(o, l, r, start=s, stop=t)
            return ins
        return P.op("pe", fn, reads, writes)

    def trg(items, reads, writes):
        def fn(e):
            ins = None
            for (o, i_, idn) in items:
                ins = e.transpose(o, i_, idn)
            return ins
        return P.op("pe", fn, reads, writes)

    class Ring:
        def __init__(self, n, n_f32, name, bf=False, shape=None):
            self.slots = []
            for i in range(n):
                ap = alloc(n_f32, name)
                if bf:
                    ap = ap.bitcast(BF16)
                self.slots.append((ap, Buf(f"{name}{i}"), newsem(f"{name}{i}")))
            self.i = 0
            ALLRINGS.append(self)

        def next(self):
            s = self.slots[self.i % len(self.slots)]
            self.i += 1
            return s

    def finish_early():
        S_e = newsem("early")
        P.barrier()
        dma("sp", out_d[0:128, :], arena[:, 0:2048], S_e)
        P.final_wait("sp", [S_e])
        with nc.Block() as block:
            P.emit(block)
        es.close()
        return nc

    smallc = alloc(NS, "smallc")
    B_smallc = Buf("smallc")
    S_const = newsem("const")
    dma("sp", smallc, smallc_d[:, :], S_const, writes=[B_smallc])
    wr_bf = alloc_bf(2048, "wr")
    wi_bf = alloc_bf(2048, "wi")
    B_wri = Buf("wri")
    S_wri = newsem("wri")
    dma("pool", wr_bf, wr_d[:, :], S_wri, writes=[B_wri])
    dma("pool", wi_bf, wi_d[:, :], S_wri, writes=[B_wri])
    if stage >= 3:
        wrt_bf = alloc_bf(512, "wrt")
        B_wrt = Buf("wrt")
        S_wrt = newsem("wrt")
        dma("pool", wrt_bf, wrt_d[:, :], S_wrt, writes=[B_wrt])
    ident_bf = alloc_bf(128, "identbf")
    B_ident = Buf("ident")
    smalld = alloc(512, "smalld")
    ident_f = smallc[:, C_ID:C_ID + 128]
    ones_f = smallc[:, C_ONE:C_ONE + 128]
    P.op("act", lambda e: e.activation(out=ident_bf, in_=ident_f, func=AF.Copy), [B_smallc], [B_ident])
    SD_SC = 0; SD_SX = 16; SD_CNEG = 32; SD_CARRY = 48; SD_RSTD = 64; SD_SUMH = 128; SD_SUMA = 208; SD_TMP = 288
    B_sc = Buf("sc"); B_cneg = Buf("cneg"); B_carry = Buf("carry"); B_rstd = Buf("rstdab")
    B_sum = [Buf(f"sum{w}") for w in range(5)]
    sc = smalld[:, SD_SC:SD_SC + 16]
    sx = smalld[:, SD_SX:SD_SX + 16]
    cneg = smalld[:, SD_CNEG:SD_CNEG + 16]
    carry = smalld[:, SD_CARRY:SD_CARRY + 16]
    act(sc, smallc[:, C_CT:C_CT + 16], AF.Silu, [B_smallc], [B_sc])
    act(sx, smallc[:, C_CX:C_CX + 16], AF.Silu, [B_smallc], [B_sc])
    act(cneg, smallc[:, C_LAM:C_LAM + 16], AF.Exp, [B_smallc], [B_cneg], scale=-1.0)
    act(cneg, cneg, AF.Ln, [B_cneg], [B_cneg], bias=1.0)
    ts("dve", cneg, cneg, -8.0, None, ALU.mult, None, [B_cneg], [B_cneg])
    persist_top = top[0]

    cb_l = alloc(2048, "cb_l").rearrange("p (k m) -> p k m", k=16)
    cx_l = alloc(2048, "cx_l").rearrange("p (k m) -> p k m", k=16)
    B_cbl = Buf("cbl")
    for k in range(16):
        ts("dve", cb_l[:, k, :], ones_f, sc[:, k:k + 1], None, ALU.mult, None, [B_smallc, B_sc], [B_cbl])
        ts("dve", cx_l[:, k, :], ones_f, sx[:, k:k + 1], None, ALU.mult, None, [B_smallc, B_sc], [B_cbl])
    wm_ring = Ring(3, 2048, "wm")
    ev_ring = Ring(2, 2048, "ev")
    bm_ring = Ring(2, 2048, "bm")
    gbuf = alloc(2048, "gbuf")
    B_gbuf = Buf("gbuf")
    S_g = newsem("gload")
    for g in range(6):
        use_ctx = g < 2
        bm_ap, bm_b, bm_s = bm_ring.next()
        dma("sp", bm_ap[0:1, :], bmod_d[0:1, g * D:(g + 1) * D], bm_s, writes=[bm_b])
        if g in (1, 4):
            dma("sp", gbuf, (gmix_d if g == 1 else gffn_d)[:, :], S_g, writes=[B_gbuf])
        banks = [nbank() for _ in range(4)]
        xbanks = [nbank() for _ in range(4)] if use_ctx else []
        for k in range(16):
            w_ap, w_b, w_s = wm_ring.next()
            dma("sp", w_ap, wmod_d[k * 128:(k + 1) * 128, g * D:(g + 1) * D], w_s, writes=[w_b])
            for n in range(4):
                mmg([(PS[banks[n]][:, :], cb_l[:, k, :], w_ap[:, n * 512:(n + 1) * 512], k == 0, False)],
                    [w_b, B_cbl], [PB[banks[n]]])
                if use_ctx:
                    mmg([(PS[xbanks[n]][:, :], cx_l[:, k, :], w_ap[:, n * 512:(n + 1) * 512], k == 0, False)],
                        [w_b, B_cbl], [PB[xbanks[n]]])
        for n in range(4):
            mmg([(PS[banks[n]][:, :], ones_f[0:1, :], bm_ap[0:1, n * 512:(n + 1) * 512], False, True)],
                [bm_b, B_smallc], [PB[banks[n]]])
            if use_ctx:
                mmg([(PS[xbanks[n]][:, :], ones_f[0:1, :], bm_ap[0:1, n * 512:(n + 1) * 512], False, True)],
                    [bm_b, B_smallc], [PB[xbanks[n]]])
        for (bks, idx) in ((banks, g), (xbanks, 6 + g)):
            if not bks:
                continue
            e_ap, e_b, e_s = ev_ring.next()
            for n in range(4):
                dst = e_ap[:, n * 512:(n + 1) * 512]
                if g in (1, 4):
                    stt("dve", dst, PS[bks[n]][:, :], 1.0, gbuf[:, n * 512:(n + 1) * 512], ALU.add, ALU.mult,
                        [PB[bks[n]], B_gbuf], [e_b])
                else:
                    act(dst, PS[bks[n]][:, :], AF.Copy, [PB[bks[n]]], [e_b])
            dma("sp", modb_d[idx], e_ap, e_s, reads=[e_b], writes=[])
    P.barrier()
    if stop == "p0":
        return finish_early()
    top[0] = persist_top
    xnT = alloc_bf(16 * W, "xnT").rearrange("p (k n) -> p k n", k=16)
    B_xnT = Buf("xnT")
    gs_b = alloc(2048, "gs_b")
    sh_b = alloc(2048, "sh_b")
    B_gs = Buf("gs")
    S_gs = newsem("gs")
    w_ring = Ring(3, 1024, "wring", bf=True)
    acc_a = alloc(2048, "acc_a")
    acc_b = alloc(2048, "acc_b")
    B_acca = Buf("acca"); B_accb = Buf("accb")
    ph1_top = top[0]
    xs_ring = Ring(2, 2048, "xs")
    t1 = alloc(2048, "t1"); B_t1 = Buf("t1")
    xnb_ring = Ring(2, 1024, "xnb", bf=True)
    junk = alloc_bf(2048, "junk"); B_junk = Buf("junk")
    top[0] = ph1_top
    T0 = alloc(W, "T0"); T1 = alloc(L, "T1"); Ta = alloc(L, "Ta"); Tb = alloc(L, "Tb")
    TsBig = alloc(2 * L, "TsBig"); Ts = TsBig[:, 0:L]; Ts2 = TsBig[:, L:2 * L]; Tz = TsBig[:, 0:W]
    xc_bf = alloc_bf(L, "xcbf")
    yb_ring = Ring(2, 1024, "ybf", bf=True)
    B_T0 = Buf("T0"); B_T1 = Buf("T1"); B_Ta = Buf("Ta"); B_Tb = Buf("Tb"); B_Ts = Buf("Ts"); B_Ts2 = Buf("Ts2"); B_xcbf = Buf("xcbf")
    B_ssq = Buf("ssq")
    ssq = smalld[:, SD_TMP:SD_TMP + 32]
    rs = smalld[:, SD_TMP + 32:SD_TMP + 64]
    B_rs = Buf("rs")
    cp_ctr = [0]

    def copy_any(out, in_, reads, writes):
        cp_ctr[0] += 1
        if cp_ctr[0] % 2:
            return act(out, in_, AF.Copy, reads, writes)
        return P.op("dve", lambda e: e.tensor_copy(out=out, in_=in_), reads, writes)

    def rstd_chain(dst, src, scale, rb, wb):
        ts("dve", dst, src, scale, EPS, ALU.mult, ALU.add, rb, wb)
        act(dst, dst, AF.Sqrt, wb, wb)
        P.op("dve", lambda e: e.reciprocal(out=dst, in_=dst), wb, wb)

    def build_xnT(row0, ntile, dstT, B_dst, g_ap, s_ap, B_g):
        for i in range(ntile):
            x_ap, x_b, x_s = xs_ring.next()
            if isinstance(row0, tuple):
                src = row0[0][row0[1] + i * 128: row0[1] + (i + 1) * 128, :]
            else:
                src = x_all[row0 + i * 128: row0 + (i + 1) * 128, :]
            dma("sp", x_ap, src, x_s, writes=[x_b])
            col = i % 32
            act(junk, x_ap, AF.Square, [x_b], [B_junk, B_ssq], accum_out=ssq[:, col:col + 1])
            rstd_chain(rs[:, col:col + 1], ssq[:, col:col + 1], 1.0 / D, [B_ssq], [B_rs])
            stt("dve", t1, x_ap, rs[:, col:col + 1], g_ap, ALU.mult, ALU.mult, [x_b, B_rs, B_g], [B_t1])
            n_ap, n_b, _ = xnb_ring.next()
            tt("pool", n_ap, t1, s_ap, ALU.add, [B_t1, B_g], [n_b])
            for kb in range(4):
                bk = nbank()
                pv = PS[bk][:, :].bitcast(BF16)
                trg([(pv[:, j * 128:(j + 1) * 128], n_ap[:, (kb * 4 + j) * 128:(kb * 4 + j + 1) * 128], ident_bf) for j in range(4)],
                    [n_b, B_ident], [PB[bk]])
                copy_any(dstT[:, kb * 4:kb * 4 + 4, i * 128:(i + 1) * 128],
                         pv[:, 0:512].rearrange("p (k n) -> p k n", k=4), [PB[bk]], [B_dst])

    def load_w(ct):
        w_ap, w_b, w_s = w_ring.next()
        dma("pool", w_ap, win_d[ct], w_s, writes=[w_b])
        return w_ap.rearrange("p (k j) -> p k j", k=16), w_b

    def inproj(w3, w_b, c0, c1, evac):
        for (a, b) in chunks(c0, c1):
            bk = nbank()
            mmg([(PS[bk][:, 0:b - a], w3[:, k, :], xnT[:, k, a:b], k == 0, k == 15) for k in range(16)],
                [w_b, B_xnT], [PB[bk]])
            evac(PS[bk][:, 0:b - a], PB[bk], a, b)

    def colp(base, idx):
        return smallc[:, base + idx: base + idx + 1]

    sumH = smalld[:, SD_SUMH:SD_SUMH + 80]
    sumA = smalld[:, SD_SUMA:SD_SUMA + 80]
    S_y = newsem("ystore")

    for w in range(5):
        Lw = WIN_L[w]
        Ww = Lw + 2 * HALO
        mine = (w == 4)
        if w == 0:
            dma("sp", gs_b, modb_d[7], S_gs, writes=[B_gs])
            dma("sp", sh_b, modb_d[6], S_gs, writes=[B_gs])
        if w == 1:
            dma("sp", gs_b, modb_d[1], S_gs, writes=[B_gs])
            dma("sp", sh_b, modb_d[0], S_gs, writes=[B_gs])
        build_xnT(WIN_ROW[w], Ww // 128, xnT, B_xnT, gs_b, sh_b, B_gs)
        P.barrier()
        if stop == f"x{w}":
            return finish_early()
        if mine:
            tmpc = smalld[:, SD_TMP + 64:SD_TMP + 80]
            B_tc = Buf("tmpc")
            P.op("dve", lambda e: e.tensor_copy(out=carry, in_=sumH[:, 0:16]), [B_sum[0]], [B_carry])
            for (lo, order, mbase) in ((0, (1, 2, 3), 0), (8, (3, 2, 1), 3)):
                for wo in order:
                    cs = carry[:, lo:lo + 8]
                    tcs = tmpc[:, lo:lo + 8]
                    tt("dve", tcs, sumA[:, wo * 16 + lo: wo * 16 + lo + 8], cs, ALU.mult, [B_sum[wo], B_carry], [B_tc])
                    tt("dve", tcs, tcs, sumH[:, wo * 16 + lo: wo * 16 + lo + 8], ALU.add, [B_sum[wo], B_tc], [B_tc])
                    tt("dve", tcs, tcs, cs, ALU.subtract, [B_tc, B_carry], [B_tc])
                    stt("dve", cs, tcs, colp(C_CM, mbase + wo - 1), cs, ALU.mult, ALU.add, [B_tc, B_carry, B_smallc], [B_carry])
            P.op("pool", lambda e: e.memset(acc_a, 0.0), [], [B_acca])
            P.op("pool", lambda e: e.memset(acc_b, 0.0), [], [B_accb])
        hm_l = colp(C_HM, 2 * w)
        hm_r = colp(C_HM, 2 * w + 1)
        for c in range(8):
            w3, w_b = load_w(8 + c)
            inproj(w3, w_b, 0, Ww, lambda ps, pb, a, b: act(T0[:, a:b], ps, AF.Copy, [pb], [B_T0]))
            ts("dve", T0[:, 0:HALO], T0[:, 0:HALO], hm_l, None, ALU.mult, None, [B_T0, B_smallc], [B_T0])
            ts("dve", T0[:, HALO + Lw:Ww], T0[:, HALO + Lw:Ww], hm_r, None, ALU.mult, None, [B_T0, B_smallc], [B_T0])
            xc = T1[:, 0:Lw]
            ts("dve", xc, T0[:, 62:62 + Lw], colp(C_CAW, c * 4 + 0), colp(C_CAB, c), ALU.mult, ALU.add, [B_T0, B_smallc], [B_T1])
            for tap in (1, 2, 3):
                stt("dve", xc, T0[:, 62 + tap:62 + tap + Lw], colp(C_CAW, c * 4 + tap), xc, ALU.mult, ALU.add, [B_T0, B_T1, B_smallc], [B_T1])
            act(xc_bf[:, 0:Lw], xc, AF.Copy, [B_T1], [B_xcbf])
            for d in range(2):
                dc = d * 8 + c
                hbuf, B_h = (Ts, B_Ts) if d == 0 else (Ts2, B_Ts2)
                for (a, b) in chunks(0, Lw):
                    bk = nbank()
                    mmg([(PS[bk][:, 0:b - a], wr_bf[:, dc * 128:(dc + 1) * 128], xc_bf[:, a:b], True, True)], [B_wri, B_xcbf], [PB[bk]])
                    act(Ta[:, a:b], PS[bk][:, 0:b - a], AF.Sigmoid, [PB[bk], B_smallc], [B_Ta], bias=colp(C_LBR, dc))
                    bk = nbank()
                    mmg([(PS[bk][:, 0:b - a], wi_bf[:, dc * 128:(dc + 1) * 128], xc_bf[:, a:b], True, True)], [B_wri, B_xcbf], [PB[bk]])
                    act(Tb[:, a:b], PS[bk][:, 0:b - a], AF.Sigmoid, [PB[bk], B_smallc], [B_Tb], bias=colp(C_LBI, dc))
                av = Ta[:, 0:Lw]; bv = Tb[:, 0:Lw]; hv = hbuf[:, 0:Lw]
                act(av, av, AF.Exp, [B_Ta, B_cneg], [B_Ta], scale=cneg[:, dc:dc + 1])
                act(hv, av, AF.Square, [B_Ta], [B_h])
                act(hv, hv, AF.Sqrt, [B_h], [B_h], scale=-1.0, bias=1.0)
                tt("dve", bv, bv, xc, ALU.mult, [B_Tb, B_T1], [B_Tb])
                tt("dve", bv, bv, hv, ALU.mult, [B_Tb, B_h], [B_Tb])
                init = carry[:, dc:dc + 1] if mine else 0.0
                if d == 0:
                    P.op("dve", lambda e, hv=hv, av=av, bv=bv, init=init: e.tensor_tensor_scan(
                        out=hv, data0=av, data1=bv, initial=init, op0=ALU.mult, op1=ALU.add), [B_Ta, B_Tb, B_carry], [B_h])
                else:
                    P.op("dve", lambda e, hv=hv, av=av, bv=bv, init=init: e.tensor_tensor_scan(
                        out=rev(hv), data0=rev(av), data1=rev(bv), initial=init, op0=ALU.mult, op1=ALU.add), [B_Ta, B_Tb, B_carry], [B_h])
                if not mine:
                    endcol = hv[:, Lw - 1:Lw] if d == 0 else hv[:, 0:1]
                    P.op("dve", lambda e, endcol=endcol, dc=dc, w=w: e.tensor_copy(out=sumH[:, w * 16 + dc:w * 16 + dc + 1], in_=endcol), [B_h], [B_sum[w]])
                    P.op("dve", lambda e, av=av, dc=dc, w=w: e.tensor_reduce(out=sumA[:, w * 16 + dc:w * 16 + dc + 1], in_=av, axis=AX.X, op=ALU.mult), [B_Ta], [B_sum[w]])
            if not mine:
                continue
            tt("dve", Ts, Ts, Ts2, ALU.add, [B_Ts, B_Ts2], [B_Ts])
            w3, w_b = load_w(c)
            Tg = T0[:, 0:L]
            inproj(w3, w_b, HALO, HALO + L, lambda ps, pb, a, b: act(Tg[:, a - HALO:b - HALO], ps, AF.Copy, [pb], [B_T0]))
            Tq = Ta
            tt("dve", Tq, Tg, Tg, ALU.mult, [B_T0], [B_Ta])
            ts("dve", Tq, Tq, 0.044715, 1.0, ALU.mult, ALU.add, [B_Ta], [B_Ta])
            tt("dve", Tq, Tq, Tg, ALU.mult, [B_Ta, B_T0], [B_Ta])
            act(Tq, Tq, AF.Sigmoid, [B_Ta], [B_Ta], scale=1.5957691216057308)
            tt("dve", Tq, Tq, Tg, ALU.mult, [B_Ta, B_T0], [B_Ta])
            tt("dve", Tq, Tq, Ts, ALU.mult, [B_Ta, B_Ts], [B_Ta])
            tt("pool", Tb, Tq, Tq, ALU.mult, [B_Ta], [B_Tb])
            tt("pool", acc_a, acc_a, Tb, ALU.add, [B_Tb, B_acca], [B_acca])
            y_ap, y_b, y_s = yb_ring.next()
            act(y_ap, Tq, AF.Copy, [B_Ta, B_smallc], [y_b], scale=colp(C_GOA, c))
            dma("sp", y_d[c], y_ap, y_s, reads=[y_b])
        if not mine:
            P.barrier()
            if stop == f"w{w}":
                return finish_early()
            continue
        for c in range(8):
            Tc = T0
            w3, w_b = load_w(24 + c)
            inproj(w3, w_b, 0, W, lambda ps, pb, a, b: act(Tc[:, a:b], ps, AF.Copy, [pb], [B_T0]))
            w3, w_b = load_w(32 + c)
            inproj(w3, w_b, 0, W, lambda ps, pb, a, b: tt("dve", Tz[:, a:b], ps, Tc[:, a:b], ALU.mult, [pb, B_T0], [B_Ts, B_Ts2]))
            ts("dve", Tz[:, 0:HALO], Tz[:, 0:HALO], hm_l, None, ALU.mult, None, [B_Ts, B_Ts2, B_smallc], [B_Ts, B_Ts2])
            ts("dve", Tz[:, HALO + L:W], Tz[:, HALO + L:W], hm_r, None, ALU.mult, None, [B_Ts, B_Ts2, B_smallc], [B_Ts, B_Ts2])
            Tcv = T1
            w0 = colp(C_CBW, c * 3 + 0); w1 = colp(C_CBW, c * 3 + 1); w2 = colp(C_CBW, c * 3 + 2)
            ts("dve", Tcv, Tz[:, HALO:HALO + L], w1, None, ALU.mult, None, [B_Ts, B_Ts2, B_smallc], [B_T1])
            if c < 4:
                zv = Tz[:, HALO:HALO + L].rearrange("p (r c) -> p r c", c=64)
                ov = Tcv.rearrange("p (r c) -> p r c", c=64)
                stt("dve", ov[:, :, 1:64], zv[:, :, 0:63], w0, ov[:, :, 1:64], ALU.mult, ALU.add, [B_Ts, B_Ts2, B_T1, B_smallc], [B_T1])
                stt("dve", ov[:, :, 0:63], zv[:, :, 1:64], w2, ov[:, :, 0:63], ALU.mult, ALU.add, [B_Ts, B_Ts2, B_T1, B_smallc], [B_T1])
            else:
                stt("dve", Tcv, Tz[:, 0:L], w0, Tcv, ALU.mult, ALU.add, [B_Ts, B_Ts2, B_T1, B_smallc], [B_T1])
                stt("dve", Tcv, Tz[:, 2 * HALO:2 * HALO + L], w2, Tcv, ALU.mult, ALU.add, [B_Ts, B_Ts2, B_T1, B_smallc], [B_T1])
            w3, w_b = load_w(16 + c)
            Ty = Ta
            inproj(w3, w_b, HALO, HALO + L, lambda ps, pb, a, b: tt("dve", Ty[:, a - HALO:b - HALO], ps, Tcv[:, a - HALO:b - HALO], ALU.mult, [pb, B_T1], [B_Ta]))
            tt("pool", Tb, Ty, Ty, ALU.mult, [B_Ta], [B_Tb])
            tt("pool", acc_b, acc_b, Tb, ALU.add, [B_Tb, B_accb], [B_accb])
            y_ap, y_b, y_s = yb_ring.next()
            act(y_ap, Ty, AF.Copy, [B_Ta, B_smallc], [y_b], scale=colp(C_GOB, c))
            dma("sp", y_d[8 + c], y_ap, y_s, reads=[y_b])
        bk = nbank()
        for g, (acc, B_acc) in enumerate(((acc_a, B_acca), (acc_b, B_accb))):
            for i in range(16):
                j = g * 16 + i
                mmg([(PS[bk][:, 2 * j:2 * j + 2], acc[:, i * 128:(i + 1) * 128], ones_f[:, 0:2], True, True)], [B_acc, B_smallc], [PB[bk]])
        rstd_ab = smalld[:, SD_RSTD:SD_RSTD + 64]
        P.op("dve", lambda e, src=PS[bk][:, 0:64], dst=rstd_ab: e.tensor_copy(out=dst, in_=src), [PB[bk]], [B_rstd])
        rstd_chain(rstd_ab, rstd_ab, 1.0 / 1024, [B_rstd], [B_rstd])
    P.barrier()
    if stop == "p1":
        return finish_early()

    top[0] = persist_top
    wout = alloc_bf(16 * D, "wout").rearrange("p (c n) -> p c n", c=16)
    B_wout = Buf("wout")
    S_wout = newsem("wout")
    for c in range(16):
        dma("pool", wout[:, c, :], wout_d[c * 128:(c + 1) * 128, :], S_wout, writes=[B_wout])
    if stop == "p2a":
        return finish_early()
    ys_ring = Ring(2, 4096, "ysb", bf=True)
    xs2_ring = Ring(2, 2048, "xs2")
    gt1_b = alloc(2048, "gt1")
    B_gt1 = Buf("gt1")
    dma("sp", gt1_b, modb_d[2], S_gs, writes=[B_gt1])
    tA_ring = Ring(2, 512, "tA")
    tB_ring = Ring(2, 512, "tB")
    S_x1 = newsem("x1store")
    rstd_ab = smalld[:, SD_RSTD:SD_RSTD + 64]
    mine_row = WIN_ROW[4] + HALO
    ys3 = None
    for i in range(16):
        if i % 4 == 0:
            y_ap, y_b, y_s = ys_ring.next()
            ys3 = y_ap.rearrange("p (c n) -> p c n", c=16)
            for c in range(16):
                dma("sp", ys3[:, c, :], y_d[c][:, (i // 4) * 512:(i // 4 + 1) * 512], y_s, reads=[], writes=[y_b])
            ys_b = y_b
        x_ap, x_b, x_s = xs2_ring.next()
        dma("sp", x_ap, x_all[mine_row + i * 128: mine_row + (i + 1) * 128, :], x_s, writes=[x_b])
        to = (i % 4) * 128
        for n in range(4):
            bA = nbank(); bB = nbank()
            mmg([(PS[bA][:, :], ys3[:, c, to:to + 128], wout[:, c, n * 512:(n + 1) * 512], c == 0, c == 7) for c in range(8)], [ys_b, B_wout], [PB[bA]])
            mmg([(PS[bB][:, :], ys3[:, c, to:to + 128], wout[:, c, n * 512:(n + 1) * 512], c == 8, c == 15) for c in range(8, 16)], [ys_b, B_wout], [PB[bB]])
            ta, ta_b, _ = tA_ring.next()
            tb, tb_b, _ = tB_ring.next()
            act(ta, PS[bA][:, :], AF.Copy, [PB[bA], B_rstd], [ta_b], scale=rstd_ab[:, 2 * i:2 * i + 1])
            stt("dve", tb, PS[bB][:, :], rstd_ab[:, 32 + 2 * i:32 + 2 * i + 1], ta, ALU.mult, ALU.add, [PB[bB], B_rstd, ta_b], [tb_b])
            tt("pool", tb, tb, gt1_b[:, n * 512:(n + 1) * 512], ALU.mult, [tb_b, B_gt1], [tb_b])
            tt("pool", x_ap[:, n * 512:(n + 1) * 512], x_ap[:, n * 512:(n + 1) * 512], tb, ALU.add, [tb_b, x_b], [x_b])
        if stop == "p2b":
            return finish_early()
        dst = out_d if stage < 3 else x1_d
        dma("sp", dst[i * 128:(i + 1) * 128, :], x_ap, x_s, reads=[x_b])
    P.barrier()
    out_sems = [s for (_, _, s) in xs2_ring.slots]


    branched = (stage >= 3 and sparse)
    if branched:
        top[0] = persist_top
        xn2_d = nc.dram_tensor("xn2_scr", [16, 128, D], BF16).ap()
        flag_d = nc.dram_tensor("flag_scr", [1, 1], mybir.dt.int32).ap()
        wreg = wr_bf.bitcast(F32)
        wreg2 = wi_bf.bitcast(F32)
        G_all = wreg[:, 0:512].rearrange("p (i e) -> p i e", i=16)
        posm_all = wreg[:, 512:1024].rearrange("p (i e) -> p i e", i=16)
        posmT = wreg2
        B_Gall = Buf("Gall"); B_posm = Buf("posm"); B_posmT = Buf("posmT")
        run_c = smalld[:, SD_TMP + 96:SD_TMP + 128]
        maxc = smalld[:, SD_TMP + 128:SD_TMP + 160]
        flagf = smalld[:, SD_TMP + 160:SD_TMP + 162]
        flagi = smalld[:, SD_TMP + 162:SD_TMP + 163].bitcast(mybir.dt.int32)
        B_run = Buf("run"); B_maxc = Buf("maxc"); B_flag = Buf("flag")
        P.op("dve", lambda e: e.memset(run_c, 0.0), [], [B_run])
        P.op("dve", lambda e: e.memset(maxc, 0.0), [], [B_maxc])
        tri_bf = alloc_bf(128, "tribf"); one_bf = alloc_bf(128, "onebf")
        B_tri = Buf("tri")
        P.op("act", lambda e: e.activation(out=tri_bf, in_=smallc[:, C_TRI:C_TRI + 128], func=AF.Copy), [B_smallc], [B_tri])
        P.op("act", lambda e: e.activation(out=one_bf, in_=ones_f, func=AF.Copy), [B_smallc], [B_tri])
        xt_ring = Ring(2, 2048, "xtR")
        t1r = alloc(2048, "t1r"); B_t1r = Buf("t1r")
        xnr_ring = Ring(2, 1024, "xnr", bf=True)
        junkr = alloc_bf(2048, "junkr"); B_junkr = Buf("junkr")
        xtT_ring = Ring(2, 1024, "xtT", bf=True)
        modR0 = alloc(2048, "modR0"); modR1 = alloc(2048, "modR1")
        B_modR = Buf("modR")
        S_modR = newsem("modR")
        dma("sp", modR0, modb_d[4], S_modR, writes=[B_modR])
        dma("sp", modR1, modb_d[3], S_modR, writes=[B_modR])
        lgR = alloc(32, "lgR"); exR = alloc(32, "exR"); mkR = alloc(32, "mkR"); m8R = alloc(8, "m8R"); smR = alloc(4, "smR")
        psR = alloc(32, "posR"); mkbR = alloc_bf(32, "mkbR")
        B_lgR = Buf("lgR"); B_exR = Buf("exR"); B_mkR = Buf("mkR"); B_m8R = Buf("m8R"); B_smR = Buf("smR"); B_psR = Buf("psR"); B_mkbR = Buf("mkbR")
        brtR = smallc[:, C_BRT:C_BRT + 32]
        wrt3R = wrt_bf.rearrange("p (k e) -> p k e", k=16)
        for i in range(16):
            x_ap, x_b, x_s = xt_ring.next()
            dma("sp", x_ap, x1_d[i * 128:(i + 1) * 128, :], x_s, writes=[x_b])
            col = 16 + (i % 8)
            act(junkr, x_ap, AF.Square, [x_b], [B_junkr, B_ssq], accum_out=ssq[:, col:col + 1])
            rstd_chain(rs[:, col:col + 1], ssq[:, col:col + 1], 1.0 / D, [B_ssq], [B_rs])
            stt("dve", t1r, x_ap, rs[:, col:col + 1], modR0, ALU.mult, ALU.mult, [x_b, B_rs, B_modR], [B_t1r])
            n_ap, n_b, n_s = xnr_ring.next()
            tt("pool", n_ap, t1r, modR1, ALU.add, [B_t1r, B_modR], [n_b])
            dma("sp", xn2_d[i], n_ap, n_s, reads=[n_b])
            xT_ap, xT_b, _ = xtT_ring.next()
            xT3 = xT_ap.rearrange("p (k n) -> p k n", k=16)
            for kb in range(4):
                bk = nbank()
                pv = PS[bk][:, :].bitcast(BF16)
                trg([(pv[:, j * 128:(j + 1) * 128], n_ap[:, (kb * 4 + j) * 128:(kb * 4 + j + 1) * 128], ident_bf) for j in range(4)],
                    [n_b, B_ident], [PB[bk]])
                copy_any(xT3[:, kb * 4:kb * 4 + 4, :], pv[:, 0:512].rearrange("p (k n) -> p k n", k=4), [PB[bk]], [xT_b])
            bk = nbank()
            mmg([(PS[bk][:, 0:32], xT3[:, k, :], wrt3R[:, k, :], k == 0, k == 15) for k in range(16)], [xT_b, B_wrt], [PB[bk]])
            tt("dve", lgR, PS[bk][:, 0:32], brtR, ALU.add, [PB[bk], B_smallc], [B_lgR])
            P.op("dve", lambda e: e.max(out=m8R, in_=lgR), [B_lgR], [B_m8R])
            ts("dve", smR[:, 0:1], m8R[:, 0:1], -1.0, None, ALU.mult, None, [B_m8R], [B_smR])
            act(exR, lgR, AF.Exp, [B_lgR, B_smR], [B_exR], bias=smR[:, 0:1])
            ts("dve", mkR, lgR, m8R[:, 3:4], None, ALU.is_ge, None, [B_lgR, B_m8R], [B_mkR])
            tt("dve", exR, exR, mkR, ALU.mult, [B_exR, B_mkR], [B_exR])
            P.op("dve", lambda e: e.tensor_reduce(out=smR[:, 1:2], in_=exR, axis=AX.X, op=ALU.add), [B_exR], [B_smR])
            P.op("dve", lambda e: e.reciprocal(out=smR[:, 2:3], in_=smR[:, 1:2]), [B_smR], [B_smR])
            ts("dve", G_all[:, i, :], exR, smR[:, 2:3], None, ALU.mult, None, [B_exR, B_smR], [B_Gall])
            P.op("dve", lambda e: e.tensor_copy(out=mkbR, in_=mkR), [B_mkR], [B_mkbR])
            bA = nbank(); bB = nbank()
            mmg([(PS[bA][:, 0:32], tri_bf, mkbR, True, True)], [B_tri, B_mkbR], [PB[bA]])
            mmg([(PS[bB][:, 0:32], one_bf, mkbR, True, True)], [B_tri, B_mkbR], [PB[bB]])
            tt("dve", psR, PS[bA][:, 0:32], run_c, ALU.add, [PB[bA], B_run], [B_psR])
            tt("dve", psR, psR, mkR, ALU.mult, [B_psR, B_mkR], [B_psR])
            stt("dve", posm_all[:, i, :], psR, -1.0, mkR, ALU.add, ALU.add, [B_psR, B_mkR], [B_posm])
            tt("dve", run_c, PS[bB][:, 0:32], run_c, ALU.add, [PB[bB], B_run], [B_run])
            if i % 8 == 7:
                tt("dve", maxc, maxc, run_c, ALU.max, [B_maxc, B_run], [B_maxc])
                P.op("dve", lambda e: e.memset(run_c, 0.0), [], [B_run])
        P.op("dve", lambda e: e.tensor_reduce(out=flagf[:, 0:1], in_=maxc, axis=AX.X, op=ALU.max), [B_maxc], [B_flag])
        ts("dve", flagf[:, 1:2], flagf[:, 0:1], float(CAP), None, ALU.is_gt, None, [B_flag], [B_flag])
        P.op("dve", lambda e: e.tensor_copy(out=flagi, in_=flagf[:, 1:2]), [B_flag], [B_flag])
        S_flag = newsem("flag")
        B_flagd = Buf("flagd")
        dma("sp", flag_d[0:1, 0:1], flagi[0:1, 0:1], S_flag, reads=[B_flag], writes=[B_flagd])
        P.barrier()
        for en in ENGS:
            P.op(en, lambda e, en=en: e.reg_load(P.regs[en], flag_d[0:1, 0:1]), [B_flagd], [])
        snap = P.snapshot([bank_ctr, top, cp_ctr])
        P.region = "A"
        top[0] = persist_top
        accA = [alloc(2048, f"accA{i}") for i in range(8)]
        B_accA = [Buf(f"accA{i}") for i in range(8)]
        S_accA = [newsem(f"accA{i}") for i in range(8)]
        xn2_tok = alloc_bf(8 * D, "xn2tok").rearrange("p (j d) -> p j d", j=8)
        B_xtok = Buf("xtok"); S_xtok = newsem("xtok")
        selA = alloc_bf(8 * CAP, "selA").rearrange("p (j s) -> p j s", j=8)
        B_sel = Buf("sel")
        GTa = selA.bitcast(F32) if False else None
        XeT = alloc_bf(16 * CAP, "XeT").rearrange("p (k s) -> p k s", k=16)
        B_XeT = Buf("XeT")
        actA = alloc_bf(16 * CAP, "actA").rearrange("p (f s) -> p f s", f=16)
        B_actA = Buf("actA")
        Ye_raw = alloc(1024 * NST, "Ye")
        Ye = Ye_raw.bitcast(BF16).rearrange("p (i d) -> p i d", i=NST)
        B_Ye = Buf("Ye")
        selT_raw = alloc(512 * NST, "selT")
        selT = selT_raw.bitcast(BF16).rearrange("p (i t) -> p i t", i=NST)
        B_selT = Buf("selT")
        wdA_ring = Ring(2, 1024, "wdA", bf=True)
        wguA_ring = Ring(2, 2048, "wguA", bf=True)
        modA0 = alloc(2048, "modA0"); B_modA0 = Buf("modA0"); S_modA0 = newsem("modA0")
        S_modA1 = newsem("modA1")
        Ee_ring = Ring(1, 128, "Ee")
        tgA = Ring(1, CAP, "tgA"); tsgA = Ring(1, CAP, "tsgA"); tuA = Ring(2, CAP, "tuA"); tdA = Ring(2, 512, "tdA")
        bgc = smallc[:, C_BG:C_BG + 512]
        buc = smallc[:, C_BU:C_BU + 512]
        iota_f = smallc[:, C_IOTA:C_IOTA + CAP]
        pidx = smallc[:, C_PIDX:C_PIDX + 128]
        print("branch A arena top", top[0], "of", ARENA)
        for h in range(2):
            dma("sp", modA0, modb_d[5], S_modA0, writes=[B_modA0])
            for j in range(8):
                i = h * 8 + j
                dma("sp", accA[j], x1_d[i * 128:(i + 1) * 128, :], S_accA[j], writes=[B_accA[j]])
                dma("sp", xn2_tok[:, j, :], xn2_d[i], S_xtok, writes=[B_xtok])
            GTh = Ye_raw[:, 0:1024]
            bdA = XeT.bitcast(F32) if False else None
            bd_v = arena[:, 0:0] if False else None
            for j in range(8):
                i = h * 8 + j
                bk = nbank()
                trg([(PS[bk][0:32, 0:128], G_all[:, i, :], ident_f)], [B_Gall, B_smallc], [PB[bk]])
                act(GTh[0:32, j * 128:(j + 1) * 128], PS[bk][0:32, 0:128], AF.Copy, [PB[bk]], [B_Ye])
                bk = nbank()
                trg([(PS[bk][0:32, 0:128], posm_all[:, i, :], ident_f)], [B_posm, B_smallc], [PB[bk]])
                act(posmT[0:32, j * 128:(j + 1) * 128], PS[bk][0:32, 0:128], AF.Copy, [PB[bk]], [B_posmT])
            bd_raw = selT_raw
            for mp in range(2):
                dma("sp", bd_raw[0:32, 0:1024], bd_d[:, mp * 1024:(mp + 1) * 1024], S_modA1, writes=[B_selT])
                for j in range(8):
                    for mm_ in range(2):
                        m = mp * 2 + mm_
                        bk = nbank()
                        mmg([(PS[bk][:, :], GTh[0:32, j * 128:(j + 1) * 128], bd_raw[0:32, mm_ * 512:(mm_ + 1) * 512], True, True)], [B_Ye, B_selT], [PB[bk]])
                        td, td_b, _ = tdA.next()
                        tt("dve", td, PS[bk][:, :], modA0[:, m * 512:(m + 1) * 512], ALU.mult, [PB[bk], B_modA0], [td_b])
                        tt("pool", accA[j][:, m * 512:(m + 1) * 512], accA[j][:, m * 512:(m + 1) * 512], td, ALU.add, [td_b, B_accA[j]], [B_accA[j]])
            P.barrier()
            units = [(e_, f_) for e_ in range(NEXP) for f_ in range(16)]
            uslot = {}
            dslot = {}
            PF = 1

            def issue_wguA(u):
                e_, f_ = units[u]
                u_ap, u_b, u_s = wguA_ring.next()
                dma("pool", u_ap, wgu_d[e_ * 16 + f_], u_s, writes=[u_b])
                uslot[u] = (u_ap, u_b)

            def issue_wdA(e_, m8):
                d_ap, d_b, d_s = wdA_ring.next()
                d3_ = d_ap.rearrange("p (f n) -> p f n", f=16)
                dma("pool", d3_, wd_d[e_].rearrange("(f p) d -> p f d", p=128)[:, :, m8 * 128:(m8 + 1) * 128], d_s, writes=[d_b])
                dslot[(e_, m8)] = (d3_, d_b)

            for u in range(PF):
                issue_wguA(u)
            for u, (ex_i, f) in enumerate(units):
                if u + PF < len(units):
                    issue_wguA(u + PF)
                if f == 0:
                    for j in range(8):
                        ts("dve", selA[:, j, :], iota_f, posm_all[:, h * 8 + j, ex_i:ex_i + 1], None, ALU.is_equal, None, [B_smallc, B_posm], [B_sel])
                    for k in range(16):
                        bk = nbank()
                        mmg([(PS[bk][:, 0:CAP], xn2_tok[:, j, k * 128:(k + 1) * 128], selA[:, j, :], j == 0, j == 7) for j in range(8)],
                            [B_xtok, B_sel], [PB[bk]])
                        copy_any(XeT[:, k, :], PS[bk][:, 0:CAP], [PB[bk]], [B_XeT])
                    e_ap, e_b, _ = Ee_ring.next()
                    ts("dve", e_ap[0:32, :], pidx[0:32, :], float(ex_i), None, ALU.is_equal, None, [B_smallc], [e_b])
                    for c2 in range(2):
                        bk = nbank()
                        mmg([(PS[bk][:, :], e_ap[0:32, :], posmT[0:32, c2 * 512:(c2 + 1) * 512], True, True)], [e_b, B_posmT], [PB[bk]])
                        for i2 in range(NST):
                            ts("dve", selT[:, i2, c2 * 512:(c2 + 1) * 512], PS[bk][:, :], smallc[:, C_SIDX + i2:C_SIDX + i2 + 1], None, ALU.is_equal, None,
                               [PB[bk], B_smallc], [B_selT])
                if f in (4, 10):
                    issue_wdA(ex_i, (f - 4) // 6)
                u_ap, u_b = uslot.pop(u)
                u4 = u_ap.rearrange("p (t k j) -> p t k j", t=2, k=16)
                bG = nbank(); bU = nbank()
                mmg([(PS[bG][:, 0:CAP], u4[:, 0, k, :], XeT[:, k, :], k == 0, k == 15) for k in range(16)], [u_b, B_XeT], [PB[bG]])
                mmg([(PS[bU][:, 0:CAP], u4[:, 1, k, :], XeT[:, k, :], k == 0, k == 15) for k in range(16)], [u_b, B_XeT], [PB[bU]])
                tg, tg_b, _ = tgA.next(); tsg, tsg_b, _ = tsgA.next(); tu, tu_b, _ = tuA.next()
                cb = ex_i * 16 + f
                ts("dve", tg, PS[bG][:, 0:CAP], bgc[:, cb:cb + 1], 7.0, ALU.add, ALU.min, [PB[bG], B_smallc], [tg_b])
                act(tsg, tg, AF.Sigmoid, [tg_b], [tsg_b], scale=1.702)
                act(tu, PS[bU][:, 0:CAP], AF.Identity, [PB[bU], B_smallc], [tu_b], bias=buc[:, cb:cb + 1])
                ts("dve", tu, tu, 7.0, -7.0, ALU.min, ALU.max, [tu_b], [tu_b])
                stt("dve", tu, tu, 1.0, tg, ALU.add, ALU.mult, [tu_b, tg_b], [tu_b])
                tt("pool", actA[:, f, :], tu, tsg, ALU.mult, [tu_b, tsg_b], [B_actA])
                if f != 15:
                    continue
                for m8 in range(16):
                    d3, d_b = dslot.pop((ex_i, m8))
                    for i2 in range(NST):
                        bk = nbank()
                        mmg([(PS[bk][:, 0:128], actA[:, ff, i2 * 128:(i2 + 1) * 128], d3[:, ff, :], ff == 0, ff == 15) for ff in range(16)], [B_actA, d_b], [PB[bk]])
                        copy_any(Ye[:, i2, m8 * 128:(m8 + 1) * 128], PS[bk][:, 0:128], [PB[bk]], [B_Ye])
                    if m8 + 2 < 16:
                        issue_wdA(ex_i, m8 + 2)
                for jt in range(8):
                    for m in range(4):
                        bk = nbank()
                        mmg([(PS[bk][:, :], selT[:, i2, jt * 128:(jt + 1) * 128], Ye[:, i2, m * 512:(m + 1) * 512], i2 == 0, i2 == NST - 1) for i2 in range(NST)],
                            [B_selT, B_Ye], [PB[bk]])
                        td, td_b, _ = tdA.next()
                        stt("dve", td, PS[bk][:, :], G_all[:, h * 8 + jt, ex_i:ex_i + 1], modA0[:, m * 512:(m + 1) * 512], ALU.mult, ALU.mult, [PB[bk], B_Gall, B_modA0], [td_b])
                        tt("pool", accA[jt][:, m * 512:(m + 1) * 512], accA[jt][:, m * 512:(m + 1) * 512], td, ALU.add, [td_b, B_accA[jt]], [B_accA[jt]])
            P.barrier()
            gfinA = Ye_raw[:, 0:2048]
            dma("sp", gfinA, gfin_d[:, :], S_modA1, writes=[B_Ye])
            junkA = XeT.rearrange("p k s -> p (k s)")[:, 0:2048]
            for j in range(8):
                r0 = (h * 8 + j) * 128
                act(junkA, accA[j], AF.Square, [B_accA[j]], [B_XeT, B_ssq], accum_out=ssq[:, 8 + j:9 + j])
                rstd_chain(rs[:, 8 + j:9 + j], ssq[:, 8 + j:9 + j], 1.0 / D, [B_ssq], [B_rs])
                stt("dve", accA[j], accA[j], rs[:, 8 + j:9 + j], gfinA, ALU.mult, ALU.mult, [B_accA[j], B_rs, B_Ye], [B_accA[j]])
                dma("sp", out_d[r0:r0 + 128, :], accA[j], S_accA[j], reads=[B_accA[j]])
            P.barrier()
        P.final_wait("sp", S_accA)
        P.restore(snap)
        P.region = "B"

    if stage >= 3:
        top[0] = persist_top
        acc = [alloc(2048, f"acc{i}") for i in range(4)]
        B_accs = [Buf(f"acc{i}") for i in range(4)]
        S_acc = [newsem(f"acc{i}") for i in range(4)]
        xn2T = alloc_bf(16 * 512, "xn2T").rearrange("p (k n) -> p k n", k=16)
        B_xn2T = Buf("xn2T")
        actb = alloc_bf(16 * 512, "actb").rearrange("p (f n) -> p f n", f=16)
        B_actb = Buf("actb")
        wd_ring = Ring(3, 4096, "wd", bf=True)
        wgu_ring = Ring(4, 2048, "wgu", bf=True)
        mod0 = alloc(2048, "mod0"); mod1 = alloc(2048, "mod1")
        B_mod0 = Buf("mod0"); B_mod1 = Buf("mod1")
        S_mod0 = newsem("mod0"); S_mod1 = newsem("mod1"); S_bd = newsem("bd")
        bd_sb = alloc(2048, "bd")
        B_bd = Buf("bd")
        dma("sp", bd_sb[0:32, :], bd_d[:, :], S_bd, writes=[B_bd])
        G = alloc(128, "G").rearrange("p (i e) -> p i e", i=4)
        B_G = Buf("G")
        GT = alloc(512, "GT")
        B_GT = Buf("GT")
        lg = alloc(32, "lg"); ex = alloc(32, "ex"); mk = alloc(32, "mk"); m8 = alloc(8, "m8"); sm = alloc(4, "sm")
        B_lg = Buf("lg"); B_ex = Buf("ex"); B_mk = Buf("mk"); B_m8 = Buf("m8"); B_sm = Buf("sm")
        tmp_top = top[0]
        t1m = alloc(2048, "t1m"); B_t1m = Buf("t1m")
        xnbm = alloc_bf(2048, "xnbm"); B_xnbm = Buf("xnbm")
        junkm = alloc_bf(2048, "junkm"); B_junkm = Buf("junkm")
        top[0] = tmp_top
        tg_ring = Ring(2, 512, "tg"); tsg_ring = Ring(2, 512, "tsg"); tu_ring = Ring(2, 512, "tu"); td_ring = Ring(2, 512, "td")
        bgc = smallc[:, C_BG:C_BG + 512]
        buc = smallc[:, C_BU:C_BU + 512]
        brt = smallc[:, C_BRT:C_BRT + 32]
        out_sems = S_acc
        for q in range(nq):
            dma("sp", mod0, modb_d[4], S_mod0, writes=[B_mod0])
            dma("sp", mod1, modb_d[3], S_mod1, writes=[B_mod1])
            for i in range(4):
                r0 = (q * 4 + i) * 128
                dma("sp", acc[i], x1_d[r0:r0 + 128, :], S_acc[i], writes=[B_accs[i]])
                act(junkm, acc[i], AF.Square, [B_accs[i]], [B_junkm, B_ssq], accum_out=ssq[:, i:i + 1])
                rstd_chain(rs[:, i:i + 1], ssq[:, i:i + 1], 1.0 / D, [B_ssq], [B_rs])
                stt("dve", t1m, acc[i], rs[:, i:i + 1], mod0, ALU.mult, ALU.mult, [B_accs[i], B_rs, B_mod0], [B_t1m])
                tt("pool", xnbm, t1m, mod1, ALU.add, [B_t1m, B_mod1], [B_xnbm])
                for kb in range(4):
                    bk = nbank()
                    pv = PS[bk][:, :].bitcast(BF16)
                    trg([(pv[:, j * 128:(j + 1) * 128], xnbm[:, (kb * 4 + j) * 128:(kb * 4 + j + 1) * 128], ident_bf) for j in range(4)],
                        [B_xnbm, B_ident], [PB[bk]])
                    copy_any(xn2T[:, kb * 4:kb * 4 + 4, i * 128:(i + 1) * 128],
                             pv[:, 0:512].rearrange("p (k n) -> p k n", k=4), [PB[bk]], [B_xn2T])
                bk = nbank()
                wrt3 = wrt_bf.rearrange("p (k e) -> p k e", k=16)
                mmg([(PS[bk][:, 0:32], xn2T[:, k, i * 128:(i + 1) * 128], wrt3[:, k, :], k == 0, k == 15) for k in range(16)],
                    [B_xn2T, B_wrt], [PB[bk]])
                tt("dve", lg, PS[bk][:, 0:32], brt, ALU.add, [PB[bk], B_smallc], [B_lg])
                P.op("dve", lambda e: e.max(out=m8, in_=lg), [B_lg], [B_m8])
                ts("dve", sm[:, 0:1], m8[:, 0:1], -1.0, None, ALU.mult, None, [B_m8], [B_sm])
                act(ex, lg, AF.Exp, [B_lg, B_sm], [B_ex], bias=sm[:, 0:1])
                ts("dve", mk, lg, m8[:, 3:4], None, ALU.is_ge, None, [B_lg, B_m8], [B_mk])
                tt("dve", ex, ex, mk, ALU.mult, [B_ex, B_mk], [B_ex])
                P.op("dve", lambda e: e.tensor_reduce(out=sm[:, 1:2], in_=ex, axis=AX.X, op=ALU.add), [B_ex], [B_sm])
                P.op("dve", lambda e: e.reciprocal(out=sm[:, 2:3], in_=sm[:, 1:2]), [B_sm], [B_sm])
                ts("dve", G[:, i, :], ex, sm[:, 2:3], None, ALU.mult, None, [B_ex, B_sm], [B_G])
                bk = nbank()
                trg([(PS[bk][0:32, 0:128], G[:, i, :], ident_f)], [B_G, B_smallc], [PB[bk]])
                act(GT[0:32, i * 128:(i + 1) * 128], PS[bk][0:32, 0:128], AF.Copy, [PB[bk]], [B_GT])
            P.barrier()
            dma("sp", mod0, modb_d[5], S_mod0, writes=[B_mod0])
            dma("sp", mod1, gfin_d[:, :], S_mod1, writes=[B_mod1])
            for i in range(4):
                for m in range(4):
                    bk = nbank()
                    mmg([(PS[bk][:, :], GT[0:32, i * 128:(i + 1) * 128], bd_sb[0:32, m * 512:(m + 1) * 512], True, True)], [B_GT, B_bd], [PB[bk]])
                    td, td_b, _ = td_ring.next()
                    tt("dve", td, PS[bk][:, :], mod0[:, m * 512:(m + 1) * 512], ALU.mult, [PB[bk], B_mod0], [td_b])
                    tt("pool", acc[i][:, m * 512:(m + 1) * 512], acc[i][:, m * 512:(m + 1) * 512], td, ALU.add, [td_b, B_accs[i]], [B_accs[i]])
            units = [(e_, f_) for e_ in range(NEXP) for f_ in range(16)]
            uslot = {}
            dslot = {}
            PF = 3

            def issue_wgu(u):
                e_, f_ = units[u]
                u_ap, u_b, u_s = wgu_ring.next()
                dma("pool", u_ap, wgu_d[e_ * 16 + f_], u_s, writes=[u_b])
                uslot[u] = (u_ap, u_b)

            def issue_wd(e_, m_):
                d_ap, d_b, d_s = wd_ring.next()
                d3_ = d_ap.rearrange("p (f n) -> p f n", f=16)
                dma("pool", d3_, wd_d[e_].rearrange("(f p) d -> p f d", p=128)[:, :, m_ * 512:(m_ + 1) * 512], d_s, writes=[d_b])
                dslot[(e_, m_)] = (d3_, d_b)

            for u in range(PF):
                issue_wgu(u)
            for u, (ex_i, f) in enumerate(units):
                if u + PF < len(units):
                    issue_wgu(u + PF)
                if f in (3, 7, 11):
                    issue_wd(ex_i, (f - 3) // 4)
                u_ap, u_b = uslot.pop(u)
                u4 = u_ap.rearrange("p (t k j) -> p t k j", t=2, k=16)
                bG = nbank(); bU = nbank()
                mmg([(PS[bG][:, :], u4[:, 0, k, :], xn2T[:, k, :], k == 0, k == 15) for k in range(16)], [u_b, B_xn2T], [PB[bG]])
                mmg([(PS[bU][:, :], u4[:, 1, k, :], xn2T[:, k, :], k == 0, k == 15) for k in range(16)], [u_b, B_xn2T], [PB[bU]])
                tg, tg_b, _ = tg_ring.next(); tsg, tsg_b, _ = tsg_ring.next(); tu, tu_b, _ = tu_ring.next()
                cb = ex_i * 16 + f
                ts("dve", tg, PS[bG][:, :], bgc[:, cb:cb + 1], 7.0, ALU.add, ALU.min, [PB[bG], B_smallc], [tg_b])
                act(tsg, tg, AF.Sigmoid, [tg_b], [tsg_b], scale=1.702)
                act(tu, PS[bU][:, :], AF.Identity, [PB[bU], B_smallc], [tu_b], bias=buc[:, cb:cb + 1])
                ts("dve", tu, tu, 7.0, -7.0, ALU.min, ALU.max, [tu_b], [tu_b])
                stt("dve", tu, tu, 1.0, tg, ALU.add, ALU.mult, [tu_b, tg_b], [tu_b])
                tt("pool", actb[:, f, :], tu, tsg, ALU.mult, [tu_b, tsg_b], [B_actb])
                if f != 15:
                    continue
                for m in range(4):
                    d3, d_b = dslot.pop((ex_i, m))
                    for i in range(4):
                        bk = nbank()
                        mmg([(PS[bk][:, :], actb[:, ff, i * 128:(i + 1) * 128], d3[:, ff, :], ff == 0, ff == 15) for ff in range(16)], [B_actb, d_b], [PB[bk]])
                        td, td_b, _ = td_ring.next()
                        stt("dve", td, PS[bk][:, :], G[:, i, ex_i:ex_i + 1], mod0[:, m * 512:(m + 1) * 512], ALU.mult, ALU.mult, [PB[bk], B_G, B_mod0], [td_b])
                        tt("pool", acc[i][:, m * 512:(m + 1) * 512], acc[i][:, m * 512:(m + 1) * 512], td, ALU.add, [td_b, B_accs[i]], [B_accs[i]])
                    if m == 0:
                        issue_wd(ex_i, 3)
            P.barrier()
            for i in range(4):
                r0 = (q * 4 + i) * 128
                act(junkm, acc[i], AF.Square, [B_accs[i]], [B_junkm, B_ssq], accum_out=ssq[:, 8 + i:9 + i])
                rstd_chain(rs[:, 8 + i:9 + i], ssq[:, 8 + i:9 + i], 1.0 / D, [B_ssq], [B_rs])
                stt("dve", acc[i], acc[i], rs[:, 8 + i:9 + i], mod1, ALU.mult, ALU.mult, [B_accs[i], B_rs, B_mod1], [B_accs[i]])
                dma("sp", out_d[r0:r0 + 128, :], acc[i], S_acc[i], reads=[B_accs[i]])
            P.barrier()
    P.final_wait("sp", out_sems)
    with nc.Block() as block:
        P.emit(block, branched=branched)
    es.close()
    return nc


_NC_CACHE = {}


def _prep_shared(inp, stage):
    f = np.float32
    sh = {}
    sh["w_mod"] = np.ascontiguousarray(inp["w_mod"][0], dtype=f)
    sh["b_mod"] = np.ascontiguousarray(inp["b_mod"][0].reshape(1, -1), dtype=f)
    sh["g_mix_b"] = np.ascontiguousarray(np.broadcast_to(inp["g_mix"][0], (128, D)), dtype=f)
    sh["g_ffn_b"] = np.ascontiguousarray(np.broadcast_to(inp["g_ffn"][0], (128, D)), dtype=f)
    sh["g_fin_b"] = np.ascontiguousarray(np.broadcast_to(inp["g_final"], (128, D)), dtype=f)
    w_in = np.asarray(inp["w_in"][0], dtype=f)
    sh["w_in_h"] = np.ascontiguousarray(w_in.reshape(16, 128, 40, 128).transpose(2, 1, 0, 3)).reshape(40, 128, 2048)
    sh["wr_h"] = np.ascontiguousarray(np.asarray(inp["lru_w_r"][0], dtype=f).transpose(2, 0, 1, 3)).reshape(128, 2048)
    sh["wi_h"] = np.ascontiguousarray(np.asarray(inp["lru_w_i"][0], dtype=f).transpose(2, 0, 1, 3)).reshape(128, 2048)
    sh["w_out"] = np.ascontiguousarray(inp["w_out"][0], dtype=f)
    if stage >= 3:
        sh["w_router_h"] = np.ascontiguousarray(np.asarray(inp["w_router"][0], dtype=f).reshape(16, 128, 32).transpose(1, 0, 2)).reshape(128, 512)
        wg = np.asarray(inp["w_gate"][0], dtype=f).reshape(NEXP, 16, 128, 16, 128)
        wu = np.asarray(inp["w_up"][0], dtype=f).reshape(NEXP, 16, 128, 16, 128)
        wgu = np.empty((NEXP, 16, 128, 2, 16, 128), dtype=f)
        wgu[:, :, :, 0] = wg.transpose(0, 3, 2, 1, 4)
        wgu[:, :, :, 1] = wu.transpose(0, 3, 2, 1, 4)
        sh["wgu_h"] = wgu.reshape(NEXP * 16, 128, 4096)
        sh["w_down"] = np.ascontiguousarray(inp["w_down"][0], dtype=f)
        sh["b_down"] = np.ascontiguousarray(inp["b_down"][0], dtype=f)
    return sh


def _prep_core(inp, k):
    f = np.float32
    b, j = k // 4, k % 4
    x = np.asarray(inp["x"], dtype=f)
    ctx = np.asarray(inp["ctx"], dtype=f)
    S = x.shape[1]
    x_all = np.zeros((XROWS, D), dtype=f)
    x_all[HALO:HALO + LC] = ctx[b]
    others = [jj for jj in range(4) if jj != j]
    chunks_ = others + [j]
    hm = np.zeros(10, dtype=f)
    for wi, jj in enumerate(chunks_):
        w = wi + 1
        t0 = jj * L - HALO
        lo, hi = max(t0, 0), min(t0 + W, S)
        r0 = WIN_ROW[w] + (lo - t0)
        x_all[r0:r0 + (hi - lo)] = x[b, lo:hi]
        hm[2 * w] = 1.0 if jj > 0 else 0.0
        hm[2 * w + 1] = 1.0 if jj < 3 else 0.0
    cm = np.zeros(6, dtype=f)
    for o, jj in enumerate(others):
        cm[o] = 1.0 if jj < j else 0.0
        cm[3 + o] = 1.0 - cm[o]
    sc = np.zeros((128, NS), dtype=f)

    def colT(v, n):
        return np.asarray(v, dtype=f).reshape(n, 128).T

    sc[:, C_CT:C_CT + 16] = colT(inp["c"][b], 16)
    sc[:, C_CX:C_CX + 16] = colT(inp["c_ctx"], 16)
    caw = np.asarray(inp["conv_a_w"][0], dtype=f)
    sc[:, C_CAW:C_CAW + 32] = caw.reshape(4, 8, 128).transpose(2, 1, 0).reshape(128, 32)
    sc[:, C_CAB:C_CAB + 8] = colT(inp["conv_a_b"][0], 8)
    sc[:, C_LBR:C_LBR + 16] = colT(np.asarray(inp["lru_b_r"][0]).reshape(-1), 16)
    sc[:, C_LBI:C_LBI + 16] = colT(np.asarray(inp["lru_b_i"][0]).reshape(-1), 16)
    sc[:, C_LAM:C_LAM + 16] = colT(np.asarray(inp["lru_lam"][0]).reshape(-1), 16)
    cbw = np.asarray(inp["conv_b_w"][0], dtype=f)
    sc[:, C_CBW:C_CBW + 24] = cbw.reshape(3, 8, 128).transpose(2, 1, 0).reshape(128, 24)
    sc[:, C_GOA:C_GOA + 8] = colT(inp["g_out_a"][0], 8)
    sc[:, C_GOB:C_GOB + 8] = colT(inp["g_out_b"][0], 8)
    sc[:, C_HM:C_HM + 10] = hm[None, :]
    sc[:, C_CM:C_CM + 6] = cm[None, :]
    sc[:, C_BG:C_BG + 512] = np.asarray(inp["b_gate"][0], dtype=f).reshape(NEXP, 16, 128).transpose(2, 0, 1).reshape(128, 512)
    sc[:, C_BU:C_BU + 512] = np.asarray(inp["b_up"][0], dtype=f).reshape(NEXP, 16, 128).transpose(2, 0, 1).reshape(128, 512)
    sc[:, C_BRT:C_BRT + 32] = np.asarray(inp["b_router"][0], dtype=f)[None, :]
    sc[:, C_ID:C_ID + 128] = np.eye(128, dtype=f)
    sc[:, C_ONE:C_ONE + 128] = 1.0
    pp = np.arange(128, dtype=f)
    sc[:, C_TRI:C_TRI + 128] = (pp[:, None] < pp[None, :]).astype(f)
    sc[:, C_PIDX:C_PIDX + 128] = pp[:, None]
    sc[:, C_IOTA:C_IOTA + CAP] = np.arange(CAP, dtype=f)[None, :]
    sc[:, C_SIDX:C_SIDX + NST] = pp[:, None] + (128.0 * np.arange(NST, dtype=f))[None, :]
    return {"x_all": x_all, "smallc": sc}


def run(inputs, stage=3, stop=None):
    if (stage, stop) not in _NC_CACHE:
        _NC_CACHE[(stage, stop)] = build(stage, stop)
    nc = _NC_CACHE[(stage, stop)]
    shared = _prep_shared(inputs, stage)
    in_maps = []
    for k in range(NCORE):
        m = dict(shared)
        m.update(_prep_core(inputs, k))
        in_maps.append(m)
    res = run_bass_kernel_spmd(nc, in_maps, core_ids=list(range(NCORE)))
    outs = [np.asarray(r["out"]) for r in res.results]
    return np.concatenate(outs, axis=0).reshape(2, 4 * L, D).astype(np.float32)


def kernel(**inputs):
    return run(inputs, stage=3)
```

```python
import numpy as np
from contextlib import ExitStack
import concourse.bass as bass
import concourse.mybir as mybir
from concourse.bass_utils import run_bass_kernel_spmd

F32 = mybir.dt.float32
BF16 = mybir.dt.bfloat16
AF = mybir.ActivationFunctionType
ALU = mybir.AluOpType
AX = mybir.AxisListType

D = 2048
L = 2048
HALO = 64
W = L + 2 * HALO
LC = 256
WC = LC + 2 * HALO
NCORE = 8
EPS = 1e-6
NEXP = 32
WIN_L = [LC, L, L, L, L]
WIN_ROW = [0, WC, WC + W, WC + 2 * W, WC + 3 * W]
XROWS = WC + 4 * W

_o = 0
def _col(n):
    global _o
    r = _o
    _o += n
    return r
C_CT = _col(16); C_CX = _col(16); C_CAW = _col(32); C_CAB = _col(8); C_LBR = _col(16); C_LBI = _col(16)
C_LAM = _col(16); C_CBW = _col(24); C_GOA = _col(8); C_GOB = _col(8); C_HM = _col(10); C_CM = _col(6)
C_BG = _col(512); C_BU = _col(512); C_BRT = _col(32); C_ID = _col(128); C_ONE = _col(128)
C_TRI = _col(128); C_PIDX = _col(128); C_IOTA = _col(384); C_SIDX = _col(3)
NS = _o
CAP = 384
NST = CAP // 128

ENGS = ("pe", "act", "dve", "pool", "sp")
SAME_ENGINE_SYNC = True


ALLBUFS = []
ALLRINGS = []


class Buf:
    __slots__ = ("name", "w", "r")

    def __init__(self, name=""):
        self.name = name
        self.w = None
        self.r = {}
        ALLBUFS.append(self)


class Sem:
    def __init__(self, h, name):
        self.h = h
        self.n = 0
        self.name = name


class Prog:
    def __init__(self):
        self.opsr = {r: {e: [] for e in ENGS} for r in ("pre", "A", "B")}
        self.region = "pre"
        self.regs = {}
        self.esem = {}
        self.waited = {e: {} for e in ENGS}
        self.pending = {e: [] for e in ENGS}
        self.allsems = []

    def op(self, eng, fn, reads=(), writes=(), dma=None):
        deps = self.pending[eng]
        self.pending[eng] = []
        for b in reads:
            if b.w is not None:
                deps.append(b.w)
        for b in writes:
            if b.w is not None:
                deps.append(b.w)
            deps.extend(b.r.values())
        wd = self.waited[eng]
        best = {}
        own = self.esem.get(eng)
        for (s, v) in deps:
            if (not SAME_ENGINE_SYNC) and s is own:
                continue
            if wd.get(id(s), 0) >= v:
                continue
            if id(s) not in best or best[id(s)][1] < v:
                best[id(s)] = (s, v)
        waits = []
        for s, v in best.values():
            waits.append((s, v))
            wd[id(s)] = v
        if dma is not None:
            dma.n += 16
            ev = (dma, dma.n)
            inc = (dma, 16)
        else:
            s = self.esem[eng]
            s.n += 1
            ev = (s, s.n)
            inc = (s, 1)
        for b in reads:
            old = b.r.get(id(ev[0]))
            if old is None or old[1] < ev[1]:
                b.r[id(ev[0])] = ev
        for b in writes:
            b.w = ev
            b.r = {}
        self.opsr[self.region][eng].append((waits, fn, inc))
        return ev

    def barrier(self):
        for e in ENGS:
            self.pending[e] = [(s, s.n) for s in self.allsems if s.n > 0]

    def final_wait(self, eng, sems):
        waits = [(s, s.n) for s in sems if s.n > 0]
        self.opsr[self.region][eng].append((waits, None, None))

    def snapshot(self, counters):
        return dict(bufs=[(b, b.w, dict(b.r)) for b in ALLBUFS], sems=[(s, s.n) for s in self.allsems],
                    waited={e: dict(self.waited[e]) for e in ENGS}, pending={e: list(self.pending[e]) for e in ENGS},
                    rings=[(r, r.i) for r in ALLRINGS], counters=[(c, c[0]) for c in counters], nsems=len(self.allsems))

    def restore(self, st):
        for b, w, r in st["bufs"]:
            b.w = w
            b.r = dict(r)
        for s_, n in st["sems"]:
            s_.n = n
        del self.allsems[st["nsems"]:]
        self.waited = {e: dict(st["waited"][e]) for e in ENGS}
        self.pending = {e: list(st["pending"][e]) for e in ENGS}
        for r, i in st["rings"]:
            r.i = i
        for c, v in st["counters"]:
            c[0] = v

    def emit(self, block, branched=False):
        def mk(name):
            def body(eng):
                def run(lst):
                    for waits, fn, inc in lst:
                        for s, v in waits:
                            eng.wait_ge(s.h, v)
                        if fn is not None:
                            ins = fn(eng)
                            ins.then_inc(inc[0].h, inc[1])
                if not branched:
                    run(self.opsr["pre"][name])
                    return
                reg = eng.alloc_register(f"flag_{name}")
                self.regs[name] = reg
                run(self.opsr["pre"][name])
                with eng.If(eng.snap(reg) > 0):
                    run(self.opsr["B"][name])
                with eng.Else():
                    run(self.opsr["A"][name])
            return body
        block.sync(mk("sp"))
        block.tensor(mk("pe"))
        block.scalar(mk("act"))
        block.vector(mk("dve"))
        block.gpsimd(mk("pool"))


def rev(ap):
    a = ap.ap
    assert len(a) == 2 and a[1][0] == 1, a
    n = a[1][1]
    return bass.AP(ap.tensor, ap.offset + (n - 1), [list(a[0]), [-1, n]])


def chunks(lo, hi, step=512):
    out = []
    while lo < hi:
        out.append((lo, min(lo + step, hi)))
        lo += step
    return out


def build(stage=3, stop=None, nq=4, sparse=True):
    del ALLBUFS[:]
    del ALLRINGS[:]
    nc = bass.Bass("TRN2", target_bir_lowering=False)
    P = Prog()
    es = ExitStack()

    def din(name, shape, dt=F32):
        return nc.dram_tensor(name, list(shape), dt, kind="ExternalInput").ap()

    x_all = din("x_all", [XROWS, D])
    smallc_d = din("smallc", [128, NS])
    wmod_d = din("w_mod", [D, 6 * D])
    bmod_d = din("b_mod", [1, 6 * D])
    gmix_d = din("g_mix_b", [128, D])
    gffn_d = din("g_ffn_b", [128, D])
    gfin_d = din("g_fin_b", [128, D])
    win_d = din("w_in_h", [40, 128, 16 * 128])
    wr_d = din("wr_h", [128, 16 * 128])
    wi_d = din("wi_h", [128, 16 * 128])
    wout_d = din("w_out", [D, D])
    if stage >= 3:
        wrt_d = din("w_router_h", [128, 16 * 32])
        wgu_d = din("wgu_h", [NEXP * 16, 128, 2 * 16 * 128])
        wd_d = din("w_down", [NEXP, D, D])
        bd_d = din("b_down", [NEXP, D])
    out_d = nc.dram_tensor("out", [L, D], F32, kind="ExternalOutput").ap()
    modb_d = nc.dram_tensor("modb", [8, 128, D], F32).ap()
    y_d = nc.dram_tensor("y_scr", [16, 128, L], BF16).ap()
    x1_d = nc.dram_tensor("x1_scr", [L, D], F32).ap()

    ARENA = 53200
    arena = es.enter_context(nc.sbuf_tensor("arena", [128, ARENA], F32))
    PS = [es.enter_context(nc.psum_tensor(f"ps{i}", [128, 512], F32)) for i in range(8)]
    PB = [Buf(f"ps{i}") for i in range(8)]
    for e in ENGS:
        P.esem[e] = Sem(es.enter_context(nc.semaphore(f"s_{e}")), e)
        P.allsems.append(P.esem[e])
    nsem = [0]

    def newsem(name):
        nsem[0] += 1
        s = Sem(es.enter_context(nc.semaphore(f"d{nsem[0]}_{name}")), name)
        P.allsems.append(s)
        return s

    bank_ctr = [0]

    def nbank():
        b = bank_ctr[0] % 8
        bank_ctr[0] += 1
        return b

    top = [0]

    def alloc(n_f32, name=""):
        a = top[0]
        top[0] += n_f32
        assert top[0] <= ARENA, (name, top[0])
        return arena[:, a:a + n_f32]

    def alloc_bf(n_bf, name=""):
        assert n_bf % 2 == 0
        return alloc(n_bf // 2, name).bitcast(BF16)

    def dma(q, out, in_, sem, reads=(), writes=()):
        return P.op(q, lambda e: e.dma_start(out=out, in_=in_), reads, writes, dma=sem)

    def act(out, in_, func, reads, writes, bias=None, scale=None, accum_out=None):
        kw = {}
        if bias is not None:
            kw["bias"] = bias
        if scale is not None:
            kw["scale"] = scale
        if accum_out is not None:
            kw["accum_out"] = accum_out
        return P.op("act", lambda e: e.activation(out=out, in_=in_, func=func, **kw), reads, writes)

    def ts(eng, out, in0, s1, s2, op0, op1, reads, writes):
        if s2 is None:
            return P.op(eng, lambda e: e.tensor_scalar(out=out, in0=in0, scalar1=s1, scalar2=None, op0=op0), reads, writes)
        return P.op(eng, lambda e: e.tensor_scalar(out=out, in0=in0, scalar1=s1, scalar2=s2, op0=op0, op1=op1), reads, writes)

    def tt(eng, out, in0, in1, op, reads, writes):
        return P.op(eng, lambda e: e.tensor_tensor(out=out, in0=in0, in1=in1, op=op), reads, writes)

    def stt(eng, out, in0, scalar, in1, op0, op1, reads, writes):
        return P.op(eng, lambda e: e.scalar_tensor_tensor(out=out, in0=in0, scalar=scalar, in1=in1, op0=op0, op1=op1), reads, writes)

    def mmg(items, reads, writes):
        def fn(e):
            ins = None
            for (o, l, r, s, t) in items:
                ins = e.matmul(o, l, r, start=s, stop=t)
            return ins
        return P.op("pe", fn, reads, writes)

    def trg(items, reads, writes):
        def fn(e):
            ins = None
            for (o, i_, idn) in items:
                ins = e.transpose(o, i_, idn)
            return ins
        return P.op("pe", fn, reads, writes)

    class Ring:
        def __init__(self, n, n_f32, name, bf=False, shape=None):
            self.slots = []
            for i in range(n):
                ap = alloc(n_f32, name)
                if bf:
                    ap = ap.bitcast(BF16)
                self.slots.append((ap, Buf(f"{name}{i}"), newsem(f"{name}{i}")))
            self.i = 0
            ALLRINGS.append(self)

        def next(self):
            s = self.slots[self.i % len(self.slots)]
            self.i += 1
            return s

    def finish_early():
        S_e = newsem("early")
        P.barrier()
        dma("sp", out_d[0:128, :], arena[:, 0:2048], S_e)
        P.final_wait("sp", [S_e])
        with nc.Block() as block:
            P.emit(block)
        es.close()
        return nc

    smallc = alloc(NS, "smallc")
    B_smallc = Buf("smallc")
    S_const = newsem("const")
    dma("sp", smallc, smallc_d[:, :], S_const, writes=[B_smallc])
    wr_bf = alloc_bf(2048, "wr")
    wi_bf = alloc_bf(2048, "wi")
    B_wri = Buf("wri")
    S_wri = newsem("wri")
    dma("pool", wr_bf, wr_d[:, :], S_wri, writes=[B_wri])
    dma("pool", wi_bf, wi_d[:, :], S_wri, writes=[B_wri])
    if stage >= 3:
        wrt_bf = alloc_bf(512, "wrt")
        B_wrt = Buf("wrt")
        S_wrt = newsem("wrt")
        dma("pool", wrt_bf, wrt_d[:, :], S_wrt, writes=[B_wrt])
    ident_bf = alloc_bf(128, "identbf")
    B_ident = Buf("ident")
    smalld = alloc(512, "smalld")
    ident_f = smallc[:, C_ID:C_ID + 128]
    ones_f = smallc[:, C_ONE:C_ONE + 128]
    P.op("act", lambda e: e.activation(out=ident_bf, in_=ident_f, func=AF.Copy), [B_smallc], [B_ident])
    SD_SC = 0; SD_SX = 16; SD_CNEG = 32; SD_CARRY = 48; SD_RSTD = 64; SD_SUMH = 128; SD_SUMA = 208; SD_TMP = 288
    B_sc = Buf("sc"); B_cneg = Buf("cneg"); B_carry = Buf("carry"); B_rstd = Buf("rstdab")
    B_sum = [Buf(f"sum{w}") for w in range(5)]
    sc = smalld[:, SD_SC:SD_SC + 16]
    sx = smalld[:, SD_SX:SD_SX + 16]
    cneg = smalld[:, SD_CNEG:SD_CNEG + 16]
    carry = smalld[:, SD_CARRY:SD_CARRY + 16]
    act(sc, smallc[:, C_CT:C_CT + 16], AF.Silu, [B_smallc], [B_sc])
    act(sx, smallc[:, C_CX:C_CX + 16], AF.Silu, [B_smallc], [B_sc])
    act(cneg, smallc[:, C_LAM:C_LAM + 16], AF.Exp, [B_smallc], [B_cneg], scale=-1.0)
    act(cneg, cneg, AF.Ln, [B_cneg], [B_cneg], bias=1.0)
    ts("dve", cneg, cneg, -8.0, None, ALU.mult, None, [B_cneg], [B_cneg])
    persist_top = top[0]

    cb_l = alloc(2048, "cb_l").rearrange("p (k m) -> p k m", k=16)
    cx_l = alloc(2048, "cx_l").rearrange("p (k m) -> p k m", k=16)
    B_cbl = Buf("cbl")
    for k in range(16):
        ts("dve", cb_l[:, k, :], ones_f, sc[:, k:k + 1], None, ALU.mult, None, [B_smallc, B_sc], [B_cbl])
        ts("dve", cx_l[:, k, :], ones_f, sx[:, k:k + 1], None, ALU.mult, None, [B_smallc, B_sc], [B_cbl])
    wm_ring = Ring(3, 2048, "wm")
    ev_ring = Ring(2, 2048, "ev")
    bm_ring = Ring(2, 2048, "bm")
    gbuf = alloc(2048, "gbuf")
    B_gbuf = Buf("gbuf")
    S_g = newsem("gload")
    for g in range(6):
        use_ctx = g < 2
        bm_ap, bm_b, bm_s = bm_ring.next()
        dma("sp", bm_ap[0:1, :], bmod_d[0:1, g * D:(g + 1) * D], bm_s, writes=[bm_b])
        if g in (1, 4):
            dma("sp", gbuf, (gmix_d if g == 1 else gffn_d)[:, :], S_g, writes=[B_gbuf])
        banks = [nbank() for _ in range(4)]
        xbanks = [nbank() for _ in range(4)] if use_ctx else []
        for k in range(16):
            w_ap, w_b, w_s = wm_ring.next()
            dma("sp", w_ap, wmod_d[k * 128:(k + 1) * 128, g * D:(g + 1) * D], w_s, writes=[w_b])
            for n in range(4):
                mmg([(PS[banks[n]][:, :], cb_l[:, k, :], w_ap[:, n * 512:(n + 1) * 512], k == 0, False)],
                    [w_b, B_cbl], [PB[banks[n]]])
                if use_ctx:
                    mmg([(PS[xbanks[n]][:, :], cx_l[:, k, :], w_ap[:, n * 512:(n + 1) * 512], k == 0, False)],
                        [w_b, B_cbl], [PB[xbanks[n]]])
        for n in range(4):
            mmg([(PS[banks[n]][:, :], ones_f[0:1, :], bm_ap[0:1, n * 512:(n + 1) * 512], False, True)],
                [bm_b, B_smallc], [PB[banks[n]]])
            if use_ctx:
                mmg([(PS[xbanks[n]][:, :], ones_f[0:1, :], bm_ap[0:1, n * 512:(n + 1) * 512], False, True)],
                    [bm_b, B_smallc], [PB[xbanks[n]]])
        for (bks, idx) in ((banks, g), (xbanks, 6 + g)):
            if not bks:
                continue
            e_ap, e_b, e_s = ev_ring.next()
            for n in range(4):
                dst = e_ap[:, n * 512:(n + 1) * 512]
                if g in (1, 4):
                    stt("dve", dst, PS[bks[n]][:, :], 1.0, gbuf[:, n * 512:(n + 1) * 512], ALU.add, ALU.mult,
                        [PB[bks[n]], B_gbuf], [e_b])
                else:
                    act(dst, PS[bks[n]][:, :], AF.Copy, [PB[bks[n]]], [e_b])
            dma("sp", modb_d[idx], e_ap, e_s, reads=[e_b], writes=[])
    P.barrier()
    if stop == "p0":
        return finish_early()
    top[0] = persist_top
    xnT = alloc_bf(16 * W, "xnT").rearrange("p (k n) -> p k n", k=16)
    B_xnT = Buf("xnT")
    gs_b = alloc(2048, "gs_b")
    sh_b = alloc(2048, "sh_b")
    B_gs = Buf("gs")
    S_gs = newsem("gs")
    w_ring = Ring(3, 1024, "wring", bf=True)
    acc_a = alloc(2048, "acc_a")
    acc_b = alloc(2048, "acc_b")
    B_acca = Buf("acca"); B_accb = Buf("accb")
    ph1_top = top[0]
    xs_ring = Ring(2, 2048, "xs")
    t1 = alloc(2048, "t1"); B_t1 = Buf("t1")
    xnb_ring = Ring(2, 1024, "xnb", bf=True)
    junk = alloc_bf(2048, "junk"); B_junk = Buf("junk")
    top[0] = ph1_top
    T0 = alloc(W, "T0"); T1 = alloc(L, "T1"); Ta = alloc(L, "Ta"); Tb = alloc(L, "Tb")
    TsBig = alloc(2 * L, "TsBig"); Ts = TsBig[:, 0:L]; Ts2 = TsBig[:, L:2 * L]; Tz = TsBig[:, 0:W]
    xc_bf = alloc_bf(L, "xcbf")
    yb_ring = Ring(2, 1024, "ybf", bf=True)
    B_T0 = Buf("T0"); B_T1 = Buf("T1"); B_Ta = Buf("Ta"); B_Tb = Buf("Tb"); B_Ts = Buf("Ts"); B_Ts2 = Buf("Ts2"); B_xcbf = Buf("xcbf")
    B_ssq = Buf("ssq")
    ssq = smalld[:, SD_TMP:SD_TMP + 32]
    rs = smalld[:, SD_TMP + 32:SD_TMP + 64]
    B_rs = Buf("rs")
    cp_ctr = [0]

    def copy_any(out, in_, reads, writes):
        cp_ctr[0] += 1
        if cp_ctr[0] % 2:
            return act(out, in_, AF.Copy, reads, writes)
        return P.op("dve", lambda e: e.tensor_copy(out=out, in_=in_), reads, writes)

    def rstd_chain(dst, src, scale, rb, wb):
        ts("dve", dst, src, scale, EPS, ALU.mult, ALU.add, rb, wb)
        act(dst, dst, AF.Sqrt, wb, wb)
        P.op("dve", lambda e: e.reciprocal(out=dst, in_=dst), wb, wb)

    def build_xnT(row0, ntile, dstT, B_dst, g_ap, s_ap, B_g):
        for i in range(ntile):
            x_ap, x_b, x_s = xs_ring.next()
            if isinstance(row0, tuple):
                src = row0[0][row0[1] + i * 128: row0[1] + (i + 1) * 128, :]
            else:
                src = x_all[row0 + i * 128: row0 + (i + 1) * 128, :]
            dma("sp", x_ap, src, x_s, writes=[x_b])
            col = i % 32
            act(junk, x_ap, AF.Square, [x_b], [B_junk, B_ssq], accum_out=ssq[:, col:col + 1])
            rstd_chain(rs[:, col:col + 1], ssq[:, col:col + 1], 1.0 / D, [B_ssq], [B_rs])
            stt("dve", t1, x_ap, rs[:, col:col + 1], g_ap, ALU.mult, ALU.mult, [x_b, B_rs, B_g], [B_t1])
            n_ap, n_b, _ = xnb_ring.next()
            tt("pool", n_ap, t1, s_ap, ALU.add, [B_t1, B_g], [n_b])
            for kb in range(4):
                bk = nbank()
                pv = PS[bk][:, :].bitcast(BF16)
                trg([(pv[:, j * 128:(j + 1) * 128], n_ap[:, (kb * 4 + j) * 128:(kb * 4 + j + 1) * 128], ident_bf) for j in range(4)],
                    [n_b, B_ident], [PB[bk]])
                copy_any(dstT[:, kb * 4:kb * 4 + 4, i * 128:(i + 1) * 128],
                         pv[:, 0:512].rearrange("p (k n) -> p k n", k=4), [PB[bk]], [B_dst])

    def load_w(ct):
        w_ap, w_b, w_s = w_ring.next()
        dma("pool", w_ap, win_d[ct], w_s, writes=[w_b])
        return w_ap.rearrange("p (k j) -> p k j", k=16), w_b

    def inproj(w3, w_b, c0, c1, evac):
        for (a, b) in chunks(c0, c1):
            bk = nbank()
            mmg([(PS[bk][:, 0:b - a], w3[:, k, :], xnT[:, k, a:b], k == 0, k == 15) for k in range(16)],
                [w_b, B_xnT], [PB[bk]])
            evac(PS[bk][:, 0:b - a], PB[bk], a, b)

    def colp(base, idx):
        return smallc[:, base + idx: base + idx + 1]

    sumH = smalld[:, SD_SUMH:SD_SUMH + 80]
    sumA = smalld[:, SD_SUMA:SD_SUMA + 80]
    S_y = newsem("ystore")

    for w in range(5):
        Lw = WIN_L[w]
        Ww = Lw + 2 * HALO
        mine = (w == 4)
        if w == 0:
            dma("sp", gs_b, modb_d[7], S_gs, writes=[B_gs])
            dma("sp", sh_b, modb_d[6], S_gs, writes=[B_gs])
        if w == 1:
            dma("sp", gs_b, modb_d[1], S_gs, writes=[B_gs])
            dma("sp", sh_b, modb_d[0], S_gs, writes=[B_gs])
        build_xnT(WIN_ROW[w], Ww // 128, xnT, B_xnT, gs_b, sh_b, B_gs)
        P.barrier()
        if stop == f"x{w}":
            return finish_early()
        if mine:
            tmpc = smalld[:, SD_TMP + 64:SD_TMP + 80]
            B_tc = Buf("tmpc")
            P.op("dve", lambda e: e.tensor_copy(out=carry, in_=sumH[:, 0:16]), [B_sum[0]], [B_carry])
            for (lo, order, mbase) in ((0, (1, 2, 3), 0), (8, (3, 2, 1), 3)):
                for wo in order:
                    cs = carry[:, lo:lo + 8]
                    tcs = tmpc[:, lo:lo + 8]
                    tt("dve", tcs, sumA[:, wo * 16 + lo: wo * 16 + lo + 8], cs, ALU.mult, [B_sum[wo], B_carry], [B_tc])
                    tt("dve", tcs, tcs, sumH[:, wo * 16 + lo: wo * 16 + lo + 8], ALU.add, [B_sum[wo], B_tc], [B_tc])
                    tt("dve", tcs, tcs, cs, ALU.subtract, [B_tc, B_carry], [B_tc])
                    stt("dve", cs, tcs, colp(C_CM, mbase + wo - 1), cs, ALU.mult, ALU.add, [B_tc, B_carry, B_smallc], [B_carry])
            P.op("pool", lambda e: e.memset(acc_a, 0.0), [], [B_acca])
            P.op("pool", lambda e: e.memset(acc_b, 0.0), [], [B_accb])
        hm_l = colp(C_HM, 2 * w)
        hm_r = colp(C_HM, 2 * w + 1)
        for c in range(8):
            w3, w_b = load_w(8 + c)
            inproj(w3, w_b, 0, Ww, lambda ps, pb, a, b: act(T0[:, a:b], ps, AF.Copy, [pb], [B_T0]))
            ts("dve", T0[:, 0:HALO], T0[:, 0:HALO], hm_l, None, ALU.mult, None, [B_T0, B_smallc], [B_T0])
            ts("dve", T0[:, HALO + Lw:Ww], T0[:, HALO + Lw:Ww], hm_r, None, ALU.mult, None, [B_T0, B_smallc], [B_T0])
            xc = T1[:, 0:Lw]
            ts("dve", xc, T0[:, 62:62 + Lw], colp(C_CAW, c * 4 + 0), colp(C_CAB, c), ALU.mult, ALU.add, [B_T0, B_smallc], [B_T1])
            for tap in (1, 2, 3):
                stt("dve", xc, T0[:, 62 + tap:62 + tap + Lw], colp(C_CAW, c * 4 + tap), xc, ALU.mult, ALU.add, [B_T0, B_T1, B_smallc], [B_T1])
            act(xc_bf[:, 0:Lw], xc, AF.Copy, [B_T1], [B_xcbf])
            for d in range(2):
                dc = d * 8 + c
                hbuf, B_h = (Ts, B_Ts) if d == 0 else (Ts2, B_Ts2)
                for (a, b) in chunks(0, Lw):
                    bk = nbank()
                    mmg([(PS[bk][:, 0:b - a], wr_bf[:, dc * 128:(dc + 1) * 128], xc_bf[:, a:b], True, True)], [B_wri, B_xcbf], [PB[bk]])
                    act(Ta[:, a:b], PS[bk][:, 0:b - a], AF.Sigmoid, [PB[bk], B_smallc], [B_Ta], bias=colp(C_LBR, dc))
                    bk = nbank()
                    mmg([(PS[bk][:, 0:b - a], wi_bf[:, dc * 128:(dc + 1) * 128], xc_bf[:, a:b], True, True)], [B_wri, B_xcbf], [PB[bk]])
                    act(Tb[:, a:b], PS[bk][:, 0:b - a], AF.Sigmoid, [PB[bk], B_smallc], [B_Tb], bias=colp(C_LBI, dc))
                av = Ta[:, 0:Lw]; bv = Tb[:, 0:Lw]; hv = hbuf[:, 0:Lw]
                act(av, av, AF.Exp, [B_Ta, B_cneg], [B_Ta], scale=cneg[:, dc:dc + 1])
                act(hv, av, AF.Square, [B_Ta], [B_h])
                act(hv, hv, AF.Sqrt, [B_h], [B_h], scale=-1.0, bias=1.0)
                tt("dve", bv, bv, xc, ALU.mult, [B_Tb, B_T1], [B_Tb])
                tt("dve", bv, bv, hv, ALU.mult, [B_Tb, B_h], [B_Tb])
                init = carry[:, dc:dc + 1] if mine else 0.0
                if d == 0:
                    P.op("dve", lambda e, hv=hv, av=av, bv=bv, init=init: e.tensor_tensor_scan(
                        out=hv, data0=av, data1=bv, initial=init, op0=ALU.mult, op1=ALU.add), [B_Ta, B_Tb, B_carry], [B_h])
                else:
                    P.op("dve", lambda e, hv=hv, av=av, bv=bv, init=init: e.tensor_tensor_scan(
                        out=rev(hv), data0=rev(av), data1=rev(bv), initial=init, op0=ALU.mult, op1=ALU.add), [B_Ta, B_Tb, B_carry], [B_h])
                if not mine:
                    endcol = hv[:, Lw - 1:Lw] if d == 0 else hv[:, 0:1]
                    P.op("dve", lambda e, endcol=endcol, dc=dc, w=w: e.tensor_copy(out=sumH[:, w * 16 + dc:w * 16 + dc + 1], in_=endcol), [B_h], [B_sum[w]])
                    P.op("dve", lambda e, av=av, dc=dc, w=w: e.tensor_reduce(out=sumA[:, w * 16 + dc:w * 16 + dc + 1], in_=av, axis=AX.X, op=ALU.mult), [B_Ta], [B_sum[w]])
            if not mine:
                continue
            tt("dve", Ts, Ts, Ts2, ALU.add, [B_Ts, B_Ts2], [B_Ts])
            w3, w_b = load_w(c)
            Tg = T0[:, 0:L]
            inproj(w3, w_b, HALO, HALO + L, lambda ps, pb, a, b: act(Tg[:, a - HALO:b - HALO], ps, AF.Copy, [pb], [B_T0]))
            Tq = Ta
            tt("dve", Tq, Tg, Tg, ALU.mult, [B_T0], [B_Ta])
            ts("dve", Tq, Tq, 0.044715, 1.0, ALU.mult, ALU.add, [B_Ta], [B_Ta])
            tt("dve", Tq, Tq, Tg, ALU.mult, [B_Ta, B_T0], [B_Ta])
            act(Tq, Tq, AF.Sigmoid, [B_Ta], [B_Ta], scale=1.5957691216057308)
            tt("dve", Tq, Tq, Tg, ALU.mult, [B_Ta, B_T0], [B_Ta])
            tt("dve", Tq, Tq, Ts, ALU.mult, [B_Ta, B_Ts], [B_Ta])
            tt("pool", Tb, Tq, Tq, ALU.mult, [B_Ta], [B_Tb])
            tt("pool", acc_a, acc_a, Tb, ALU.add, [B_Tb, B_acca], [B_acca])
            y_ap, y_b, y_s = yb_ring.next()
            act(y_ap, Tq, AF.Copy, [B_Ta, B_smallc], [y_b], scale=colp(C_GOA, c))
            dma("sp", y_d[c], y_ap, y_s, reads=[y_b])
        if not mine:
            P.barrier()
            if stop == f"w{w}":
                return finish_early()
            continue
        for c in range(8):
            Tc = T0
            w3, w_b = load_w(24 + c)
            inproj(w3, w_b, 0, W, lambda ps, pb, a, b: act(Tc[:, a:b], ps, AF.Copy, [pb], [B_T0]))
            w3, w_b = load_w(32 + c)
            inproj(w3, w_b, 0, W, lambda ps, pb, a, b: tt("dve", Tz[:, a:b], ps, Tc[:, a:b], ALU.mult, [pb, B_T0], [B_Ts, B_Ts2]))
            ts("dve", Tz[:, 0:HALO], Tz[:, 0:HALO], hm_l, None, ALU.mult, None, [B_Ts, B_Ts2, B_smallc], [B_Ts, B_Ts2])
            ts("dve", Tz[:, HALO + L:W], Tz[:, HALO + L:W], hm_r, None, ALU.mult, None, [B_Ts, B_Ts2, B_smallc], [B_Ts, B_Ts2])
            Tcv = T1
            w0 = colp(C_CBW, c * 3 + 0); w1 = colp(C_CBW, c * 3 + 1); w2 = colp(C_CBW, c * 3 + 2)
            ts("dve", Tcv, Tz[:, HALO:HALO + L], w1, None, ALU.mult, None, [B_Ts, B_Ts2, B_smallc], [B_T1])
            if c < 4:
                zv = Tz[:, HALO:HALO + L].rearrange("p (r c) -> p r c", c=64)
                ov = Tcv.rearrange("p (r c) -> p r c", c=64)
                stt("dve", ov[:, :, 1:64], zv[:, :, 0:63], w0, ov[:, :, 1:64], ALU.mult, ALU.add, [B_Ts, B_Ts2, B_T1, B_smallc], [B_T1])
                stt("dve", ov[:, :, 0:63], zv[:, :, 1:64], w2, ov[:, :, 0:63], ALU.mult, ALU.add, [B_Ts, B_Ts2, B_T1, B_smallc], [B_T1])
            else:
                stt("dve", Tcv, Tz[:, 0:L], w0, Tcv, ALU.mult, ALU.add, [B_Ts, B_Ts2, B_T1, B_smallc], [B_T1])
                stt("dve", Tcv, Tz[:, 2 * HALO:2 * HALO + L], w2, Tcv, ALU.mult, ALU.add, [B_Ts, B_Ts2, B_T1, B_smallc], [B_T1])
            w3, w_b = load_w(16 + c)
            Ty = Ta
            inproj(w3, w_b, HALO, HALO + L, lambda ps, pb, a, b: tt("dve", Ty[:, a - HALO:b - HALO], ps, Tcv[:, a - HALO:b - HALO], ALU.mult, [pb, B_T1], [B_Ta]))
            tt("pool", Tb, Ty, Ty, ALU.mult, [B_Ta], [B_Tb])
            tt("pool", acc_b, acc_b, Tb, ALU.add, [B_Tb, B_accb], [B_accb])
            y_ap, y_b, y_s = yb_ring.next()
            act(y_ap, Ty, AF.Copy, [B_Ta, B_smallc], [y_b], scale=colp(C_GOB, c))
            dma("sp", y_d[8 + c], y_ap, y_s, reads=[y_b])
        bk = nbank()
        for g, (acc, B_acc) in enumerate(((acc_a, B_acca), (acc_b, B_accb))):
            for i in range(16):
                j = g * 16 + i
                mmg([(PS[bk][:, 2 * j:2 * j + 2], acc[:, i * 128:(i + 1) * 128], ones_f[:, 0:2], True, True)], [B_acc, B_smallc], [PB[bk]])
        rstd_ab = smalld[:, SD_RSTD:SD_RSTD + 64]
        P.op("dve", lambda e, src=PS[bk][:, 0:64], dst=rstd_ab: e.tensor_copy(out=dst, in_=src), [PB[bk]], [B_rstd])
        rstd_chain(rstd_ab, rstd_ab, 1.0 / 1024, [B_rstd], [B_rstd])
    P.barrier()
    if stop == "p1":
        return finish_early()

    top[0] = persist_top
    wout = alloc_bf(16 * D, "wout").rearrange("p (c n) -> p c n", c=16)
    B_wout = Buf("wout")
    S_wout = newsem("wout")
    for c in range(16):
        dma("pool", wout[:, c, :], wout_d[c * 128:(c + 1) * 128, :], S_wout, writes=[B_wout])
    if stop == "p2a":
        return finish_early()
    ys_ring = Ring(2, 4096, "ysb", bf=True)
    xs2_ring = Ring(2, 2048, "xs2")
    gt1_b = alloc(2048, "gt1")
    B_gt1 = Buf("gt1")
    dma("sp", gt1_b, modb_d[2], S_gs, writes=[B_gt1])
    tA_ring = Ring(2, 512, "tA")
    tB_ring = Ring(2, 512, "tB")
    S_x1 = newsem("x1store")
    rstd_ab = smalld[:, SD_RSTD:SD_RSTD + 64]
    mine_row = WIN_ROW[4] + HALO
    ys3 = None
    for i in range(16):
        if i % 4 == 0:
            y_ap, y_b, y_s = ys_ring.next()
            ys3 = y_ap.rearrange("p (c n) -> p c n", c=16)
            for c in range(16):
                dma("sp", ys3[:, c, :], y_d[c][:, (i // 4) * 512:(i // 4 + 1) * 512], y_s, reads=[], writes=[y_b])
            ys_b = y_b
        x_ap, x_b, x_s = xs2_ring.next()
        dma("sp", x_ap, x_all[mine_row + i * 128: mine_row + (i + 1) * 128, :], x_s, writes=[x_b])
        to = (i % 4) * 128
        for n in range(4):
            bA = nbank(); bB = nbank()
            mmg([(PS[bA][:, :], ys3[:, c, to:to + 128], wout[:, c, n * 512:(n + 1) * 512], c == 0, c == 7) for c in range(8)], [ys_b, B_wout], [PB[bA]])
            mmg([(PS[bB][:, :], ys3[:, c, to:to + 128], wout[:, c, n * 512:(n + 1) * 512], c == 8, c == 15) for c in range(8, 16)], [ys_b, B_wout], [PB[bB]])
            ta, ta_b, _ = tA_ring.next()
            tb, tb_b, _ = tB_ring.next()
            act(ta, PS[bA][:, :], AF.Copy, [PB[bA], B_rstd], [ta_b], scale=rstd_ab[:, 2 * i:2 * i + 1])
            stt("dve", tb, PS[bB][:, :], rstd_ab[:, 32 + 2 * i:32 + 2 * i + 1], ta, ALU.mult, ALU.add, [PB[bB], B_rstd, ta_b], [tb_b])
            tt("pool", tb, tb, gt1_b[:, n * 512:(n + 1) * 512], ALU.mult, [tb_b, B_gt1], [tb_b])
            tt("pool", x_ap[:, n * 512:(n + 1) * 512], x_ap[:, n * 512:(n + 1) * 512], tb, ALU.add, [tb_b, x_b], [x_b])
        if stop == "p2b":
            return finish_early()
        dst = out_d if stage < 3 else x1_d
        dma("sp", dst[i * 128:(i + 1) * 128, :], x_ap, x_s, reads=[x_b])
    P.barrier()
    out_sems = [s for (_, _, s) in xs2_ring.slots]


    branched = (stage >= 3 and sparse)
    if branched:
        top[0] = persist_top
        xn2_d = nc.dram_tensor("xn2_scr", [16, 128, D], BF16).ap()
        flag_d = nc.dram_tensor("flag_scr", [1, 1], mybir.dt.int32).ap()
        wreg = wr_bf.bitcast(F32)
        wreg2 = wi_bf.bitcast(F32)
        G_all = wreg[:, 0:512].rearrange("p (i e) -> p i e", i=16)
        posm_all = wreg[:, 512:1024].rearrange("p (i e) -> p i e", i=16)
        posmT = wreg2
        B_Gall = Buf("Gall"); B_posm = Buf("posm"); B_posmT = Buf("posmT")
        run_c = smalld[:, SD_TMP + 96:SD_TMP + 128]
        maxc = smalld[:, SD_TMP + 128:SD_TMP + 160]
        flagf = smalld[:, SD_TMP + 160:SD_TMP + 162]
        flagi = smalld[:, SD_TMP + 162:SD_TMP + 163].bitcast(mybir.dt.int32)
        B_run = Buf("run"); B_maxc = Buf("maxc"); B_flag = Buf("flag")
        P.op("dve", lambda e: e.memset(run_c, 0.0), [], [B_run])
        P.op("dve", lambda e: e.memset(maxc, 0.0), [], [B_maxc])
        tri_bf = alloc_bf(128, "tribf"); one_bf = alloc_bf(128, "onebf")
        B_tri = Buf("tri")
        P.op("act", lambda e: e.activation(out=tri_bf, in_=smallc[:, C_TRI:C_TRI + 128], func=AF.Copy), [B_smallc], [B_tri])
        P.op("act", lambda e: e.activation(out=one_bf, in_=ones_f, func=AF.Copy), [B_smallc], [B_tri])
        xt_ring = Ring(2, 2048, "xtR")
        t1r = alloc(2048, "t1r"); B_t1r = Buf("t1r")
        xnr_ring = Ring(2, 1024, "xnr", bf=True)
        junkr = alloc_bf(2048, "junkr"); B_junkr = Buf("junkr")
        xtT_ring = Ring(2, 1024, "xtT", bf=True)
        modR0 = alloc(2048, "modR0"); modR1 = alloc(2048, "modR1")
        B_modR = Buf("modR")
        S_modR = newsem("modR")
        dma("sp", modR0, modb_d[4], S_modR, writes=[B_modR])
        dma("sp", modR1, modb_d[3], S_modR, writes=[B_modR])
        lgR = alloc(32, "lgR"); exR = alloc(32, "exR"); mkR = alloc(32, "mkR"); m8R = alloc(8, "m8R"); smR = alloc(4, "smR")
        psR = alloc(32, "posR"); mkbR = alloc_bf(32, "mkbR")
        B_lgR = Buf("lgR"); B_exR = Buf("exR"); B_mkR = Buf("mkR"); B_m8R = Buf("m8R"); B_smR = Buf("smR"); B_psR = Buf("psR"); B_mkbR = Buf("mkbR")
        brtR = smallc[:, C_BRT:C_BRT + 32]
        wrt3R = wrt_bf.rearrange("p (k e) -> p k e", k=16)
        for i in range(16):
            x_ap, x_b, x_s = xt_ring.next()
            dma("sp", x_ap, x1_d[i * 128:(i + 1) * 128, :], x_s, writes=[x_b])
            col = 16 + (i % 8)
            act(junkr, x_ap, AF.Square, [x_b], [B_junkr, B_ssq], accum_out=ssq[:, col:col + 1])
            rstd_chain(rs[:, col:col + 1], ssq[:, col:col + 1], 1.0 / D, [B_ssq], [B_rs])
            stt("dve", t1r, x_ap, rs[:, col:col + 1], modR0, ALU.mult, ALU.mult, [x_b, B_rs, B_modR], [B_t1r])
            n_ap, n_b, n_s = xnr_ring.next()
            tt("pool", n_ap, t1r, modR1, ALU.add, [B_t1r, B_modR], [n_b])
            dma("sp", xn2_d[i], n_ap, n_s, reads=[n_b])
            xT_ap, xT_b, _ = xtT_ring.next()
            xT3 = xT_ap.rearrange("p (k n) -> p k n", k=16)
            for kb in range(4):
                bk = nbank()
                pv = PS[bk][:, :].bitcast(BF16)
                trg([(pv[:, j * 128:(j + 1) * 128], n_ap[:, (kb * 4 + j) * 128:(kb * 4 + j + 1) * 128], ident_bf) for j in range(4)],
                    [n_b, B_ident], [PB[bk]])
                copy_any(xT3[:, kb * 4:kb * 4 + 4, :], pv[:, 0:512].rearrange("p (k n) -> p k n", k=4), [PB[bk]], [xT_b])
            bk = nbank()
            mmg([(PS[bk][:, 0:32], xT3[:, k, :], wrt3R[:, k, :], k == 0, k == 15) for k in range(16)], [xT_b, B_wrt], [PB[bk]])
            tt("dve", lgR, PS[bk][:, 0:32], brtR, ALU.add, [PB[bk], B_smallc], [B_lgR])
            P.op("dve", lambda e: e.max(out=m8R, in_=lgR), [B_lgR], [B_m8R])
            ts("dve", smR[:, 0:1], m8R[:, 0:1], -1.0, None, ALU.mult, None, [B_m8R], [B_smR])
            act(exR, lgR, AF.Exp, [B_lgR, B_smR], [B_exR], bias=smR[:, 0:1])
            ts("dve", mkR, lgR, m8R[:, 3:4], None, ALU.is_ge, None, [B_lgR, B_m8R], [B_mkR])
            tt("dve", exR, exR, mkR, ALU.mult, [B_exR, B_mkR], [B_exR])
            P.op("dve", lambda e: e.tensor_reduce(out=smR[:, 1:2], in_=exR, axis=AX.X, op=ALU.add), [B_exR], [B_smR])
            P.op("dve", lambda e: e.reciprocal(out=smR[:, 2:3], in_=smR[:, 1:2]), [B_smR], [B_smR])
            ts("dve", G_all[:, i, :], exR, smR[:, 2:3], None, ALU.mult, None, [B_exR, B_smR], [B_Gall])
            P.op("dve", lambda e: e.tensor_copy(out=mkbR, in_=mkR), [B_mkR], [B_mkbR])
            bA = nbank(); bB = nbank()
            mmg([(PS[bA][:, 0:32], tri_bf, mkbR, True, True)], [B_tri, B_mkbR], [PB[bA]])
            mmg([(PS[bB][:, 0:32], one_bf, mkbR, True, True)], [B_tri, B_mkbR], [PB[bB]])
            tt("dve", psR, PS[bA][:, 0:32], run_c, ALU.add, [PB[bA], B_run], [B_psR])
            tt("dve", psR, psR, mkR, ALU.mult, [B_psR, B_mkR], [B_psR])
            stt("dve", posm_all[:, i, :], psR, -1.0, mkR, ALU.add, ALU.add, [B_psR, B_mkR], [B_posm])
            tt("dve", run_c, PS[bB][:, 0:32], run_c, ALU.add, [PB[bB], B_run], [B_run])
            if i % 8 == 7:
                tt("dve", maxc, maxc, run_c, ALU.max, [B_maxc, B_run], [B_maxc])
                P.op("dve", lambda e: e.memset(run_c, 0.0), [], [B_run])
        P.op("dve", lambda e: e.tensor_reduce(out=flagf[:, 0:1], in_=maxc, axis=AX.X, op=ALU.max), [B_maxc], [B_flag])
        ts("dve", flagf[:, 1:2], flagf[:, 0:1], float(CAP), None, ALU.is_gt, None, [B_flag], [B_flag])
        P.op("dve", lambda e: e.tensor_copy(out=flagi, in_=flagf[:, 1:2]), [B_flag], [B_flag])
        S_flag = newsem("flag")
        B_flagd = Buf("flagd")
        dma("sp", flag_d[0:1, 0:1], flagi[0:1, 0:1], S_flag, reads=[B_flag], writes=[B_flagd])
        P.barrier()
        for en in ENGS:
            P.op(en, lambda e, en=en: e.reg_load(P.regs[en], flag_d[0:1, 0:1]), [B_flagd], [])
        snap = P.snapshot([bank_ctr, top, cp_ctr])
        P.region = "A"
        top[0] = persist_top
        accA = [alloc(2048, f"accA{i}") for i in range(8)]
        B_accA = [Buf(f"accA{i}") for i in range(8)]
        S_accA = [newsem(f"accA{i}") for i in range(8)]
        xn2_tok = alloc_bf(8 * D, "xn2tok").rearrange("p (j d) -> p j d", j=8)
        B_xtok = Buf("xtok"); S_xtok = newsem("xtok")
        selA = alloc_bf(8 * CAP, "selA").rearrange("p (j s) -> p j s", j=8)
        B_sel = Buf("sel")
        XeT_raw = alloc(8 * CAP, "XeT")
        XeT = XeT_raw.bitcast(BF16).rearrange("p (k s) -> p k s", k=16)
        B_XeT = Buf("XeT")
        actA_raw = alloc(8 * CAP, "actA")
        actA = actA_raw.bitcast(BF16).rearrange("p (f s) -> p f s", f=16)
        B_actA = Buf("actA")
        Ye_raw = alloc(1024 * NST, "Ye")
        Ye = Ye_raw.bitcast(BF16).rearrange("p (i d) -> p i d", i=NST)
        B_Ye = Buf("Ye")
        selT_raw = alloc(512 * NST, "selT")
        selT = selT_raw.bitcast(BF16).rearrange("p (i t) -> p i t", i=NST)
        B_selT = Buf("selT")
        wdA_ring = Ring(4, 1024, "wdA", bf=True)
        wguA_ring = Ring(4, 1024, "wguA", bf=True)
        S_modA1 = newsem("modA1"); S_gt2A = newsem("gt2A"); S_xtmp = newsem("xtmpA")
        Ee_ring = Ring(1, 128, "Ee")
        tgA = Ring(1, CAP, "tgA"); tsgA = Ring(1, CAP, "tsgA"); tuA = Ring(2, CAP, "tuA"); tdA = Ring(2, 512, "tdA")
        bgc = smallc[:, C_BG:C_BG + 512]
        buc = smallc[:, C_BU:C_BU + 512]
        iota_f = smallc[:, C_IOTA:C_IOTA + CAP]
        pidx = smallc[:, C_PIDX:C_PIDX + 128]
        print("branch A arena top", top[0], "of", ARENA)
        add_ctr = [0]

        def add_any(out, in0, in1, reads, writes):
            add_ctr[0] += 1
            return tt("pool" if add_ctr[0] % 2 else "dve", out, in0, in1, ALU.add, reads, writes)

        for h in range(2):
            for j in range(8):
                i = h * 8 + j
                dma("sp", xn2_tok[:, j, :], xn2_d[i], S_xtok, writes=[B_xtok])
            GTh = Ye_raw[:, 0:1024]
            for j in range(8):
                i = h * 8 + j
                bk = nbank()
                trg([(PS[bk][0:32, 0:128], G_all[:, i, :], ident_f)], [B_Gall, B_smallc], [PB[bk]])
                act(GTh[0:32, j * 128:(j + 1) * 128], PS[bk][0:32, 0:128], AF.Copy, [PB[bk]], [B_Ye])
                bk = nbank()
                trg([(PS[bk][0:32, 0:128], posm_all[:, i, :], ident_f)], [B_posm, B_smallc], [PB[bk]])
                act(posmT[0:32, j * 128:(j + 1) * 128], PS[bk][0:32, 0:128], AF.Copy, [PB[bk]], [B_posmT])
            bd_raw = selT_raw
            for mp in range(2):
                dma("sp", bd_raw[0:32, 0:1024], bd_d[:, mp * 1024:(mp + 1) * 1024], S_modA1, writes=[B_selT])
                for j in range(8):
                    for mm_ in range(2):
                        m = mp * 2 + mm_
                        bk = nbank()
                        mmg([(PS[bk][:, :], GTh[0:32, j * 128:(j + 1) * 128], bd_raw[0:32, mm_ * 512:(mm_ + 1) * 512], True, True)], [B_Ye, B_selT], [PB[bk]])
                        copy_any(accA[j][:, m * 512:(m + 1) * 512], PS[bk][:, :], [PB[bk]], [B_accA[j]])
            P.barrier()
            units = [(e_, f_, t_) for e_ in range(NEXP) for f_ in range(16) for t_ in range(2)]
            uslot = {}
            dslot = {}
            PF = 3

            def issue_wguA(u):
                e_, f_, t_ = units[u]
                u_ap, u_b, u_s = wguA_ring.next()
                dma("pool", u_ap, wgu_d[e_ * 16 + f_][:, t_ * 2048:(t_ + 1) * 2048], u_s, writes=[u_b])
                uslot[u] = (u_ap, u_b)

            def issue_wdA(e_, m8):
                d_ap, d_b, d_s = wdA_ring.next()
                d3_ = d_ap.rearrange("p (f n) -> p f n", f=16)
                dma("pool", d3_, wd_d[e_].rearrange("(f p) d -> p f d", p=128)[:, :, m8 * 128:(m8 + 1) * 128], d_s, writes=[d_b])
                dslot[(e_, m8)] = (d3_, d_b)

            for u in range(PF):
                issue_wguA(u)
            for u2 in range(NEXP * 16):
                ex_i, f = u2 // 16, u2 % 16
                if f == 0:
                    for j in range(8):
                        ts("dve", selA[:, j, :], iota_f, posm_all[:, h * 8 + j, ex_i:ex_i + 1], None, ALU.is_equal, None, [B_smallc, B_posm], [B_sel])
                    for k in range(16):
                        bk = nbank()
                        mmg([(PS[bk][:, 0:CAP], xn2_tok[:, j, k * 128:(k + 1) * 128], selA[:, j, :], j == 0, j == 7) for j in range(8)],
                            [B_xtok, B_sel], [PB[bk]])
                        copy_any(XeT[:, k, :], PS[bk][:, 0:CAP], [PB[bk]], [B_XeT])
                    e_ap, e_b, _ = Ee_ring.next()
                    ts("dve", e_ap[0:32, :], pidx[0:32, :], float(ex_i), None, ALU.is_equal, None, [B_smallc], [e_b])
                    for c2 in range(2):
                        bk = nbank()
                        mmg([(PS[bk][:, :], e_ap[0:32, :], posmT[0:32, c2 * 512:(c2 + 1) * 512], True, True)], [e_b, B_posmT], [PB[bk]])
                        for i2 in range(NST):
                            ts("dve", selT[:, i2, c2 * 512:(c2 + 1) * 512], PS[bk][:, :], smallc[:, C_SIDX + i2:C_SIDX + i2 + 1], None, ALU.is_equal, None,
                               [PB[bk], B_smallc], [B_selT])
                if f in (2, 6, 10, 14):
                    issue_wdA(ex_i, (f - 2) // 4)
                banks = []
                for t_ in range(2):
                    u = u2 * 2 + t_
                    if u + PF < len(units):
                        issue_wguA(u + PF)
                    u_ap, u_b = uslot.pop(u)
                    u3 = u_ap.rearrange("p (k j) -> p k j", k=16)
                    bk = nbank()
                    mmg([(PS[bk][:, 0:CAP], u3[:, k, :], XeT[:, k, :], k == 0, k == 15) for k in range(16)], [u_b, B_XeT], [PB[bk]])
                    banks.append(bk)
                bG, bU = banks
                tg, tg_b, _ = tgA.next(); tsg, tsg_b, _ = tsgA.next(); tu, tu_b, _ = tuA.next()
                cb = ex_i * 16 + f
                ts("dve", tg, PS[bG][:, 0:CAP], bgc[:, cb:cb + 1], 7.0, ALU.add, ALU.min, [PB[bG], B_smallc], [tg_b])
                act(tsg, tg, AF.Sigmoid, [tg_b], [tsg_b], scale=1.702)
                act(tu, PS[bU][:, 0:CAP], AF.Identity, [PB[bU], B_smallc], [tu_b], bias=buc[:, cb:cb + 1])
                ts("dve", tu, tu, 7.0, -7.0, ALU.min, ALU.max, [tu_b], [tu_b])
                stt("dve", tu, tu, 1.0, tg, ALU.add, ALU.mult, [tu_b, tg_b], [tu_b])
                tt("pool", actA[:, f, :], tu, tsg, ALU.mult, [tu_b, tsg_b], [B_actA])
                if f != 15:
                    continue
                for m8 in range(16):
                    d3, d_b = dslot.pop((ex_i, m8))
                    for i2 in range(NST):
                        bk = nbank()
                        mmg([(PS[bk][:, 0:128], actA[:, ff, i2 * 128:(i2 + 1) * 128], d3[:, ff, :], ff == 0, ff == 15) for ff in range(16)], [B_actA, d_b], [PB[bk]])
                        copy_any(Ye[:, i2, m8 * 128:(m8 + 1) * 128], PS[bk][:, 0:128], [PB[bk]], [B_Ye])
                    if m8 + 4 < 16:
                        issue_wdA(ex_i, m8 + 4)
                for jt in range(8):
                    for m in range(4):
                        bk = nbank()
                        mmg([(PS[bk][:, :], selT[:, i2, jt * 128:(jt + 1) * 128], Ye[:, i2, m * 512:(m + 1) * 512], i2 == 0, i2 == NST - 1) for i2 in range(NST)],
                            [B_selT, B_Ye], [PB[bk]])
                        td, td_b, _ = tdA.next()
                        P.op("act", lambda e, td=td, src=PS[bk][:, :], sc_=G_all[:, h * 8 + jt, ex_i:ex_i + 1]: e.activation(out=td, in_=src, func=AF.Copy, scale=sc_),
                             [PB[bk], B_Gall], [td_b])
                        add_any(accA[jt][:, m * 512:(m + 1) * 512], accA[jt][:, m * 512:(m + 1) * 512], td, [td_b, B_accA[jt]], [B_accA[jt]])
            P.barrier()
            gfinA = Ye_raw[:, 0:2048]
            gt2A = XeT_raw[:, 0:2048]
            xtmp = actA_raw[:, 0:2048]
            B_xtmp = Buf("xtmpA")
            dma("sp", gfinA, gfin_d[:, :], S_modA1, writes=[B_Ye])
            dma("sp", gt2A, modb_d[5], S_gt2A, writes=[B_XeT])
            junkA = selT_raw.bitcast(BF16)[:, 0:2048]
            for j in range(8):
                r0 = (h * 8 + j) * 128
                dma("sp", xtmp, x1_d[r0:r0 + 128, :], S_xtmp, writes=[B_xtmp])
                tt("dve", accA[j], accA[j], gt2A, ALU.mult, [B_accA[j], B_XeT], [B_accA[j]])
                tt("pool", accA[j], accA[j], xtmp, ALU.add, [B_accA[j], B_xtmp], [B_accA[j]])
                act(junkA, accA[j], AF.Square, [B_accA[j]], [B_selT, B_ssq], accum_out=ssq[:, 8 + j:9 + j])
                rstd_chain(rs[:, 8 + j:9 + j], ssq[:, 8 + j:9 + j], 1.0 / D, [B_ssq], [B_rs])
                stt("dve", accA[j], accA[j], rs[:, 8 + j:9 + j], gfinA, ALU.mult, ALU.mult, [B_accA[j], B_rs, B_Ye], [B_accA[j]])
                dma("sp", out_d[r0:r0 + 128, :], accA[j], S_accA[j], reads=[B_accA[j]])
            P.barrier()
        P.final_wait("sp", S_accA)
        P.restore(snap)
        P.region = "B"

    if stage >= 3:
        top[0] = persist_top
        acc = [alloc(2048, f"acc{i}") for i in range(4)]
        B_accs = [Buf(f"acc{i}") for i in range(4)]
        S_acc = [newsem(f"acc{i}") for i in range(4)]
        xn2T = alloc_bf(16 * 512, "xn2T").rearrange("p (k n) -> p k n", k=16)
        B_xn2T = Buf("xn2T")
        actb = alloc_bf(16 * 512, "actb").rearrange("p (f n) -> p f n", f=16)
        B_actb = Buf("actb")
        wd_ring = Ring(3, 4096, "wd", bf=True)
        wgu_ring = Ring(4, 2048, "wgu", bf=True)
        mod0 = alloc(2048, "mod0"); mod1 = alloc(2048, "mod1")
        B_mod0 = Buf("mod0"); B_mod1 = Buf("mod1")
        S_mod0 = newsem("mod0"); S_mod1 = newsem("mod1"); S_bd = newsem("bd")
        bd_sb = alloc(2048, "bd")
        B_bd = Buf("bd")
        dma("sp", bd_sb[0:32, :], bd_d[:, :], S_bd, writes=[B_bd])
        G = alloc(128, "G").rearrange("p (i e) -> p i e", i=4)
        B_G = Buf("G")
        GT = alloc(512, "GT")
        B_GT = Buf("GT")
        lg = alloc(32, "lg"); ex = alloc(32, "ex"); mk = alloc(32, "mk"); m8 = alloc(8, "m8"); sm = alloc(4, "sm")
        B_lg = Buf("lg"); B_ex = Buf("ex"); B_mk = Buf("mk"); B_m8 = Buf("m8"); B_sm = Buf("sm")
        tmp_top = top[0]
        t1m = alloc(2048, "t1m"); B_t1m = Buf("t1m")
        xnbm = alloc_bf(2048, "xnbm"); B_xnbm = Buf("xnbm")
        junkm = alloc_bf(2048, "junkm"); B_junkm = Buf("junkm")
        top[0] = tmp_top
        tg_ring = Ring(2, 512, "tg"); tsg_ring = Ring(2, 512, "tsg"); tu_ring = Ring(2, 512, "tu"); td_ring = Ring(2, 512, "td")
        bgc = smallc[:, C_BG:C_BG + 512]
        buc = smallc[:, C_BU:C_BU + 512]
        brt = smallc[:, C_BRT:C_BRT + 32]
        out_sems = S_acc
        for q in range(nq):
            dma("sp", mod0, modb_d[4], S_mod0, writes=[B_mod0])
            dma("sp", mod1, modb_d[3], S_mod1, writes=[B_mod1])
            for i in range(4):
                r0 = (q * 4 + i) * 128
                dma("sp", acc[i], x1_d[r0:r0 + 128, :], S_acc[i], writes=[B_accs[i]])
                act(junkm, acc[i], AF.Square, [B_accs[i]], [B_junkm, B_ssq], accum_out=ssq[:, i:i + 1])
                rstd_chain(rs[:, i:i + 1], ssq[:, i:i + 1], 1.0 / D, [B_ssq], [B_rs])
                stt("dve", t1m, acc[i], rs[:, i:i + 1], mod0, ALU.mult, ALU.mult, [B_accs[i], B_rs, B_mod0], [B_t1m])
                tt("pool", xnbm, t1m, mod1, ALU.add, [B_t1m, B_mod1], [B_xnbm])
                for kb in range(4):
                    bk = nbank()
                    pv = PS[bk][:, :].bitcast(BF16)
                    trg([(pv[:, j * 128:(j + 1) * 128], xnbm[:, (kb * 4 + j) * 128:(kb * 4 + j + 1) * 128], ident_bf) for j in range(4)],
                        [B_xnbm, B_ident], [PB[bk]])
                    copy_any(xn2T[:, kb * 4:kb * 4 + 4, i * 128:(i + 1) * 128],
                             pv[:, 0:512].rearrange("p (k n) -> p k n", k=4), [PB[bk]], [B_xn2T])
                bk = nbank()
                wrt3 = wrt_bf.rearrange("p (k e) -> p k e", k=16)
                mmg([(PS[bk][:, 0:32], xn2T[:, k, i * 128:(i + 1) * 128], wrt3[:, k, :], k == 0, k == 15) for k in range(16)],
                    [B_xn2T, B_wrt], [PB[bk]])
                tt("dve", lg, PS[bk][:, 0:32], brt, ALU.add, [PB[bk], B_smallc], [B_lg])
                P.op("dve", lambda e: e.max(out=m8, in_=lg), [B_lg], [B_m8])
                ts("dve", sm[:, 0:1], m8[:, 0:1], -1.0, None, ALU.mult, None, [B_m8], [B_sm])
                act(ex, lg, AF.Exp, [B_lg, B_sm], [B_ex], bias=sm[:, 0:1])
                ts("dve", mk, lg, m8[:, 3:4], None, ALU.is_ge, None, [B_lg, B_m8], [B_mk])
                tt("dve", ex, ex, mk, ALU.mult, [B_ex, B_mk], [B_ex])
                P.op("dve", lambda e: e.tensor_reduce(out=sm[:, 1:2], in_=ex, axis=AX.X, op=ALU.add), [B_ex], [B_sm])
                P.op("dve", lambda e: e.reciprocal(out=sm[:, 2:3], in_=sm[:, 1:2]), [B_sm], [B_sm])
                ts("dve", G[:, i, :], ex, sm[:, 2:3], None, ALU.mult, None, [B_ex, B_sm], [B_G])
                bk = nbank()
                trg([(PS[bk][0:32, 0:128], G[:, i, :], ident_f)], [B_G, B_smallc], [PB[bk]])
                act(GT[0:32, i * 128:(i + 1) * 128], PS[bk][0:32, 0:128], AF.Copy, [PB[bk]], [B_GT])
            P.barrier()
            dma("sp", mod0, modb_d[5], S_mod0, writes=[B_mod0])
            dma("sp", mod1, gfin_d[:, :], S_mod1, writes=[B_mod1])
            for i in range(4):
                for m in range(4):
                    bk = nbank()
                    mmg([(PS[bk][:, :], GT[0:32, i * 128:(i + 1) * 128], bd_sb[0:32, m * 512:(m + 1) * 512], True, True)], [B_GT, B_bd], [PB[bk]])
                    td, td_b, _ = td_ring.next()
                    tt("dve", td, PS[bk][:, :], mod0[:, m * 512:(m + 1) * 512], ALU.mult, [PB[bk], B_mod0], [td_b])
                    tt("pool", acc[i][:, m * 512:(m + 1) * 512], acc[i][:, m * 512:(m + 1) * 512], td, ALU.add, [td_b, B_accs[i]], [B_accs[i]])
            units = [(e_, f_) for e_ in range(NEXP) for f_ in range(16)]
            uslot = {}
            dslot = {}
            PF = 3

            def issue_wgu(u):
                e_, f_ = units[u]
                u_ap, u_b, u_s = wgu_ring.next()
                dma("pool", u_ap, wgu_d[e_ * 16 + f_], u_s, writes=[u_b])
                uslot[u] = (u_ap, u_b)

            def issue_wd(e_, m_):
                d_ap, d_b, d_s = wd_ring.next()
                d3_ = d_ap.rearrange("p (f n) -> p f n", f=16)
                dma("pool", d3_, wd_d[e_].rearrange("(f p) d -> p f d", p=128)[:, :, m_ * 512:(m_ + 1) * 512], d_s, writes=[d_b])
                dslot[(e_, m_)] = (d3_, d_b)

            for u in range(PF):
                issue_wgu(u)
            for u, (ex_i, f) in enumerate(units):
                if u + PF < len(units):
                    issue_wgu(u + PF)
                if f in (3, 7, 11):
                    issue_wd(ex_i, (f - 3) // 4)
                u_ap, u_b = uslot.pop(u)
                u4 = u_ap.rearrange("p (t k j) -> p t k j", t=2, k=16)
                bG = nbank(); bU = nbank()
                mmg([(PS[bG][:, :], u4[:, 0, k, :], xn2T[:, k, :], k == 0, k == 15) for k in range(16)], [u_b, B_xn2T], [PB[bG]])
                mmg([(PS[bU][:, :], u4[:, 1, k, :], xn2T[:, k, :], k == 0, k == 15) for k in range(16)], [u_b, B_xn2T], [PB[bU]])
                tg, tg_b, _ = tg_ring.next(); tsg, tsg_b, _ = tsg_ring.next(); tu, tu_b, _ = tu_ring.next()
                cb = ex_i * 16 + f
                ts("dve", tg, PS[bG][:, :], bgc[:, cb:cb + 1], 7.0, ALU.add, ALU.min, [PB[bG], B_smallc], [tg_b])
                act(tsg, tg, AF.Sigmoid, [tg_b], [tsg_b], scale=1.702)
                act(tu, PS[bU][:, :], AF.Identity, [PB[bU], B_smallc], [tu_b], bias=buc[:, cb:cb + 1])
                ts("dve", tu, tu, 7.0, -7.0, ALU.min, ALU.max, [tu_b], [tu_b])
                stt("dve", tu, tu, 1.0, tg, ALU.add, ALU.mult, [tu_b, tg_b], [tu_b])
                tt("pool", actb[:, f, :], tu, tsg, ALU.mult, [tu_b, tsg_b], [B_actb])
                if f != 15:
                    continue
                for m in range(4):
                    d3, d_b = dslot.pop((ex_i, m))
                    for i in range(4):
                        bk = nbank()
                        mmg([(PS[bk][:, :], actb[:, ff, i * 128:(i + 1) * 128], d3[:, ff, :], ff == 0, ff == 15) for ff in range(16)], [B_actb, d_b], [PB[bk]])
                        td, td_b, _ = td_ring.next()
                        stt("dve", td, PS[bk][:, :], G[:, i, ex_i:ex_i + 1], mod0[:, m * 512:(m + 1) * 512], ALU.mult, ALU.mult, [PB[bk], B_G, B_mod0], [td_b])
                        tt("pool", acc[i][:, m * 512:(m + 1) * 512], acc[i][:, m * 512:(m + 1) * 512], td, ALU.add, [td_b, B_accs[i]], [B_accs[i]])
                    if m == 0:
                        issue_wd(ex_i, 3)
            P.barrier()
            for i in range(4):
                r0 = (q * 4 + i) * 128
                act(junkm, acc[i], AF.Square, [B_accs[i]], [B_junkm, B_ssq], accum_out=ssq[:, 8 + i:9 + i])
                rstd_chain(rs[:, 8 + i:9 + i], ssq[:, 8 + i:9 + i], 1.0 / D, [B_ssq], [B_rs])
                stt("dve", acc[i], acc[i], rs[:, 8 + i:9 + i], mod1, ALU.mult, ALU.mult, [B_accs[i], B_rs, B_mod1], [B_accs[i]])
                dma("sp", out_d[r0:r0 + 128, :], acc[i], S_acc[i], reads=[B_accs[i]])
            P.barrier()
    P.final_wait("sp", out_sems)
    with nc.Block() as block:
        P.emit(block, branched=branched)
    es.close()
    return nc


_NC_CACHE = {}


def _prep_shared(inp, stage):
    f = np.float32
    sh = {}
    sh["w_mod"] = np.ascontiguousarray(inp["w_mod"][0], dtype=f)
    sh["b_mod"] = np.ascontiguousarray(inp["b_mod"][0].reshape(1, -1), dtype=f)
    sh["g_mix_b"] = np.ascontiguousarray(np.broadcast_to(inp["g_mix"][0], (128, D)), dtype=f)
    sh["g_ffn_b"] = np.ascontiguousarray(np.broadcast_to(inp["g_ffn"][0], (128, D)), dtype=f)
    sh["g_fin_b"] = np.ascontiguousarray(np.broadcast_to(inp["g_final"], (128, D)), dtype=f)
    w_in = np.asarray(inp["w_in"][0], dtype=f)
    sh["w_in_h"] = np.ascontiguousarray(w_in.reshape(16, 128, 40, 128).transpose(2, 1, 0, 3)).reshape(40, 128, 2048)
    sh["wr_h"] = np.ascontiguousarray(np.asarray(inp["lru_w_r"][0], dtype=f).transpose(2, 0, 1, 3)).reshape(128, 2048)
    sh["wi_h"] = np.ascontiguousarray(np.asarray(inp["lru_w_i"][0], dtype=f).transpose(2, 0, 1, 3)).reshape(128, 2048)
    sh["w_out"] = np.ascontiguousarray(inp["w_out"][0], dtype=f)
    if stage >= 3:
        sh["w_router_h"] = np.ascontiguousarray(np.asarray(inp["w_router"][0], dtype=f).reshape(16, 128, 32).transpose(1, 0, 2)).reshape(128, 512)
        wg = np.asarray(inp["w_gate"][0], dtype=f).reshape(NEXP, 16, 128, 16, 128)
        wu = np.asarray(inp["w_up"][0], dtype=f).reshape(NEXP, 16, 128, 16, 128)
        wgu = np.empty((NEXP, 16, 128, 2, 16, 128), dtype=f)
        wgu[:, :, :, 0] = wg.transpose(0, 3, 2, 1, 4)
        wgu[:, :, :, 1] = wu.transpose(0, 3, 2, 1, 4)
        sh["wgu_h"] = wgu.reshape(NEXP * 16, 128, 4096)
        sh["w_down"] = np.ascontiguousarray(inp["w_down"][0], dtype=f)
        sh["b_down"] = np.ascontiguousarray(inp["b_down"][0], dtype=f)
    return sh


def _prep_core(inp, k):
    f = np.float32
    b, j = k // 4, k % 4
    x = np.asarray(inp["x"], dtype=f)
    ctx = np.asarray(inp["ctx"], dtype=f)
    S = x.shape[1]
    x_all = np.zeros((XROWS, D), dtype=f)
    x_all[HALO:HALO + LC] = ctx[b]
    others = [jj for jj in range(4) if jj != j]
    chunks_ = others + [j]
    hm = np.zeros(10, dtype=f)
    for wi, jj in enumerate(chunks_):
        w = wi + 1
        t0 = jj * L - HALO
        lo, hi = max(t0, 0), min(t0 + W, S)
        r0 = WIN_ROW[w] + (lo - t0)
        x_all[r0:r0 + (hi - lo)] = x[b, lo:hi]
        hm[2 * w] = 1.0 if jj > 0 else 0.0
        hm[2 * w + 1] = 1.0 if jj < 3 else 0.0
    cm = np.zeros(6, dtype=f)
    for o, jj in enumerate(others):
        cm[o] = 1.0 if jj < j else 0.0
        cm[3 + o] = 1.0 - cm[o]
    sc = np.zeros((128, NS), dtype=f)

    def colT(v, n):
        return np.asarray(v, dtype=f).reshape(n, 128).T

    sc[:, C_CT:C_CT + 16] = colT(inp["c"][b], 16)
    sc[:, C_CX:C_CX + 16] = colT(inp["c_ctx"], 16)
    caw = np.asarray(inp["conv_a_w"][0], dtype=f)
    sc[:, C_CAW:C_CAW + 32] = caw.reshape(4, 8, 128).transpose(2, 1, 0).reshape(128, 32)
    sc[:, C_CAB:C_CAB + 8] = colT(inp["conv_a_b"][0], 8)
    sc[:, C_LBR:C_LBR + 16] = colT(np.asarray(inp["lru_b_r"][0]).reshape(-1), 16)
    sc[:, C_LBI:C_LBI + 16] = colT(np.asarray(inp["lru_b_i"][0]).reshape(-1), 16)
    sc[:, C_LAM:C_LAM + 16] = colT(np.asarray(inp["lru_lam"][0]).reshape(-1), 16)
    cbw = np.asarray(inp["conv_b_w"][0], dtype=f)
    sc[:, C_CBW:C_CBW + 24] = cbw.reshape(3, 8, 128).transpose(2, 1, 0).reshape(128, 24)
    sc[:, C_GOA:C_GOA + 8] = colT(inp["g_out_a"][0], 8)
    sc[:, C_GOB:C_GOB + 8] = colT(inp["g_out_b"][0], 8)
    sc[:, C_HM:C_HM + 10] = hm[None, :]
    sc[:, C_CM:C_CM + 6] = cm[None, :]
    sc[:, C_BG:C_BG + 512] = np.asarray(inp["b_gate"][0], dtype=f).reshape(NEXP, 16, 128).transpose(2, 0, 1).reshape(128, 512)
    sc[:, C_BU:C_BU + 512] = np.asarray(inp["b_up"][0], dtype=f).reshape(NEXP, 16, 128).transpose(2, 0, 1).reshape(128, 512)
    sc[:, C_BRT:C_BRT + 32] = np.asarray(inp["b_router"][0], dtype=f)[None, :]
    sc[:, C_ID:C_ID + 128] = np.eye(128, dtype=f)
    sc[:, C_ONE:C_ONE + 128] = 1.0
    pp = np.arange(128, dtype=f)
    sc[:, C_TRI:C_TRI + 128] = (pp[:, None] < pp[None, :]).astype(f)
    sc[:, C_PIDX:C_PIDX + 128] = pp[:, None]
    sc[:, C_IOTA:C_IOTA + CAP] = np.arange(CAP, dtype=f)[None, :]
    sc[:, C_SIDX:C_SIDX + NST] = pp[:, None] + (128.0 * np.arange(NST, dtype=f))[None, :]
    return {"x_all": x_all, "smallc": sc}


def run(inputs, stage=3, stop=None):
    if (stage, stop) not in _NC_CACHE:
        _NC_CACHE[(stage, stop)] = build(stage, stop)
    nc = _NC_CACHE[(stage, stop)]
    shared = _prep_shared(inputs, stage)
    in_maps = []
    for k in range(NCORE):
        m = dict(shared)
        m.update(_prep_core(inputs, k))
        in_maps.append(m)
    res = run_bass_kernel_spmd(nc, in_maps, core_ids=list(range(NCORE)))
    outs = [np.asarray(r["out"]) for r in res.results]
    return np.concatenate(outs, axis=0).reshape(2, 4 * L, D).astype(np.float32)


def kernel(**inputs):
    return run(inputs, stage=3)
```

```python
import numpy as np
from contextlib import ExitStack
import concourse.bass as bass
import concourse.mybir as mybir
from concourse.bass_utils import run_bass_kernel_spmd

F32 = mybir.dt.float32
BF16 = mybir.dt.bfloat16
AF = mybir.ActivationFunctionType
ALU = mybir.AluOpType
AX = mybir.AxisListType

D = 2048
L = 2048
HALO = 64
W = L + 2 * HALO
LC = 256
WC = LC + 2 * HALO
NCORE = 8
EPS = 1e-6
NEXP = 32
WIN_L = [LC, L, L, L, L]
WIN_ROW = [0, WC, WC + W, WC + 2 * W, WC + 3 * W]
XROWS = WC + 4 * W

_o = 0
def _col(n):
    global _o
    r = _o
    _o += n
    return r
C_CT = _col(16); C_CX = _col(16); C_CAW = _col(32); C_CAB = _col(8); C_LBR = _col(16); C_LBI = _col(16)
C_LAM = _col(16); C_CBW = _col(24); C_GOA = _col(8); C_GOB = _col(8); C_HM = _col(10); C_CM = _col(6)
C_BG = _col(512); C_BU = _col(512); C_BRT = _col(32); C_ID = _col(128); C_ONE = _col(128)
C_TRI = _col(128); C_PIDX = _col(128); C_IOTA = _col(384); C_SIDX = _col(3)
NS = _o
CAP = 384
NST = CAP // 128

ENGS = ("pe", "act", "dve", "pool", "sp")
SAME_ENGINE_SYNC = True


ALLBUFS = []
ALLRINGS = []


class Buf:
    __slots__ = ("name", "w", "r")

    def __init__(self, name=""):
        self.name = name
        self.w = None
        self.r = {}
        ALLBUFS.append(self)


class Sem:
    def __init__(self, h, name):
        self.h = h
        self.n = 0
        self.name = name


class Prog:
    def __init__(self):
        self.opsr = {r: {e: [] for e in ENGS} for r in ("pre", "A", "B")}
        self.region = "pre"
        self.regs = {}
        self.esem = {}
        self.waited = {e: {} for e in ENGS}
        self.pending = {e: [] for e in ENGS}
        self.allsems = []

    def op(self, eng, fn, reads=(), writes=(), dma=None):
        deps = self.pending[eng]
        self.pending[eng] = []
        for b in reads:
            if b.w is not None:
                deps.append(b.w)
        for b in writes:
            if b.w is not None:
                deps.append(b.w)
            deps.extend(b.r.values())
        wd = self.waited[eng]
        best = {}
        own = self.esem.get(eng)
        for (s, v) in deps:
            if (not SAME_ENGINE_SYNC) and s is own:
                continue
            if wd.get(id(s), 0) >= v:
                continue
            if id(s) not in best or best[id(s)][1] < v:
                best[id(s)] = (s, v)
        waits = []
        for s, v in best.values():
            waits.append((s, v))
            wd[id(s)] = v
        if dma is not None:
            dma.n += 16
            ev = (dma, dma.n)
            inc = (dma, 16)
        else:
            s = self.esem[eng]
            s.n += 1
            ev = (s, s.n)
            inc = (s, 1)
        for b in reads:
            old = b.r.get(id(ev[0]))
            if old is None or old[1] < ev[1]:
                b.r[id(ev[0])] = ev
        for b in writes:
            b.w = ev
            b.r = {}
        self.opsr[self.region][eng].append((waits, fn, inc))
        return ev

    def barrier(self):
        for e in ENGS:
            self.pending[e] = [(s, s.n) for s in self.allsems if s.n > 0]

    def final_wait(self, eng, sems):
        waits = [(s, s.n) for s in sems if s.n > 0]
        self.opsr[self.region][eng].append((waits, None, None))

    def snapshot(self, counters):
        return dict(bufs=[(b, b.w, dict(b.r)) for b in ALLBUFS], sems=[(s, s.n) for s in self.allsems],
                    waited={e: dict(self.waited[e]) for e in ENGS}, pending={e: list(self.pending[e]) for e in ENGS},
                    rings=[(r, r.i) for r in ALLRINGS], counters=[(c, c[0]) for c in counters], nsems=len(self.allsems))

    def restore(self, st):
        for b, w, r in st["bufs"]:
            b.w = w
            b.r = dict(r)
        for s_, n in st["sems"]:
            s_.n = n
        del self.allsems[st["nsems"]:]
        self.waited = {e: dict(st["waited"][e]) for e in ENGS}
        self.pending = {e: list(st["pending"][e]) for e in ENGS}
        for r, i in st["rings"]:
            r.i = i
        for c, v in st["counters"]:
            c[0] = v

    def emit(self, block, branched=False):
        def mk(name):
            def body(eng):
                def run(lst):
                    for waits, fn, inc in lst:
                        for s, v in waits:
                            eng.wait_ge(s.h, v)
                        if fn is not None:
                            ins = fn(eng)
                            ins.then_inc(inc[0].h, inc[1])
                if not branched:
                    run(self.opsr["pre"][name])
                    return
                reg = eng.alloc_register(f"flag_{name}")
                self.regs[name] = reg
                run(self.opsr["pre"][name])
                with eng.If(eng.snap(reg) > 0):
                    run(self.opsr["B"][name])
                with eng.Else():
                    run(self.opsr["A"][name])
            return body
        block.sync(mk("sp"))
        block.tensor(mk("pe"))
        block.scalar(mk("act"))
        block.vector(mk("dve"))
        block.gpsimd(mk("pool"))


def rev(ap):
    a = ap.ap
    assert len(a) == 2 and a[1][0] == 1, a
    n = a[1][1]
    return bass.AP(ap.tensor, ap.offset + (n - 1), [list(a[0]), [-1, n]])


def chunks(lo, hi, step=512):
    out = []
    while lo < hi:
        out.append((lo, min(lo + step, hi)))
        lo += step
    return out


def build(stage=3, stop=None, nq=4, sparse=True):
    del ALLBUFS[:]
    del ALLRINGS[:]
    nc = bass.Bass("TRN2", target_bir_lowering=False)
    P = Prog()
    es = ExitStack()

    def din(name, shape, dt=F32):
        return nc.dram_tensor(name, list(shape), dt, kind="ExternalInput").ap()

    x_all = din("x_all", [XROWS, D])
    smallc_d = din("smallc", [128, NS])
    wmod_d = din("w_mod", [D, 6 * D])
    bmod_d = din("b_mod", [1, 6 * D])
    gmix_d = din("g_mix_b", [128, D])
    gffn_d = din("g_ffn_b", [128, D])
    gfin_d = din("g_fin_b", [128, D])
    win_d = din("w_in_h", [40, 128, 16 * 128])
    wr_d = din("wr_h", [128, 16 * 128])
    wi_d = din("wi_h", [128, 16 * 128])
    wout_d = din("w_out", [D, D])
    if stage >= 3:
        wrt_d = din("w_router_h", [128, 16 * 32])
        wgu_d = din("wgu_h", [NEXP * 16, 128, 2 * 16 * 128])
        wd_d = din("w_down", [NEXP, D, D])
        bd_d = din("b_down", [NEXP, D])
    out_d = nc.dram_tensor("out", [L, D], F32, kind="ExternalOutput").ap()
    modb_d = nc.dram_tensor("modb", [8, 128, D], F32).ap()
    y_d = nc.dram_tensor("y_scr", [16, 128, L], BF16).ap()
    x1_d = nc.dram_tensor("x1_scr", [L, D], F32).ap()

    ARENA = 53200
    arena = es.enter_context(nc.sbuf_tensor("arena", [128, ARENA], F32))
    PS = [es.enter_context(nc.psum_tensor(f"ps{i}", [128, 512], F32)) for i in range(8)]
    PB = [Buf(f"ps{i}") for i in range(8)]
    for e in ENGS:
        P.esem[e] = Sem(es.enter_context(nc.semaphore(f"s_{e}")), e)
        P.allsems.append(P.esem[e])
    nsem = [0]

    def newsem(name):
        nsem[0] += 1
        s = Sem(es.enter_context(nc.semaphore(f"d{nsem[0]}_{name}")), name)
        P.allsems.append(s)
        return s

    bank_ctr = [0]

    def nbank():
        b = bank_ctr[0] % 8
        bank_ctr[0] += 1
        return b

    top = [0]

    def alloc(n_f32, name=""):
        a = top[0]
        top[0] += n_f32
        assert top[0] <= ARENA, (name, top[0])
        return arena[:, a:a + n_f32]

    def alloc_bf(n_bf, name=""):
        assert n_bf % 2 == 0
        return alloc(n_bf // 2, name).bitcast(BF16)

    def dma(q, out, in_, sem, reads=(), writes=()):
        return P.op(q, lambda e: e.dma_start(out=out, in_=in_), reads, writes, dma=sem)

    def act(out, in_, func, reads, writes, bias=None, scale=None, accum_out=None):
        kw = {}
        if bias is not None:
            kw["bias"] = bias
        if scale is not None:
            kw["scale"] = scale
        if accum_out is not None:
            kw["accum_out"] = accum_out
        return P.op("act", lambda e: e.activation(out=out, in_=in_, func=func, **kw), reads, writes)

    def ts(eng, out, in0, s1, s2, op0, op1, reads, writes):
        if s2 is None:
            return P.op(eng, lambda e: e.tensor_scalar(out=out, in0=in0, scalar1=s1, scalar2=None, op0=op0), reads, writes)
        return P.op(eng, lambda e: e.tensor_scalar(out=out, in0=in0, scalar1=s1, scalar2=s2, op0=op0, op1=op1), reads, writes)

    def tt(eng, out, in0, in1, op, reads, writes):
        return P.op(eng, lambda e: e.tensor_tensor(out=out, in0=in0, in1=in1, op=op), reads, writes)

    def stt(eng, out, in0, scalar, in1, op0, op1, reads, writes):
        return P.op(eng, lambda e: e.scalar_tensor_tensor(out=out, in0=in0, scalar=scalar, in1=in1, op0=op0, op1=op1), reads, writes)

    def mmg(items, reads, writes):
        def fn(e):
            ins = None
            for (o, l, r, s, t) in items:
                ins = e.matmul(o, l, r, start=s, stop=t)
            return ins
        return P.op("pe", fn, reads, writes)

    def trg(items, reads, writes):
        def fn(e):
            ins = None
            for (o, i_, idn) in items:
                ins = e.transpose(o, i_, idn)
            return ins
        return P.op("pe", fn, reads, writes)

    class Ring:
        def __init__(self, n, n_f32, name, bf=False, shape=None):
            self.slots = []
            for i in range(n):
                ap = alloc(n_f32, name)
                if bf:
                    ap = ap.bitcast(BF16)
                self.slots.append((ap, Buf(f"{name}{i}"), newsem(f"{name}{i}")))
            self.i = 0
            ALLRINGS.append(self)

        def next(self):
            s = self.slots[self.i % len(self.slots)]
            self.i += 1
            return s

    def finish_early():
        S_e = newsem("early")
        P.barrier()
        dma("sp", out_d[0:128, :], arena[:, 0:2048], S_e)
        P.final_wait("sp", [S_e])
        with nc.Block() as block:
            P.emit(block)
        es.close()
        return nc

    smallc = alloc(NS, "smallc")
    B_smallc = Buf("smallc")
    S_const = newsem("const")
    dma("sp", smallc, smallc_d[:, :], S_const, writes=[B_smallc])
    wr_bf = alloc_bf(2048, "wr")
    wi_bf = alloc_bf(2048, "wi")
    B_wri = Buf("wri")
    S_wri = newsem("wri")
    dma("pool", wr_bf, wr_d[:, :], S_wri, writes=[B_wri])
    dma("pool", wi_bf, wi_d[:, :], S_wri, writes=[B_wri])
    if stage >= 3:
        wrt_bf = alloc_bf(512, "wrt")
        B_wrt = Buf("wrt")
        S_wrt = newsem("wrt")
        dma("pool", wrt_bf, wrt_d[:, :], S_wrt, writes=[B_wrt])
    ident_bf = alloc_bf(128, "identbf")
    B_ident = Buf("ident")
    smalld = alloc(512, "smalld")
    ident_f = smallc[:, C_ID:C_ID + 128]
    ones_f = smallc[:, C_ONE:C_ONE + 128]
    P.op("act", lambda e: e.activation(out=ident_bf, in_=ident_f, func=AF.Copy), [B_smallc], [B_ident])
    SD_SC = 0; SD_SX = 16; SD_CNEG = 32; SD_CARRY = 48; SD_RSTD = 64; SD_SUMH = 128; SD_SUMA = 208; SD_TMP = 288
    B_sc = Buf("sc"); B_cneg = Buf("cneg"); B_carry = Buf("carry"); B_rstd = Buf("rstdab")
    B_sum = [Buf(f"sum{w}") for w in range(5)]
    sc = smalld[:, SD_SC:SD_SC + 16]
    sx = smalld[:, SD_SX:SD_SX + 16]
    cneg = smalld[:, SD_CNEG:SD_CNEG + 16]
    carry = smalld[:, SD_CARRY:SD_CARRY + 16]
    act(sc, smallc[:, C_CT:C_CT + 16], AF.Silu, [B_smallc], [B_sc])
    act(sx, smallc[:, C_CX:C_CX + 16], AF.Silu, [B_smallc], [B_sc])
    act(cneg, smallc[:, C_LAM:C_LAM + 16], AF.Exp, [B_smallc], [B_cneg], scale=-1.0)
    act(cneg, cneg, AF.Ln, [B_cneg], [B_cneg], bias=1.0)
    ts("dve", cneg, cneg, -8.0, None, ALU.mult, None, [B_cneg], [B_cneg])
    persist_top = top[0]

    cb_l = alloc(2048, "cb_l").rearrange("p (k m) -> p k m", k=16)
    cx_l = alloc(2048, "cx_l").rearrange("p (k m) -> p k m", k=16)
    B_cbl = Buf("cbl")
    for k in range(16):
        ts("dve", cb_l[:, k, :], ones_f, sc[:, k:k + 1], None, ALU.mult, None, [B_smallc, B_sc], [B_cbl])
        ts("dve", cx_l[:, k, :], ones_f, sx[:, k:k + 1], None, ALU.mult, None, [B_smallc, B_sc], [B_cbl])
    wm_ring = Ring(3, 2048, "wm")
    ev_ring = Ring(2, 2048, "ev")
    bm_ring = Ring(2, 2048, "bm")
    gbuf = alloc(2048, "gbuf")
    B_gbuf = Buf("gbuf")
    S_g = newsem("gload")
    for g in range(6):
        use_ctx = g < 2
        bm_ap, bm_b, bm_s = bm_ring.next()
        dma("sp", bm_ap[0:1, :], bmod_d[0:1, g * D:(g + 1) * D], bm_s, writes=[bm_b])
        if g in (1, 4):
            dma("sp", gbuf, (gmix_d if g == 1 else gffn_d)[:, :], S_g, writes=[B_gbuf])
        banks = [nbank() for _ in range(4)]
        xbanks = [nbank() for _ in range(4)] if use_ctx else []
        for k in range(16):
            w_ap, w_b, w_s = wm_ring.next()
            dma("sp", w_ap, wmod_d[k * 128:(k + 1) * 128, g * D:(g + 1) * D], w_s, writes=[w_b])
            for n in range(4):
                mmg([(PS[banks[n]][:, :], cb_l[:, k, :], w_ap[:, n * 512:(n + 1) * 512], k == 0, False)],
                    [w_b, B_cbl], [PB[banks[n]]])
                if use_ctx:
                    mmg([(PS[xbanks[n]][:, :], cx_l[:, k, :], w_ap[:, n * 512:(n + 1) * 512], k == 0, False)],
                        [w_b, B_cbl], [PB[xbanks[n]]])
        for n in range(4):
            mmg([(PS[banks[n]][:, :], ones_f[0:1, :], bm_ap[0:1, n * 512:(n + 1) * 512], False, True)],
                [bm_b, B_smallc], [PB[banks[n]]])
            if use_ctx:
                mmg([(PS[xbanks[n]][:, :], ones_f[0:1, :], bm_ap[0:1, n * 512:(n + 1) * 512], False, True)],
                    [bm_b, B_smallc], [PB[xbanks[n]]])
        for (bks, idx) in ((banks, g), (xbanks, 6 + g)):
            if not bks:
                continue
            e_ap, e_b, e_s = ev_ring.next()
            for n in range(4):
                dst = e_ap[:, n * 512:(n + 1) * 512]
                if g in (1, 4):
                    stt("dve", dst, PS[bks[n]][:, :], 1.0, gbuf[:, n * 512:(n + 1) * 512], ALU.add, ALU.mult,
                        [PB[bks[n]], B_gbuf], [e_b])
                else:
                    act(dst, PS[bks[n]][:, :], AF.Copy, [PB[bks[n]]], [e_b])
            dma("sp", modb_d[idx], e_ap, e_s, reads=[e_b], writes=[])
    P.barrier()
    if stop == "p0":
        return finish_early()
    top[0] = persist_top
    xnT = alloc_bf(16 * W, "xnT").rearrange("p (k n) -> p k n", k=16)
    B_xnT = Buf("xnT")
    gs_b = alloc(2048, "gs_b")
    sh_b = alloc(2048, "sh_b")
    B_gs = Buf("gs")
    S_gs = newsem("gs")
    w_ring = Ring(3, 1024, "wring", bf=True)
    acc_a = alloc(2048, "acc_a")
    acc_b = alloc(2048, "acc_b")
    B_acca = Buf("acca"); B_accb = Buf("accb")
    ph1_top = top[0]
    xs_ring = Ring(2, 2048, "xs")
    t1 = alloc(2048, "t1"); B_t1 = Buf("t1")
    xnb_ring = Ring(2, 1024, "xnb", bf=True)
    junk = alloc_bf(2048, "junk"); B_junk = Buf("junk")
    top[0] = ph1_top
    T0 = alloc(W, "T0"); T1 = alloc(L, "T1"); Ta = alloc(L, "Ta"); Tb = alloc(L, "Tb")
    TsBig = alloc(2 * L, "TsBig"); Ts = TsBig[:, 0:L]; Ts2 = TsBig[:, L:2 * L]; Tz = TsBig[:, 0:W]
    xc_bf = alloc_bf(L, "xcbf")
    yb_ring = Ring(2, 1024, "ybf", bf=True)
    B_T0 = Buf("T0"); B_T1 = Buf("T1"); B_Ta = Buf("Ta"); B_Tb = Buf("Tb"); B_Ts = Buf("Ts"); B_Ts2 = Buf("Ts2"); B_xcbf = Buf("xcbf")
    B_ssq = Buf("ssq")
    ssq = smalld[:, SD_TMP:SD_TMP + 32]
    rs = smalld[:, SD_TMP + 32:SD_TMP + 64]
    B_rs = Buf("rs")
    cp_ctr = [0]

    def copy_any(out, in_, reads, writes):
        cp_ctr[0] += 1
        if cp_ctr[0] % 2:
            return act(out, in_, AF.Copy, reads, writes)
        return P.op("dve", lambda e: e.tensor_copy(out=out, in_=in_), reads, writes)

    def rstd_chain(dst, src, scale, rb, wb):
        ts("dve", dst, src, scale, EPS, ALU.mult, ALU.add, rb, wb)
        act(dst, dst, AF.Sqrt, wb, wb)
        P.op("dve", lambda e: e.reciprocal(out=dst, in_=dst), wb, wb)

    def build_xnT(row0, ntile, dstT, B_dst, g_ap, s_ap, B_g):
        for i in range(ntile):
            x_ap, x_b, x_s = xs_ring.next()
            if isinstance(row0, tuple):
                src = row0[0][row0[1] + i * 128: row0[1] + (i + 1) * 128, :]
            else:
                src = x_all[row0 + i * 128: row0 + (i + 1) * 128, :]
            dma("sp", x_ap, src, x_s, writes=[x_b])
            col = i % 32
            act(junk, x_ap, AF.Square, [x_b], [B_junk, B_ssq], accum_out=ssq[:, col:col + 1])
            rstd_chain(rs[:, col:col + 1], ssq[:, col:col + 1], 1.0 / D, [B_ssq], [B_rs])
            stt("dve", t1, x_ap, rs[:, col:col + 1], g_ap, ALU.mult, ALU.mult, [x_b, B_rs, B_g], [B_t1])
            n_ap, n_b, _ = xnb_ring.next()
            tt("pool", n_ap, t1, s_ap, ALU.add, [B_t1, B_g], [n_b])
            for kb in range(4):
                bk = nbank()
                pv = PS[bk][:, :].bitcast(BF16)
                trg([(pv[:, j * 128:(j + 1) * 128], n_ap[:, (kb * 4 + j) * 128:(kb * 4 + j + 1) * 128], ident_bf) for j in range(4)],
                    [n_b, B_ident], [PB[bk]])
                copy_any(dstT[:, kb * 4:kb * 4 + 4, i * 128:(i + 1) * 128],
                         pv[:, 0:512].rearrange("p (k n) -> p k n", k=4), [PB[bk]], [B_dst])

    def load_w(ct):
        w_ap, w_b, w_s = w_ring.next()
        dma("pool", w_ap, win_d[ct], w_s, writes=[w_b])
        return w_ap.rearrange("p (k j) -> p k j", k=16), w_b

    def inproj(w3, w_b, c0, c1, evac):
        for (a, b) in chunks(c0, c1):
            bk = nbank()
            mmg([(PS[bk][:, 0:b - a], w3[:, k, :], xnT[:, k, a:b], k == 0, k == 15) for k in range(16)],
                [w_b, B_xnT], [PB[bk]])
            evac(PS[bk][:, 0:b - a], PB[bk], a, b)

    def colp(base, idx):
        return smallc[:, base + idx: base + idx + 1]

    sumH = smalld[:, SD_SUMH:SD_SUMH + 80]
    sumA = smalld[:, SD_SUMA:SD_SUMA + 80]
    S_y = newsem("ystore")

    for w in range(5):
        Lw = WIN_L[w]
        Ww = Lw + 2 * HALO
        mine = (w == 4)
        if w == 0:
            dma("sp", gs_b, modb_d[7], S_gs, writes=[B_gs])
            dma("sp", sh_b, modb_d[6], S_gs, writes=[B_gs])
        if w == 1:
            dma("sp", gs_b, modb_d[1], S_gs, writes=[B_gs])
            dma("sp", sh_b, modb_d[0], S_gs, writes=[B_gs])
        build_xnT(WIN_ROW[w], Ww // 128, xnT, B_xnT, gs_b, sh_b, B_gs)
        P.barrier()
        if stop == f"x{w}":
            return finish_early()
        if mine:
            tmpc = smalld[:, SD_TMP + 64:SD_TMP + 80]
            B_tc = Buf("tmpc")
            P.op("dve", lambda e: e.tensor_copy(out=carry, in_=sumH[:, 0:16]), [B_sum[0]], [B_carry])
            for (lo, order, mbase) in ((0, (1, 2, 3), 0), (8, (3, 2, 1), 3)):
                for wo in order:
                    cs = carry[:, lo:lo + 8]
                    tcs = tmpc[:, lo:lo + 8]
                    tt("dve", tcs, sumA[:, wo * 16 + lo: wo * 16 + lo + 8], cs, ALU.mult, [B_sum[wo], B_carry], [B_tc])
                    tt("dve", tcs, tcs, sumH[:, wo * 16 + lo: wo * 16 + lo + 8], ALU.add, [B_sum[wo], B_tc], [B_tc])
                    tt("dve", tcs, tcs, cs, ALU.subtract, [B_tc, B_carry], [B_tc])
                    stt("dve", cs, tcs, colp(C_CM, mbase + wo - 1), cs, ALU.mult, ALU.add, [B_tc, B_carry, B_smallc], [B_carry])
            P.op("pool", lambda e: e.memset(acc_a, 0.0), [], [B_acca])
            P.op("pool", lambda e: e.memset(acc_b, 0.0), [], [B_accb])
        hm_l = colp(C_HM, 2 * w)
        hm_r = colp(C_HM, 2 * w + 1)
        for c in range(8):
            w3, w_b = load_w(8 + c)
            inproj(w3, w_b, 0, Ww, lambda ps, pb, a, b: act(T0[:, a:b], ps, AF.Copy, [pb], [B_T0]))
            ts("dve", T0[:, 0:HALO], T0[:, 0:HALO], hm_l, None, ALU.mult, None, [B_T0, B_smallc], [B_T0])
            ts("dve", T0[:, HALO + Lw:Ww], T0[:, HALO + Lw:Ww], hm_r, None, ALU.mult, None, [B_T0, B_smallc], [B_T0])
            xc = T1[:, 0:Lw]
            ts("dve", xc, T0[:, 62:62 + Lw], colp(C_CAW, c * 4 + 0), colp(C_CAB, c), ALU.mult, ALU.add, [B_T0, B_smallc], [B_T1])
            for tap in (1, 2, 3):
                stt("dve", xc, T0[:, 62 + tap:62 + tap + Lw], colp(C_CAW, c * 4 + tap), xc, ALU.mult, ALU.add, [B_T0, B_T1, B_smallc], [B_T1])
            act(xc_bf[:, 0:Lw], xc, AF.Copy, [B_T1], [B_xcbf])
            for d in range(2):
                dc = d * 8 + c
                hbuf, B_h = (Ts, B_Ts) if d == 0 else (Ts2, B_Ts2)
                for (a, b) in chunks(0, Lw):
                    bk = nbank()
                    mmg([(PS[bk][:, 0:b - a], wr_bf[:, dc * 128:(dc + 1) * 128], xc_bf[:, a:b], True, True)], [B_wri, B_xcbf], [PB[bk]])
                    act(Ta[:, a:b], PS[bk][:, 0:b - a], AF.Sigmoid, [PB[bk], B_smallc], [B_Ta], bias=colp(C_LBR, dc))
                    bk = nbank()
                    mmg([(PS[bk][:, 0:b - a], wi_bf[:, dc * 128:(dc + 1) * 128], xc_bf[:, a:b], True, True)], [B_wri, B_xcbf], [PB[bk]])
                    act(Tb[:, a:b], PS[bk][:, 0:b - a], AF.Sigmoid, [PB[bk], B_smallc], [B_Tb], bias=colp(C_LBI, dc))
                av = Ta[:, 0:Lw]; bv = Tb[:, 0:Lw]; hv = hbuf[:, 0:Lw]
                act(av, av, AF.Exp, [B_Ta, B_cneg], [B_Ta], scale=cneg[:, dc:dc + 1])
                act(hv, av, AF.Square, [B_Ta], [B_h])
                act(hv, hv, AF.Sqrt, [B_h], [B_h], scale=-1.0, bias=1.0)
                tt("dve", bv, bv, xc, ALU.mult, [B_Tb, B_T1], [B_Tb])
                tt("dve", bv, bv, hv, ALU.mult, [B_Tb, B_h], [B_Tb])
                init = carry[:, dc:dc + 1] if mine else 0.0
                if d == 0:
                    P.op("dve", lambda e, hv=hv, av=av, bv=bv, init=init: e.tensor_tensor_scan(
                        out=hv, data0=av, data1=bv, initial=init, op0=ALU.mult, op1=ALU.add), [B_Ta, B_Tb, B_carry], [B_h])
                else:
                    P.op("dve", lambda e, hv=hv, av=av, bv=bv, init=init: e.tensor_tensor_scan(
                        out=rev(hv), data0=rev(av), data1=rev(bv), initial=init, op0=ALU.mult, op1=ALU.add), [B_Ta, B_Tb, B_carry], [B_h])
                if not mine:
                    endcol = hv[:, Lw - 1:Lw] if d == 0 else hv[:, 0:1]
                    P.op("dve", lambda e, endcol=endcol, dc=dc, w=w: e.tensor_copy(out=sumH[:, w * 16 + dc:w * 16 + dc + 1], in_=endcol), [B_h], [B_sum[w]])
                    P.op("dve", lambda e, av=av, dc=dc, w=w: e.tensor_reduce(out=sumA[:, w * 16 + dc:w * 16 + dc + 1], in_=av, axis=AX.X, op=ALU.mult), [B_Ta], [B_sum[w]])
            if not mine:
                continue
            tt("dve", Ts, Ts, Ts2, ALU.add, [B_Ts, B_Ts2], [B_Ts])
            w3, w_b = load_w(c)
            Tg = T0[:, 0:L]
            inproj(w3, w_b, HALO, HALO + L, lambda ps, pb, a, b: act(Tg[:, a - HALO:b - HALO], ps, AF.Copy, [pb], [B_T0]))
            Tq = Ta
            tt("dve", Tq, Tg, Tg, ALU.mult, [B_T0], [B_Ta])
            ts("dve", Tq, Tq, 0.044715, 1.0, ALU.mult, ALU.add, [B_Ta], [B_Ta])
            tt("dve", Tq, Tq, Tg, ALU.mult, [B_Ta, B_T0], [B_Ta])
            act(Tq, Tq, AF.Sigmoid, [B_Ta], [B_Ta], scale=1.5957691216057308)
            tt("dve", Tq, Tq, Tg, ALU.mult, [B_Ta, B_T0], [B_Ta])
            tt("dve", Tq, Tq, Ts, ALU.mult, [B_Ta, B_Ts], [B_Ta])
            tt("pool", Tb, Tq, Tq, ALU.mult, [B_Ta], [B_Tb])
            tt("pool", acc_a, acc_a, Tb, ALU.add, [B_Tb, B_acca], [B_acca])
            y_ap, y_b, y_s = yb_ring.next()
            act(y_ap, Tq, AF.Copy, [B_Ta, B_smallc], [y_b], scale=colp(C_GOA, c))
            dma("sp", y_d[c], y_ap, y_s, reads=[y_b])
        if not mine:
            P.barrier()
            if stop == f"w{w}":
                return finish_early()
            continue
        for c in range(8):
            Tc = T0
            w3, w_b = load_w(24 + c)
            inproj(w3, w_b, 0, W, lambda ps, pb, a, b: act(Tc[:, a:b], ps, AF.Copy, [pb], [B_T0]))
            w3, w_b = load_w(32 + c)
            inproj(w3, w_b, 0, W, lambda ps, pb, a, b: tt("dve", Tz[:, a:b], ps, Tc[:, a:b], ALU.mult, [pb, B_T0], [B_Ts, B_Ts2]))
            ts("dve", Tz[:, 0:HALO], Tz[:, 0:HALO], hm_l, None, ALU.mult, None, [B_Ts, B_Ts2, B_smallc], [B_Ts, B_Ts2])
            ts("dve", Tz[:, HALO + L:W], Tz[:, HALO + L:W], hm_r, None, ALU.mult, None, [B_Ts, B_Ts2, B_smallc], [B_Ts, B_Ts2])
            Tcv = T1
            w0 = colp(C_CBW, c * 3 + 0); w1 = colp(C_CBW, c * 3 + 1); w2 = colp(C_CBW, c * 3 + 2)
            ts("dve", Tcv, Tz[:, HALO:HALO + L], w1, None, ALU.mult, None, [B_Ts, B_Ts2, B_smallc], [B_T1])
            if c < 4:
                zv = Tz[:, HALO:HALO + L].rearrange("p (r c) -> p r c", c=64)
                ov = Tcv.rearrange("p (r c) -> p r c", c=64)
                stt("dve", ov[:, :, 1:64], zv[:, :, 0:63], w0, ov[:, :, 1:64], ALU.mult, ALU.add, [B_Ts, B_Ts2, B_T1, B_smallc], [B_T1])
                stt("dve", ov[:, :, 0:63], zv[:, :, 1:64], w2, ov[:, :, 0:63], ALU.mult, ALU.add, [B_Ts, B_Ts2, B_T1, B_smallc], [B_T1])
            else:
                stt("dve", Tcv, Tz[:, 0:L], w0, Tcv, ALU.mult, ALU.add, [B_Ts, B_Ts2, B_T1, B_smallc], [B_T1])
                stt("dve", Tcv, Tz[:, 2 * HALO:2 * HALO + L], w2, Tcv, ALU.mult, ALU.add, [B_Ts, B_Ts2, B_T1, B_smallc], [B_T1])
            w3, w_b = load_w(16 + c)
            Ty = Ta
            inproj(w3, w_b, HALO, HALO + L, lambda ps, pb, a, b: tt("dve", Ty[:, a - HALO:b - HALO], ps, Tcv[:, a - HALO:b - HALO], ALU.mult, [pb, B_T1], [B_Ta]))
            tt("pool", Tb, Ty, Ty, ALU.mult, [B_Ta], [B_Tb])
            tt("pool", acc_b, acc_b, Tb, ALU.add, [B_Tb, B_accb], [B_accb])
            y_ap, y_b, y_s = yb_ring.next()
            act(y_ap, Ty, AF.Copy, [B_Ta, B_smallc], [y_b], scale=colp(C_GOB, c))
            dma("sp", y_d[8 + c], y_ap, y_s, reads=[y_b])
        bk = nbank()
        for g, (acc, B_acc) in enumerate(((acc_a, B_acca), (acc_b, B_accb))):
            for i in range(16):
                j = g * 16 + i
                mmg([(PS[bk][:, 2 * j:2 * j + 2], acc[:, i * 128:(i + 1) * 128], ones_f[:, 0:2], True, True)], [B_acc, B_smallc], [PB[bk]])
        rstd_ab = smalld[:, SD_RSTD:SD_RSTD + 64]
        P.op("dve", lambda e, src=PS[bk][:, 0:64], dst=rstd_ab: e.tensor_copy(out=dst, in_=src), [PB[bk]], [B_rstd])
        rstd_chain(rstd_ab, rstd_ab, 1.0 / 1024, [B_rstd], [B_rstd])
    P.barrier()
    if stop == "p1":
        return finish_early()

    top[0] = persist_top
    wout = alloc_bf(16 * D, "wout").rearrange("p (c n) -> p c n", c=16)
    B_wout = Buf("wout")
    S_wout = newsem("wout")
    for c in range(16):
        dma("pool", wout[:, c, :], wout_d[c * 128:(c + 1) * 128, :], S_wout, writes=[B_wout])
    if stop == "p2a":
        return finish_early()
    ys_ring = Ring(2, 4096, "ysb", bf=True)
    xs2_ring = Ring(2, 2048, "xs2")
    gt1_b = alloc(2048, "gt1")
    B_gt1 = Buf("gt1")
    dma("sp", gt1_b, modb_d[2], S_gs, writes=[B_gt1])
    tA_ring = Ring(2, 512, "tA")
    tB_ring = Ring(2, 512, "tB")
    S_x1 = newsem("x1store")
    rstd_ab = smalld[:, SD_RSTD:SD_RSTD + 64]
    mine_row = WIN_ROW[4] + HALO
    ys3 = None
    for i in range(16):
        if i % 4 == 0:
            y_ap, y_b, y_s = ys_ring.next()
            ys3 = y_ap.rearrange("p (c n) -> p c n", c=16)
            for c in range(16):
                dma("sp", ys3[:, c, :], y_d[c][:, (i // 4) * 512:(i // 4 + 1) * 512], y_s, reads=[], writes=[y_b])
            ys_b = y_b
        x_ap, x_b, x_s = xs2_ring.next()
        dma("sp", x_ap, x_all[mine_row + i * 128: mine_row + (i + 1) * 128, :], x_s, writes=[x_b])
        to = (i % 4) * 128
        for n in range(4):
            bA = nbank(); bB = nbank()
            mmg([(PS[bA][:, :], ys3[:, c, to:to + 128], wout[:, c, n * 512:(n + 1) * 512], c == 0, c == 7) for c in range(8)], [ys_b, B_wout], [PB[bA]])
            mmg([(PS[bB][:, :], ys3[:, c, to:to + 128], wout[:, c, n * 512:(n + 1) * 512], c == 8, c == 15) for c in range(8, 16)], [ys_b, B_wout], [PB[bB]])
            ta, ta_b, _ = tA_ring.next()
            tb, tb_b, _ = tB_ring.next()
            act(ta, PS[bA][:, :], AF.Copy, [PB[bA], B_rstd], [ta_b], scale=rstd_ab[:, 2 * i:2 * i + 1])
            stt("dve", tb, PS[bB][:, :], rstd_ab[:, 32 + 2 * i:32 + 2 * i + 1], ta, ALU.mult, ALU.add, [PB[bB], B_rstd, ta_b], [tb_b])
            tt("pool", tb, tb, gt1_b[:, n * 512:(n + 1) * 512], ALU.mult, [tb_b, B_gt1], [tb_b])
            tt("pool", x_ap[:, n * 512:(n + 1) * 512], x_ap[:, n * 512:(n + 1) * 512], tb, ALU.add, [tb_b, x_b], [x_b])
        if stop == "p2b":
            return finish_early()
        dst = out_d if stage < 3 else x1_d
        dma("sp", dst[i * 128:(i + 1) * 128, :], x_ap, x_s, reads=[x_b])
    P.barrier()
    out_sems = [s for (_, _, s) in xs2_ring.slots]


    branched = (stage >= 3 and sparse)
    if branched:
        top[0] = persist_top
        xn2_d = nc.dram_tensor("xn2_scr", [16, 128, D], BF16).ap()
        flag_d = nc.dram_tensor("flag_scr", [1, 1], mybir.dt.int32).ap()
        wreg = wr_bf.bitcast(F32)
        wreg2 = wi_bf.bitcast(F32)
        G_all = wreg[:, 0:512].rearrange("p (i e) -> p i e", i=16)
        posm_all = wreg[:, 512:1024].rearrange("p (i e) -> p i e", i=16)
        posmT = wreg2
        B_Gall = Buf("Gall"); B_posm = Buf("posm"); B_posmT = Buf("posmT")
        run_c = smalld[:, SD_TMP + 96:SD_TMP + 128]
        maxc = smalld[:, SD_TMP + 128:SD_TMP + 160]
        flagf = smalld[:, SD_TMP + 160:SD_TMP + 162]
        flagi = smalld[:, SD_TMP + 162:SD_TMP + 163].bitcast(mybir.dt.int32)
        B_run = Buf("run"); B_maxc = Buf("maxc"); B_flag = Buf("flag")
        P.op("dve", lambda e: e.memset(run_c, 0.0), [], [B_run])
        P.op("dve", lambda e: e.memset(maxc, 0.0), [], [B_maxc])
        tri_bf = alloc_bf(128, "tribf"); one_bf = alloc_bf(128, "onebf")
        B_tri = Buf("tri")
        P.op("act", lambda e: e.activation(out=tri_bf, in_=smallc[:, C_TRI:C_TRI + 128], func=AF.Copy), [B_smallc], [B_tri])
        P.op("act", lambda e: e.activation(out=one_bf, in_=ones_f, func=AF.Copy), [B_smallc], [B_tri])
        xt_ring = Ring(2, 2048, "xtR")
        t1r = alloc(2048, "t1r"); B_t1r = Buf("t1r")
        xnr_ring = Ring(2, 1024, "xnr", bf=True)
        junkr = alloc_bf(2048, "junkr"); B_junkr = Buf("junkr")
        xtT_ring = Ring(2, 1024, "xtT", bf=True)
        modR0 = alloc(2048, "modR0"); modR1 = alloc(2048, "modR1")
        B_modR = Buf("modR")
        S_modR = newsem("modR")
        dma("sp", modR0, modb_d[4], S_modR, writes=[B_modR])
        dma("sp", modR1, modb_d[3], S_modR, writes=[B_modR])
        lgR = alloc(32, "lgR"); exR = alloc(32, "exR"); mkR = alloc(32, "mkR"); m8R = alloc(8, "m8R"); smR = alloc(4, "smR")
        psR = alloc(32, "posR"); mkbR = alloc_bf(32, "mkbR")
        B_lgR = Buf("lgR"); B_exR = Buf("exR"); B_mkR = Buf("mkR"); B_m8R = Buf("m8R"); B_smR = Buf("smR"); B_psR = Buf("psR"); B_mkbR = Buf("mkbR")
        brtR = smallc[:, C_BRT:C_BRT + 32]
        wrt3R = wrt_bf.rearrange("p (k e) -> p k e", k=16)
        for i in range(16):
            x_ap, x_b, x_s = xt_ring.next()
            dma("sp", x_ap, x1_d[i * 128:(i + 1) * 128, :], x_s, writes=[x_b])
            col = 16 + (i % 8)
            act(junkr, x_ap, AF.Square, [x_b], [B_junkr, B_ssq], accum_out=ssq[:, col:col + 1])
            rstd_chain(rs[:, col:col + 1], ssq[:, col:col + 1], 1.0 / D, [B_ssq], [B_rs])
            stt("dve", t1r, x_ap, rs[:, col:col + 1], modR0, ALU.mult, ALU.mult, [x_b, B_rs, B_modR], [B_t1r])
            n_ap, n_b, n_s = xnr_ring.next()
            tt("pool", n_ap, t1r, modR1, ALU.add, [B_t1r, B_modR], [n_b])
            dma("sp", xn2_d[i], n_ap, n_s, reads=[n_b])
            xT_ap, xT_b, _ = xtT_ring.next()
            xT3 = xT_ap.rearrange("p (k n) -> p k n", k=16)
            for kb in range(4):
                bk = nbank()
                pv = PS[bk][:, :].bitcast(BF16)
                trg([(pv[:, j * 128:(j + 1) * 128], n_ap[:, (kb * 4 + j) * 128:(kb * 4 + j + 1) * 128], ident_bf) for j in range(4)],
                    [n_b, B_ident], [PB[bk]])
                copy_any(xT3[:, kb * 4:kb * 4 + 4, :], pv[:, 0:512].rearrange("p (k n) -> p k n", k=4), [PB[bk]], [xT_b])
            bk = nbank()
            mmg([(PS[bk][:, 0:32], xT3[:, k, :], wrt3R[:, k, :], k == 0, k == 15) for k in range(16)], [xT_b, B_wrt], [PB[bk]])
            tt("dve", lgR, PS[bk][:, 0:32], brtR, ALU.add, [PB[bk], B_smallc], [B_lgR])
            P.op("dve", lambda e: e.max(out=m8R, in_=lgR), [B_lgR], [B_m8R])
            ts("dve", smR[:, 0:1], m8R[:, 0:1], -1.0, None, ALU.mult, None, [B_m8R], [B_smR])
            act(exR, lgR, AF.Exp, [B_lgR, B_smR], [B_exR], bias=smR[:, 0:1])
            ts("dve", mkR, lgR, m8R[:, 3:4], None, ALU.is_ge, None, [B_lgR, B_m8R], [B_mkR])
            tt("dve", exR, exR, mkR, ALU.mult, [B_exR, B_mkR], [B_exR])
            P.op("dve", lambda e: e.tensor_reduce(out=smR[:, 1:2], in_=exR, axis=AX.X, op=ALU.add), [B_exR], [B_smR])
            P.op("dve", lambda e: e.reciprocal(out=smR[:, 2:3], in_=smR[:, 1:2]), [B_smR], [B_smR])
            ts("dve", G_all[:, i, :], exR, smR[:, 2:3], None, ALU.mult, None, [B_exR, B_smR], [B_Gall])
            P.op("dve", lambda e: e.tensor_copy(out=mkbR, in_=mkR), [B_mkR], [B_mkbR])
            bA = nbank(); bB = nbank()
            mmg([(PS[bA][:, 0:32], tri_bf, mkbR, True, True)], [B_tri, B_mkbR], [PB[bA]])
            mmg([(PS[bB][:, 0:32], one_bf, mkbR, True, True)], [B_tri, B_mkbR], [PB[bB]])
            tt("dve", psR, PS[bA][:, 0:32], run_c, ALU.add, [PB[bA], B_run], [B_psR])
            tt("dve", psR, psR, mkR, ALU.mult, [B_psR, B_mkR], [B_psR])
            stt("dve", posm_all[:, i, :], psR, -1.0, mkR, ALU.add, ALU.add, [B_psR, B_mkR], [B_posm])
            tt("dve", run_c, PS[bB][:, 0:32], run_c, ALU.add, [PB[bB], B_run], [B_run])
            if i % 8 == 7:
                tt("dve", maxc, maxc, run_c, ALU.max, [B_maxc, B_run], [B_maxc])
                P.op("dve", lambda e: e.memset(run_c, 0.0), [], [B_run])
        P.op("dve", lambda e: e.tensor_reduce(out=flagf[:, 0:1], in_=maxc, axis=AX.X, op=ALU.max), [B_maxc], [B_flag])
        ts("dve", flagf[:, 1:2], flagf[:, 0:1], float(CAP), None, ALU.is_gt, None, [B_flag], [B_flag])
        P.op("dve", lambda e: e.tensor_copy(out=flagi, in_=flagf[:, 1:2]), [B_flag], [B_flag])
        S_flag = newsem("flag")
        B_flagd = Buf("flagd")
        dma("sp", flag_d[0:1, 0:1], flagi[0:1, 0:1], S_flag, reads=[B_flag], writes=[B_flagd])
        P.barrier()
        for en in ENGS:
            P.op(en, lambda e, en=en: e.reg_load(P.regs[en], flag_d[0:1, 0:1]), [B_flagd], [])
        snap = P.snapshot([bank_ctr, top, cp_ctr])
        P.region = "A"
        top[0] = persist_top
        accA = [alloc(2048, f"accA{i}") for i in range(8)]
        B_accA = [Buf(f"accA{i}") for i in range(8)]
        S_accA = [newsem(f"accA{i}") for i in range(8)]
        xn2_tok = alloc_bf(8 * D, "xn2tok").rearrange("p (j d) -> p j d", j=8)
        B_xtok = Buf("xtok"); S_xtok = newsem("xtok")
        selA = alloc_bf(8 * CAP, "selA").rearrange("p (j s) -> p j s", j=8)
        B_sel = Buf("sel")
        XeT_raw = alloc(8 * CAP, "XeT")
        XeT = XeT_raw.bitcast(BF16).rearrange("p (k s) -> p k s", k=16)
        B_XeT = Buf("XeT")
        actA_raw = alloc(8 * CAP, "actA")
        actA = actA_raw.bitcast(BF16).rearrange("p (f s) -> p f s", f=16)
        B_actA = Buf("actA")
        Ye_raw = alloc(1024 * NST, "Ye")
        Ye = Ye_raw.bitcast(BF16).rearrange("p (i d) -> p i d", i=NST)
        B_Ye = Buf("Ye")
        selT_raw = alloc(512 * NST, "selT")
        selT = selT_raw.bitcast(BF16).rearrange("p (i t) -> p i t", i=NST)
        B_selT = Buf("selT")
        wdA_ring = Ring(4, 1024, "wdA", bf=True)
        wguA_ring = Ring(4, 1024, "wguA", bf=True)
        S_modA1 = newsem("modA1"); S_gt2A = newsem("gt2A"); S_xtmp = newsem("xtmpA")
        Ee_ring = Ring(1, 128, "Ee")
        tgA = Ring(1, CAP, "tgA"); tsgA = Ring(1, CAP, "tsgA"); tuA = Ring(2, CAP, "tuA"); tdA = Ring(2, 512, "tdA")
        bgc = smallc[:, C_BG:C_BG + 512]
        buc = smallc[:, C_BU:C_BU + 512]
        iota_f = smallc[:, C_IOTA:C_IOTA + CAP]
        pidx = smallc[:, C_PIDX:C_PIDX + 128]
        print("branch A arena top", top[0], "of", ARENA)
        add_ctr = [0]

        def add_any(out, in0, in1, reads, writes):
            add_ctr[0] += 1
            return tt("dve", out, in0, in1, ALU.add, reads, writes)

        for h in range(2):
            for j in range(8):
                i = h * 8 + j
                dma("sp", xn2_tok[:, j, :], xn2_d[i], S_xtok, writes=[B_xtok])
            GTh = Ye_raw[:, 0:1024]
            for j in range(8):
                i = h * 8 + j
                bk = nbank()
                trg([(PS[bk][0:32, 0:128], G_all[:, i, :], ident_f)], [B_Gall, B_smallc], [PB[bk]])
                act(GTh[0:32, j * 128:(j + 1) * 128], PS[bk][0:32, 0:128], AF.Copy, [PB[bk]], [B_Ye])
                bk = nbank()
                trg([(PS[bk][0:32, 0:128], posm_all[:, i, :], ident_f)], [B_posm, B_smallc], [PB[bk]])
                act(posmT[0:32, j * 128:(j + 1) * 128], PS[bk][0:32, 0:128], AF.Copy, [PB[bk]], [B_posmT])
            bd_raw = selT_raw
            for mp in range(2):
                dma("sp", bd_raw[0:32, 0:1024], bd_d[:, mp * 1024:(mp + 1) * 1024], S_modA1, writes=[B_selT])
                for j in range(8):
                    for mm_ in range(2):
                        m = mp * 2 + mm_
                        bk = nbank()
                        mmg([(PS[bk][:, :], GTh[0:32, j * 128:(j + 1) * 128], bd_raw[0:32, mm_ * 512:(mm_ + 1) * 512], True, True)], [B_Ye, B_selT], [PB[bk]])
                        copy_any(accA[j][:, m * 512:(m + 1) * 512], PS[bk][:, :], [PB[bk]], [B_accA[j]])
            P.barrier()
            units = [(e_, f_, t_) for e_ in range(NEXP) for f_ in range(16) for t_ in range(2)]
            uslot = {}
            dslot = {}
            PF = 3

            def issue_wguA(u):
                e_, f_, t_ = units[u]
                u_ap, u_b, u_s = wguA_ring.next()
                dma("pool", u_ap, wgu_d[e_ * 16 + f_][:, t_ * 2048:(t_ + 1) * 2048], u_s, writes=[u_b])
                uslot[u] = (u_ap, u_b)

            def issue_wdA(e_, m8):
                d_ap, d_b, d_s = wdA_ring.next()
                d3_ = d_ap.rearrange("p (f n) -> p f n", f=16)
                dma("pool", d3_, wd_d[e_].rearrange("(f p) d -> p f d", p=128)[:, :, m8 * 128:(m8 + 1) * 128], d_s, writes=[d_b])
                dslot[(e_, m8)] = (d3_, d_b)

            for u in range(PF):
                issue_wguA(u)
            for u2 in range(NEXP * 16):
                ex_i, f = u2 // 16, u2 % 16
                if f == 0:
                    for j in range(8):
                        ts("dve", selA[:, j, :], iota_f, posm_all[:, h * 8 + j, ex_i:ex_i + 1], None, ALU.is_equal, None, [B_smallc, B_posm], [B_sel])
                    for k in range(16):
                        bk = nbank()
                        mmg([(PS[bk][:, 0:CAP], xn2_tok[:, j, k * 128:(k + 1) * 128], selA[:, j, :], j == 0, j == 7) for j in range(8)],
                            [B_xtok, B_sel], [PB[bk]])
                        copy_any(XeT[:, k, :], PS[bk][:, 0:CAP], [PB[bk]], [B_XeT])
                    e_ap, e_b, _ = Ee_ring.next()
                    ts("dve", e_ap[0:32, :], pidx[0:32, :], float(ex_i), None, ALU.is_equal, None, [B_smallc], [e_b])
                    for c2 in range(2):
                        bk = nbank()
                        mmg([(PS[bk][:, :], e_ap[0:32, :], posmT[0:32, c2 * 512:(c2 + 1) * 512], True, True)], [e_b, B_posmT], [PB[bk]])
                        for i2 in range(NST):
                            ts("dve", selT[:, i2, c2 * 512:(c2 + 1) * 512], PS[bk][:, :], smallc[:, C_SIDX + i2:C_SIDX + i2 + 1], None, ALU.is_equal, None,
                               [PB[bk], B_smallc], [B_selT])
                if f in (2, 6, 10, 14):
                    issue_wdA(ex_i, (f - 2) // 4)
                banks = []
                for t_ in range(2):
                    u = u2 * 2 + t_
                    if u + PF < len(units):
                        issue_wguA(u + PF)
                    u_ap, u_b = uslot.pop(u)
                    u3 = u_ap.rearrange("p (k j) -> p k j", k=16)
                    bk = nbank()
                    mmg([(PS[bk][:, 0:CAP], u3[:, k, :], XeT[:, k, :], k == 0, k == 15) for k in range(16)], [u_b, B_XeT], [PB[bk]])
                    banks.append(bk)
                bG, bU = banks
                tg, tg_b, _ = tgA.next(); tsg, tsg_b, _ = tsgA.next(); tu, tu_b, _ = tuA.next()
                cb = ex_i * 16 + f
                ts("dve", tg, PS[bG][:, 0:CAP], bgc[:, cb:cb + 1], 7.0, ALU.add, ALU.min, [PB[bG], B_smallc], [tg_b])
                act(tsg, tg, AF.Sigmoid, [tg_b], [tsg_b], scale=1.702)
                act(tu, PS[bU][:, 0:CAP], AF.Identity, [PB[bU], B_smallc], [tu_b], bias=buc[:, cb:cb + 1])
                ts("dve", tu, tu, 7.0, -7.0, ALU.min, ALU.max, [tu_b], [tu_b])
                stt("dve", tu, tu, 1.0, tg, ALU.add, ALU.mult, [tu_b, tg_b], [tu_b])
                tt("dve", actA[:, f, :], tu, tsg, ALU.mult, [tu_b, tsg_b], [B_actA])
                if f != 15:
                    continue
                for m8 in range(16):
                    d3, d_b = dslot.pop((ex_i, m8))
                    for i2 in range(NST):
                        bk = nbank()
                        mmg([(PS[bk][:, 0:128], actA[:, ff, i2 * 128:(i2 + 1) * 128], d3[:, ff, :], ff == 0, ff == 15) for ff in range(16)], [B_actA, d_b], [PB[bk]])
                        copy_any(Ye[:, i2, m8 * 128:(m8 + 1) * 128], PS[bk][:, 0:128], [PB[bk]], [B_Ye])
                    if m8 + 4 < 16:
                        issue_wdA(ex_i, m8 + 4)
                for jt in range(8):
                    for m in range(4):
                        bk = nbank()
                        mmg([(PS[bk][:, :], selT[:, i2, jt * 128:(jt + 1) * 128], Ye[:, i2, m * 512:(m + 1) * 512], i2 == 0, i2 == NST - 1) for i2 in range(NST)],
                            [B_selT, B_Ye], [PB[bk]])
                        td, td_b, _ = tdA.next()
                        P.op("act", lambda e, td=td, src=PS[bk][:, :], sc_=G_all[:, h * 8 + jt, ex_i:ex_i + 1]: e.activation(out=td, in_=src, func=AF.Copy, scale=sc_),
                             [PB[bk], B_Gall], [td_b])
                        add_any(accA[jt][:, m * 512:(m + 1) * 512], accA[jt][:, m * 512:(m + 1) * 512], td, [td_b, B_accA[jt]], [B_accA[jt]])
            P.barrier()
            gfinA = Ye_raw[:, 0:2048]
            gt2A = XeT_raw[:, 0:2048]
            xtmp = actA_raw[:, 0:2048]
            B_xtmp = Buf("xtmpA")
            dma("sp", gfinA, gfin_d[:, :], S_modA1, writes=[B_Ye])
            dma("sp", gt2A, modb_d[5], S_gt2A, writes=[B_XeT])
            junkA = selT_raw.bitcast(BF16)[:, 0:2048]
            for j in range(8):
                r0 = (h * 8 + j) * 128
                dma("sp", xtmp, x1_d[r0:r0 + 128, :], S_xtmp, writes=[B_xtmp])
                tt("dve", accA[j], accA[j], gt2A, ALU.mult, [B_accA[j], B_XeT], [B_accA[j]])
                tt("pool", accA[j], accA[j], xtmp, ALU.add, [B_accA[j], B_xtmp], [B_accA[j]])
                act(junkA, accA[j], AF.Square, [B_accA[j]], [B_selT, B_ssq], accum_out=ssq[:, 8 + j:9 + j])
                rstd_chain(rs[:, 8 + j:9 + j], ssq[:, 8 + j:9 + j], 1.0 / D, [B_ssq], [B_rs])
                stt("dve", accA[j], accA[j], rs[:, 8 + j:9 + j], gfinA, ALU.mult, ALU.mult, [B_accA[j], B_rs, B_Ye], [B_accA[j]])
                dma("sp", out_d[r0:r0 + 128, :], accA[j], S_accA[j], reads=[B_accA[j]])
            P.barrier()
        P.final_wait("sp", S_accA)
        P.restore(snap)
        P.region = "B"

    if stage >= 3:
        top[0] = persist_top
        acc = [alloc(2048, f"acc{i}") for i in range(4)]
        B_accs = [Buf(f"acc{i}") for i in range(4)]
        S_acc = [newsem(f"acc{i}") for i in range(4)]
        xn2T = alloc_bf(16 * 512, "xn2T").rearrange("p (k n) -> p k n", k=16)
        B_xn2T = Buf("xn2T")
        actb = alloc_bf(16 * 512, "actb").rearrange("p (f n) -> p f n", f=16)
        B_actb = Buf("actb")
        wd_ring = Ring(3, 4096, "wd", bf=True)
        wgu_ring = Ring(4, 2048, "wgu", bf=True)
        mod0 = alloc(2048, "mod0"); mod1 = alloc(2048, "mod1")
        B_mod0 = Buf("mod0"); B_mod1 = Buf("mod1")
        S_mod0 = newsem("mod0"); S_mod1 = newsem("mod1"); S_bd = newsem("bd")
        bd_sb = alloc(2048, "bd")
        B_bd = Buf("bd")
        dma("sp", bd_sb[0:32, :], bd_d[:, :], S_bd, writes=[B_bd])
        G = alloc(128, "G").rearrange("p (i e) -> p i e", i=4)
        B_G = Buf("G")
        GT = alloc(512, "GT")
        B_GT = Buf("GT")
        lg = alloc(32, "lg"); ex = alloc(32, "ex"); mk = alloc(32, "mk"); m8 = alloc(8, "m8"); sm = alloc(4, "sm")
        B_lg = Buf("lg"); B_ex = Buf("ex"); B_mk = Buf("mk"); B_m8 = Buf("m8"); B_sm = Buf("sm")
        tmp_top = top[0]
        t1m = alloc(2048, "t1m"); B_t1m = Buf("t1m")
        xnbm = alloc_bf(2048, "xnbm"); B_xnbm = Buf("xnbm")
        junkm = alloc_bf(2048, "junkm"); B_junkm = Buf("junkm")
        top[0] = tmp_top
        tg_ring = Ring(2, 512, "tg"); tsg_ring = Ring(2, 512, "tsg"); tu_ring = Ring(2, 512, "tu"); td_ring = Ring(2, 512, "td")
        bgc = smallc[:, C_BG:C_BG + 512]
        buc = smallc[:, C_BU:C_BU + 512]
        brt = smallc[:, C_BRT:C_BRT + 32]
        out_sems = S_acc
        for q in range(nq):
            dma("sp", mod0, modb_d[4], S_mod0, writes=[B_mod0])
            dma("sp", mod1, modb_d[3], S_mod1, writes=[B_mod1])
            for i in range(4):
                r0 = (q * 4 + i) * 128
                dma("sp", acc[i], x1_d[r0:r0 + 128, :], S_acc[i], writes=[B_accs[i]])
                act(junkm, acc[i], AF.Square, [B_accs[i]], [B_junkm, B_ssq], accum_out=ssq[:, i:i + 1])
                rstd_chain(rs[:, i:i + 1], ssq[:, i:i + 1], 1.0 / D, [B_ssq], [B_rs])
                stt("dve", t1m, acc[i], rs[:, i:i + 1], mod0, ALU.mult, ALU.mult, [B_accs[i], B_rs, B_mod0], [B_t1m])
                tt("pool", xnbm, t1m, mod1, ALU.add, [B_t1m, B_mod1], [B_xnbm])
                for kb in range(4):
                    bk = nbank()
                    pv = PS[bk][:, :].bitcast(BF16)
                    trg([(pv[:, j * 128:(j + 1) * 128], xnbm[:, (kb * 4 + j) * 128:(kb * 4 + j + 1) * 128], ident_bf) for j in range(4)],
                        [B_xnbm, B_ident], [PB[bk]])
                    copy_any(xn2T[:, kb * 4:kb * 4 + 4, i * 128:(i + 1) * 128],
                             pv[:, 0:512].rearrange("p (k n) -> p k n", k=4), [PB[bk]], [B_xn2T])
                bk = nbank()
                wrt3 = wrt_bf.rearrange("p (k e) -> p k e", k=16)
                mmg([(PS[bk][:, 0:32], xn2T[:, k, i * 128:(i + 1) * 128], wrt3[:, k, :], k == 0, k == 15) for k in range(16)],
                    [B_xn2T, B_wrt], [PB[bk]])
                tt("dve", lg, PS[bk][:, 0:32], brt, ALU.add, [PB[bk], B_smallc], [B_lg])
                P.op("dve", lambda e: e.max(out=m8, in_=lg), [B_lg], [B_m8])
                ts("dve", sm[:, 0:1], m8[:, 0:1], -1.0, None, ALU.mult, None, [B_m8], [B_sm])
                act(ex, lg, AF.Exp, [B_lg, B_sm], [B_ex], bias=sm[:, 0:1])
                ts("dve", mk, lg, m8[:, 3:4], None, ALU.is_ge, None, [B_lg, B_m8], [B_mk])
                tt("dve", ex, ex, mk, ALU.mult, [B_ex, B_mk], [B_ex])
                P.op("dve", lambda e: e.tensor_reduce(out=sm[:, 1:2], in_=ex, axis=AX.X, op=ALU.add), [B_ex], [B_sm])
                P.op("dve", lambda e: e.reciprocal(out=sm[:, 2:3], in_=sm[:, 1:2]), [B_sm], [B_sm])
                ts("dve", G[:, i, :], ex, sm[:, 2:3], None, ALU.mult, None, [B_ex, B_sm], [B_G])
                bk = nbank()
                trg([(PS[bk][0:32, 0:128], G[:, i, :], ident_f)], [B_G, B_smallc], [PB[bk]])
                act(GT[0:32, i * 128:(i + 1) * 128], PS[bk][0:32, 0:128], AF.Copy, [PB[bk]], [B_GT])
            P.barrier()
            dma("sp", mod0, modb_d[5], S_mod0, writes=[B_mod0])
            dma("sp", mod1, gfin_d[:, :], S_mod1, writes=[B_mod1])
            for i in range(4):
                for m in range(4):
                    bk = nbank()
                    mmg([(PS[bk][:, :], GT[0:32, i * 128:(i + 1) * 128], bd_sb[0:32, m * 512:(m + 1) * 512], True, True)], [B_GT, B_bd], [PB[bk]])
                    td, td_b, _ = td_ring.next()
                    tt("dve", td, PS[bk][:, :], mod0[:, m * 512:(m + 1) * 512], ALU.mult, [PB[bk], B_mod0], [td_b])
                    tt("pool", acc[i][:, m * 512:(m + 1) * 512], acc[i][:, m * 512:(m + 1) * 512], td, ALU.add, [td_b, B_accs[i]], [B_accs[i]])
            units = [(e_, f_) for e_ in range(NEXP) for f_ in range(16)]
            uslot = {}
            dslot = {}
            PF = 3

            def issue_wgu(u):
                e_, f_ = units[u]
                u_ap, u_b, u_s = wgu_ring.next()
                dma("pool", u_ap, wgu_d[e_ * 16 + f_], u_s, writes=[u_b])
                uslot[u] = (u_ap, u_b)

            def issue_wd(e_, m_):
                d_ap, d_b, d_s = wd_ring.next()
                d3_ = d_ap.rearrange("p (f n) -> p f n", f=16)
                dma("pool", d3_, wd_d[e_].rearrange("(f p) d -> p f d", p=128)[:, :, m_ * 512:(m_ + 1) * 512], d_s, writes=[d_b])
                dslot[(e_, m_)] = (d3_, d_b)

            for u in range(PF):
                issue_wgu(u)
            for u, (ex_i, f) in enumerate(units):
                if u + PF < len(units):
                    issue_wgu(u + PF)
                if f in (3, 7, 11):
                    issue_wd(ex_i, (f - 3) // 4)
                u_ap, u_b = uslot.pop(u)
                u4 = u_ap.rearrange("p (t k j) -> p t k j", t=2, k=16)
                bG = nbank(); bU = nbank()
                mmg([(PS[bG][:, :], u4[:, 0, k, :], xn2T[:, k, :], k == 0, k == 15) for k in range(16)], [u_b, B_xn2T], [PB[bG]])
                mmg([(PS[bU][:, :], u4[:, 1, k, :], xn2T[:, k, :], k == 0, k == 15) for k in range(16)], [u_b, B_xn2T], [PB[bU]])
                tg, tg_b, _ = tg_ring.next(); tsg, tsg_b, _ = tsg_ring.next(); tu, tu_b, _ = tu_ring.next()
                cb = ex_i * 16 + f
                ts("dve", tg, PS[bG][:, :], bgc[:, cb:cb + 1], 7.0, ALU.add, ALU.min, [PB[bG], B_smallc], [tg_b])
                act(tsg, tg, AF.Sigmoid, [tg_b], [tsg_b], scale=1.702)
                act(tu, PS[bU][:, :], AF.Identity, [PB[bU], B_smallc], [tu_b], bias=buc[:, cb:cb + 1])
                ts("dve", tu, tu, 7.0, -7.0, ALU.min, ALU.max, [tu_b], [tu_b])
                stt("dve", tu, tu, 1.0, tg, ALU.add, ALU.mult, [tu_b, tg_b], [tu_b])
                tt("pool", actb[:, f, :], tu, tsg, ALU.mult, [tu_b, tsg_b], [B_actb])
                if f != 15:
                    continue
                for m in range(4):
                    d3, d_b = dslot.pop((ex_i, m))
                    for i in range(4):
                        bk = nbank()
                        mmg([(PS[bk][:, :], actb[:, ff, i * 128:(i + 1) * 128], d3[:, ff, :], ff == 0, ff == 15) for ff in range(16)], [B_actb, d_b], [PB[bk]])
                        td, td_b, _ = td_ring.next()
                        stt("dve", td, PS[bk][:, :], G[:, i, ex_i:ex_i + 1], mod0[:, m * 512:(m + 1) * 512], ALU.mult, ALU.mult, [PB[bk], B_G, B_mod0], [td_b])
                        tt("pool", acc[i][:, m * 512:(m + 1) * 512], acc[i][:, m * 512:(m + 1) * 512], td, ALU.add, [td_b, B_accs[i]], [B_accs[i]])
                    if m == 0:
                        issue_wd(ex_i, 3)
            P.barrier()
            for i in range(4):
                r0 = (q * 4 + i) * 128
                act(junkm, acc[i], AF.Square, [B_accs[i]], [B_junkm, B_ssq], accum_out=ssq[:, 8 + i:9 + i])
                rstd_chain(rs[:, 8 + i:9 + i], ssq[:, 8 + i:9 + i], 1.0 / D, [B_ssq], [B_rs])
                stt("dve", acc[i], acc[i], rs[:, 8 + i:9 + i], mod1, ALU.mult, ALU.mult, [B_accs[i], B_rs, B_mod1], [B_accs[i]])
                dma("sp", out_d[r0:r0 + 128, :], acc[i], S_acc[i], reads=[B_accs[i]])
            P.barrier()
    P.final_wait("sp", out_sems)
    with nc.Block() as block:
        P.emit(block, branched=branched)
    es.close()
    return nc


_NC_CACHE = {}


def _prep_shared(inp, stage):
    f = np.float32
    sh = {}
    sh["w_mod"] = np.ascontiguousarray(inp["w_mod"][0], dtype=f)
    sh["b_mod"] = np.ascontiguousarray(inp["b_mod"][0].reshape(1, -1), dtype=f)
    sh["g_mix_b"] = np.ascontiguousarray(np.broadcast_to(inp["g_mix"][0], (128, D)), dtype=f)
    sh["g_ffn_b"] = np.ascontiguousarray(np.broadcast_to(inp["g_ffn"][0], (128, D)), dtype=f)
    sh["g_fin_b"] = np.ascontiguousarray(np.broadcast_to(inp["g_final"], (128, D)), dtype=f)
    w_in = np.asarray(inp["w_in"][0], dtype=f)
    sh["w_in_h"] = np.ascontiguousarray(w_in.reshape(16, 128, 40, 128).transpose(2, 1, 0, 3)).reshape(40, 128, 2048)
    sh["wr_h"] = np.ascontiguousarray(np.asarray(inp["lru_w_r"][0], dtype=f).transpose(2, 0, 1, 3)).reshape(128, 2048)
    sh["wi_h"] = np.ascontiguousarray(np.asarray(inp["lru_w_i"][0], dtype=f).transpose(2, 0, 1, 3)).reshape(128, 2048)
    sh["w_out"] = np.ascontiguousarray(inp["w_out"][0], dtype=f)
    if stage >= 3:
        sh["w_router_h"] = np.ascontiguousarray(np.asarray(inp["w_router"][0], dtype=f).reshape(16, 128, 32).transpose(1, 0, 2)).reshape(128, 512)
        wg = np.asarray(inp["w_gate"][0], dtype=f).reshape(NEXP, 16, 128, 16, 128)
        wu = np.asarray(inp["w_up"][0], dtype=f).reshape(NEXP, 16, 128, 16, 128)
        wgu = np.empty((NEXP, 16, 128, 2, 16, 128), dtype=f)
        wgu[:, :, :, 0] = wg.transpose(0, 3, 2, 1, 4)
        wgu[:, :, :, 1] = wu.transpose(0, 3, 2, 1, 4)
        sh["wgu_h"] = wgu.reshape(NEXP * 16, 128, 4096)
        sh["w_down"] = np.ascontiguousarray(inp["w_down"][0], dtype=f)
        sh["b_down"] = np.ascontiguousarray(inp["b_down"][0], dtype=f)
    return sh


def _prep_core(inp, k):
    f = np.float32
    b, j = k // 4, k % 4
    x = np.asarray(inp["x"], dtype=f)
    ctx = np.asarray(inp["ctx"], dtype=f)
    S = x.shape[1]
    x_all = np.zeros((XROWS, D), dtype=f)
    x_all[HALO:HALO + LC] = ctx[b]
    others = [jj for jj in range(4) if jj != j]
    chunks_ = others + [j]
    hm = np.zeros(10, dtype=f)
    for wi, jj in enumerate(chunks_):
        w = wi + 1
        t0 = jj * L - HALO
        lo, hi = max(t0, 0), min(t0 + W, S)
        r0 = WIN_ROW[w] + (lo - t0)
        x_all[r0:r0 + (hi - lo)] = x[b, lo:hi]
        hm[2 * w] = 1.0 if jj > 0 else 0.0
        hm[2 * w + 1] = 1.0 if jj < 3 else 0.0
    cm = np.zeros(6, dtype=f)
    for o, jj in enumerate(others):
        cm[o] = 1.0 if jj < j else 0.0
        cm[3 + o] = 1.0 - cm[o]
    sc = np.zeros((128, NS), dtype=f)

    def colT(v, n):
        return np.asarray(v, dtype=f).reshape(n, 128).T

    sc[:, C_CT:C_CT + 16] = colT(inp["c"][b], 16)
    sc[:, C_CX:C_CX + 16] = colT(inp["c_ctx"], 16)
    caw = np.asarray(inp["conv_a_w"][0], dtype=f)
    sc[:, C_CAW:C_CAW + 32] = caw.reshape(4, 8, 128).transpose(2, 1, 0).reshape(128, 32)
    sc[:, C_CAB:C_CAB + 8] = colT(inp["conv_a_b"][0], 8)
    sc[:, C_LBR:C_LBR + 16] = colT(np.asarray(inp["lru_b_r"][0]).reshape(-1), 16)
    sc[:, C_LBI:C_LBI + 16] = colT(np.asarray(inp["lru_b_i"][0]).reshape(-1), 16)
    sc[:, C_LAM:C_LAM + 16] = colT(np.asarray(inp["lru_lam"][0]).reshape(-1), 16)
    cbw = np.asarray(inp["conv_b_w"][0], dtype=f)
    sc[:, C_CBW:C_CBW + 24] = cbw.reshape(3, 8, 128).transpose(2, 1, 0).reshape(128, 24)
    sc[:, C_GOA:C_GOA + 8] = colT(inp["g_out_a"][0], 8)
    sc[:, C_GOB:C_GOB + 8] = colT(inp["g_out_b"][0], 8)
    sc[:, C_HM:C_HM + 10] = hm[None, :]
    sc[:, C_CM:C_CM + 6] = cm[None, :]
    sc[:, C_BG:C_BG + 512] = np.asarray(inp["b_gate"][0], dtype=f).reshape(NEXP, 16, 128).transpose(2, 0, 1).reshape(128, 512)
    sc[:, C_BU:C_BU + 512] = np.asarray(inp["b_up"][0], dtype=f).reshape(NEXP, 16, 128).transpose(2, 0, 1).reshape(128, 512)
    sc[:, C_BRT:C_BRT + 32] = np.asarray(inp["b_router"][0], dtype=f)[None, :]
    sc[:, C_ID:C_ID + 128] = np.eye(128, dtype=f)
    sc[:, C_ONE:C_ONE + 128] = 1.0
    pp = np.arange(128, dtype=f)
    sc[:, C_TRI:C_TRI + 128] = (pp[:, None] < pp[None, :]).astype(f)
    sc[:, C_PIDX:C_PIDX + 128] = pp[:, None]
    sc[:, C_IOTA:C_IOTA + CAP] = np.arange(CAP, dtype=f)[None, :]
    sc[:, C_SIDX:C_SIDX + NST] = pp[:, None] + (128.0 * np.arange(NST, dtype=f))[None, :]
    return {"x_all": x_all, "smallc": sc}


def run(inputs, stage=3, stop=None):
    if (stage, stop) not in _NC_CACHE:
        _NC_CACHE[(stage, stop)] = build(stage, stop)
    nc = _NC_CACHE[(stage, stop)]
    shared = _prep_shared(inputs, stage)
    in_maps = []
    for k in range(NCORE):
        m = dict(shared)
        m.update(_prep_core(inputs, k))
        in_maps.append(m)
    res = run_bass_kernel_spmd(nc, in_maps, core_ids=list(range(NCORE)))
    outs = [np.asarray(r["out"]) for r in res.results]
    return np.concatenate(outs, axis=0).reshape(2, 4 * L, D).astype(np.float32)


def kernel(**inputs):
    return run(inputs, stage=3)
```
